# Optimizing a Trainium2 kernel written in Bass

```python
import jax, jax.numpy as jnp
from jax import lax
import numpy as np

D_MODEL = 1024
BATCH = 8
SEQ = 4096
DEPTH = 2

D_MIX = D_MODEL
S5_WIDTH = D_MIX // 4
S5_GROUP = 16
S5_NGROUPS = S5_WIDTH // S5_GROUP
S5_STATE = 64
SSD_WIDTH = (3 * D_MIX) // 8
SSD_HEADDIM = 64
SSD_HEADS = SSD_WIDTH // SSD_HEADDIM
SSD_NGROUPS = 2
SSD_STATE = 128
SSD_CHUNK = 128
GDN_WIDTH = D_MIX - S5_WIDTH - SSD_WIDTH
GDN_HEADDIM = 64
GDN_HEADS = GDN_WIDTH // GDN_HEADDIM
GDN_CHUNK = 64
CONV_K = 4
D_FF = 3584
N_EXPERTS = 8
TOP_K = 2
N_DENSE = (DEPTH + 1) // 2
N_MOE = DEPTH // 2
EPS = 1e-6

SSD_CONV_DIM = SSD_WIDTH + 2 * SSD_NGROUPS * SSD_STATE
GDN_CONV_DIM = 3 * GDN_WIDTH
IN_SIZES = (S5_WIDTH, SSD_WIDTH, SSD_CONV_DIM, SSD_HEADS, GDN_CONV_DIM, GDN_WIDTH, GDN_HEADS, GDN_HEADS)
D_IN_PROJ = sum(IN_SIZES)
IN_SPLITS = tuple(sum(IN_SIZES[:i + 1]) for i in range(len(IN_SIZES) - 1))

kernel_name = "hymba_s5_ssd_gdn_moe_trunk"


def rms_norm(x, w):
    xf = x.astype(jnp.float32)
    y = xf * lax.rsqrt(jnp.mean(xf * xf, axis=-1, keepdims=True) + EPS)
    return (y * w.astype(jnp.float32)).astype(x.dtype)


def grouped_rms_norm(y, w, groups):
    shp = y.shape
    yg = y.reshape(shp[:-1] + (groups, shp[-1] // groups))
    yg = yg * lax.rsqrt(jnp.mean(yg * yg, axis=-1, keepdims=True) + EPS)
    return yg.reshape(shp) * w


def causal_conv(x, w):
    k, c = w.shape
    return lax.conv_general_dilated(x, w[:, None, :], window_strides=(1,), padding=[(k - 1, 0)],
                                    dimension_numbers=('NWC', 'WIO', 'NWC'), feature_group_count=c)


def swiglu(h, w_gate, w_up, w_down):
    return (jax.nn.silu(h @ w_gate) * (h @ w_up)) @ w_down


def s5_mixer(u, a_re, a_im, b_re, b_im, c_re, c_im, d_skip, log_step, w_glu, norm_w):
    a_re, a_im, b_re, b_im, c_re, c_im, d_skip, log_step, w_glu, norm_w = (
        t.astype(jnp.float32) for t in (a_re, a_im, b_re, b_im, c_re, c_im, d_skip, log_step, w_glu, norm_w))
    bsz, seq, _ = u.shape
    ug = u.reshape(bsz, seq, S5_NGROUPS, S5_GROUP)
    step = jnp.exp(log_step)[:, None]
    mag = jnp.exp(a_re * step)
    ang = a_im * step
    lb_re, lb_im = mag * jnp.cos(ang), mag * jnp.sin(ang)
    den = a_re * a_re + a_im * a_im
    f_re = ((lb_re - 1.0) * a_re + lb_im * a_im) / den
    f_im = (lb_im * a_re - (lb_re - 1.0) * a_im) / den
    bb_re = f_re[..., None] * b_re - f_im[..., None] * b_im
    bb_im = f_re[..., None] * b_im + f_im[..., None] * b_re
    bu_re = jnp.einsum('gph,blgh->blgp', bb_re, ug)
    bu_im = jnp.einsum('gph,blgh->blgp', bb_im, ug)
    ar = jnp.broadcast_to(lb_re, bu_re.shape)
    ai = jnp.broadcast_to(lb_im, bu_im.shape)

    def combine(e1, e2):
        a1r, a1i, b1r, b1i = e1
        a2r, a2i, b2r, b2i = e2
        return (a2r * a1r - a2i * a1i, a2r * a1i + a2i * a1r,
                a2r * b1r - a2i * b1i + b2r, a2r * b1i + a2i * b1r + b2i)

    _, _, s_re, s_im = lax.associative_scan(combine, (ar, ai, bu_re, bu_im), axis=1)
    y = (jnp.einsum('ghp,blgp->blgh', c_re, s_re) - jnp.einsum('ghp,blgp->blgh', c_im, s_im)
         + d_skip * ug).reshape(bsz, seq, S5_WIDTH)
    y = jax.nn.gelu(y)
    y = y * jax.nn.sigmoid(y @ w_glu)
    return rms_norm(y, norm_w)


def ssd_chunked(x, a, b, c):
    bsz, seq, nh, hp = x.shape
    ns = b.shape[-1]
    nc, q = seq // SSD_CHUNK, SSD_CHUNK
    x = x.reshape(bsz, nc, q, nh, hp)
    b = b.reshape(bsz, nc, q, nh, ns)
    c = c.reshape(bsz, nc, q, nh, ns)
    a_cs = jnp.cumsum(a.reshape(bsz, nc, q, nh).transpose(0, 1, 3, 2), axis=-1)
    causal = jnp.tril(jnp.ones((q, q), dtype=bool))
    decay = jnp.exp(jnp.where(causal, a_cs[..., :, None] - a_cs[..., None, :], -jnp.inf))
    scores = jnp.einsum('bclhn,bcshn->bchls', c, b) * decay
    y_diag = jnp.einsum('bchls,bcshp->bclhp', scores, x)
    decay_states = jnp.exp(a_cs[..., -1:] - a_cs).transpose(0, 1, 3, 2)
    states = jnp.einsum('bclhn,bclhp->bchpn', b * decay_states[..., None], x)
    chunk_decay = jnp.exp(a_cs[..., -1])

    def step(prev, inp):
        st, dec = inp
        return prev * dec[..., None, None] + st, prev

    init = jnp.zeros((bsz, nh, hp, ns), x.dtype)
    _, prev_states = lax.scan(step, init, (jnp.moveaxis(states, 1, 0), jnp.moveaxis(chunk_decay, 1, 0)))
    prev_states = jnp.moveaxis(prev_states, 0, 1)
    y_off = jnp.einsum('bclhn,bchpn->bclhp', c, prev_states) * jnp.exp(a_cs).transpose(0, 1, 3, 2)[..., None]
    return (y_diag + y_off).reshape(bsz, seq, nh, hp)


def ssd_mixer(z, xbc, dt, conv_w, conv_b, dt_bias, a_log, d_skip, norm_w):
    conv_w, conv_b, dt_bias, a_log, d_skip, norm_w = (
        t.astype(jnp.float32) for t in (conv_w, conv_b, dt_bias, a_log, d_skip, norm_w))
    bsz, seq, _ = xbc.shape
    xbc = jax.nn.silu(causal_conv(xbc, conv_w) + conv_b)
    xs, bm, cm = jnp.split(xbc, (SSD_WIDTH, SSD_WIDTH + SSD_NGROUPS * SSD_STATE), axis=-1)
    xs = xs.reshape(bsz, seq, SSD_HEADS, SSD_HEADDIM)
    rep = SSD_HEADS // SSD_NGROUPS
    bm = jnp.repeat(bm.reshape(bsz, seq, SSD_NGROUPS, SSD_STATE), rep, axis=2)
    cm = jnp.repeat(cm.reshape(bsz, seq, SSD_NGROUPS, SSD_STATE), rep, axis=2)
    dt = jax.nn.softplus(dt + dt_bias)
    a = -jnp.exp(a_log)
    y = ssd_chunked(xs * dt[..., None], dt * a, bm, cm)
    y = (y + d_skip[:, None] * xs).reshape(bsz, seq, SSD_WIDTH)
    return grouped_rms_norm(y * jax.nn.silu(z), norm_w, SSD_NGROUPS)


def gated_delta_chunked(q, k, v, g, beta):
    bsz, seq, nh, hd = q.shape
    cs, nc = GDN_CHUNK, seq // GDN_CHUNK

    def chunks(t):
        return t.reshape(bsz, nc, cs, nh, -1).transpose(0, 3, 1, 2, 4)

    q, k, v = chunks(q), chunks(k), chunks(v)
    beta = beta.reshape(bsz, nc, cs, nh).transpose(0, 3, 1, 2)
    g = jnp.cumsum(g.reshape(bsz, nc, cs, nh).transpose(0, 3, 1, 2), axis=-1)
    k_beta = k * beta[..., None]
    v_beta = v * beta[..., None]
    causal = jnp.tril(jnp.ones((cs, cs), dtype=bool))
    strict = jnp.tril(jnp.ones((cs, cs), dtype=bool), k=-1)
    decay = jnp.exp(jnp.where(causal, g[..., :, None] - g[..., None, :], -jnp.inf))
    lmat = jnp.where(strict, jnp.einsum('bhncd,bhnsd->bhncs', k_beta, k) * decay, 0.0)
    eye = jnp.eye(cs, dtype=q.dtype)
    t_inv = lax.linalg.triangular_solve(eye + lmat, jnp.broadcast_to(eye, lmat.shape),
                                        left_side=True, lower=True)
    value = jnp.einsum('bhncs,bhnsd->bhncd', t_inv, v_beta)
    k_cumdecay = jnp.einsum('bhncs,bhnsd->bhncd', t_inv, k_beta * jnp.exp(g)[..., None])
    qk_intra = jnp.where(causal, jnp.einsum('bhncd,bhnsd->bhncs', q, k) * decay, 0.0)
    q_decay = q * jnp.exp(g)[..., None]
    k_to_end = k * jnp.exp(g[..., -1:] - g)[..., None]
    chunk_decay = jnp.exp(g[..., -1])

    def step(state, inp):
        qk_i, val_i, kcd_i, qd_i, kend_i, dec_i = inp
        v_new = val_i - jnp.einsum('bhcd,bhde->bhce', kcd_i, state)
        o_i = jnp.einsum('bhcd,bhde->bhce', qd_i, state) + jnp.einsum('bhcs,bhse->bhce', qk_i, v_new)
        state = state * dec_i[..., None, None] + jnp.einsum('bhcd,bhce->bhde', kend_i, v_new)
        return state, o_i

    xs = tuple(jnp.moveaxis(t, 2, 0) for t in (qk_intra, value, k_cumdecay, q_decay, k_to_end, chunk_decay))
    init = jnp.zeros((bsz, nh, hd, hd), q.dtype)
    _, o = lax.scan(step, init, xs)
    return o.transpose(1, 0, 3, 2, 4).reshape(bsz, seq, nh, hd)


def gdn_mixer(qkv, z, beta_in, a_in, conv_w, a_log, dt_bias, norm_w):
    conv_w, a_log, dt_bias, norm_w = (t.astype(jnp.float32) for t in (conv_w, a_log, dt_bias, norm_w))
    bsz, seq, _ = qkv.shape
    qkv = jax.nn.silu(causal_conv(qkv, conv_w))
    q, k, v = (t.reshape(bsz, seq, GDN_HEADS, GDN_HEADDIM) for t in jnp.split(qkv, 3, axis=-1))
    q = q * lax.rsqrt(jnp.sum(q * q, axis=-1, keepdims=True) + EPS) * (GDN_HEADDIM ** -0.5)
    k = k * lax.rsqrt(jnp.sum(k * k, axis=-1, keepdims=True) + EPS)
    beta = jax.nn.sigmoid(beta_in)
    g = -jnp.exp(a_log) * jax.nn.softplus(a_in + dt_bias)
    o = gated_delta_chunked(q, k, v, g, beta)
    o = rms_norm(o, norm_w) * jax.nn.silu(z.reshape(bsz, seq, GDN_HEADS, GDN_HEADDIM))
    return o.reshape(bsz, seq, GDN_WIDTH)


def moe_ffn(h, w_router, w_gate, w_up, w_down):
    logits = (h @ w_router).astype(jnp.float32)
    top_v, top_i = lax.top_k(logits, TOP_K)
    top_w = jax.nn.softmax(top_v, axis=-1)
    combine = jnp.sum(jax.nn.one_hot(top_i, N_EXPERTS, dtype=jnp.float32) * top_w[..., None], axis=-2)
    out = jnp.zeros_like(h)
    for e in range(N_EXPERTS):
        out = out + combine[..., e:e + 1].astype(h.dtype) * swiglu(h, w_gate[e], w_up[e], w_down[e])
    return out


def setup_inputs(seed: int = 0) -> dict:
    key = jax.random.key(seed)
    ks = iter(jax.random.split(key, 48))

    def nrm(shape, scale):
        return jax.random.normal(next(ks), shape, jnp.float32) * scale

    def unif(shape, lo, hi):
        return jax.random.uniform(next(ks), shape, jnp.float32, lo, hi)

    def gain(shape):
        return 1.0 + nrm(shape, 0.01)

    def dt_bias_init(shape):
        dt = jnp.exp(unif(shape, float(np.log(1e-3)), float(np.log(1e-1))))
        return dt + jnp.log(-jnp.expm1(-dt))

    L_, G, P, H = DEPTH, S5_NGROUPS, S5_STATE, S5_GROUP
    n_idx = jnp.arange(P, dtype=jnp.float32)
    inp = {}
    inp['x'] = nrm((BATCH, SEQ, D_MODEL), 1.0)
    inp['norm_mix'] = gain((L_, D_MODEL))
    inp['w_in'] = nrm((L_, D_MODEL, D_IN_PROJ), D_MODEL ** -0.5)
    inp['w_out'] = nrm((L_, D_MIX, D_MODEL), D_MIX ** -0.5)
    inp['s5_a_re'] = -0.5 * jnp.exp(nrm((L_, G, P), 0.05))
    inp['s5_a_im'] = jnp.pi * n_idx + nrm((L_, G, P), 0.01)
    inp['s5_b_re'] = nrm((L_, G, P, H), (2.0 * H) ** -0.5)
    inp['s5_b_im'] = nrm((L_, G, P, H), (2.0 * H) ** -0.5)
    inp['s5_c_re'] = nrm((L_, G, H, P), (2.0 * P) ** -0.5)
    inp['s5_c_im'] = nrm((L_, G, H, P), (2.0 * P) ** -0.5)
    inp['s5_d'] = nrm((L_, G, H), 1.0)
    inp['s5_log_step'] = unif((L_, G), float(np.log(1e-3)), float(np.log(1e-1)))
    inp['s5_w_glu'] = nrm((L_, S5_WIDTH, S5_WIDTH), S5_WIDTH ** -0.5)
    inp['s5_norm'] = gain((L_, S5_WIDTH))
    inp['ssd_conv_w'] = nrm((L_, CONV_K, SSD_CONV_DIM), CONV_K ** -0.5)
    inp['ssd_conv_b'] = nrm((L_, SSD_CONV_DIM), 0.02)
    inp['ssd_dt_bias'] = dt_bias_init((L_, SSD_HEADS))
    inp['ssd_a_log'] = jnp.log(unif((L_, SSD_HEADS), 1.0, 16.0))
    inp['ssd_d'] = 1.0 + nrm((L_, SSD_HEADS), 0.1)
    inp['ssd_norm'] = gain((L_, SSD_WIDTH))
    inp['gdn_conv_w'] = nrm((L_, CONV_K, GDN_CONV_DIM), CONV_K ** -0.5)
    inp['gdn_a_log'] = jnp.log(unif((L_, GDN_HEADS), 1.0, 16.0))
    inp['gdn_dt_bias'] = dt_bias_init((L_, GDN_HEADS))
    inp['gdn_norm'] = gain((L_, GDN_HEADDIM))
    inp['norm_ffn'] = gain((L_, D_MODEL))
    inp['ff_w_gate'] = nrm((N_DENSE, D_MODEL, D_FF), D_MODEL ** -0.5)
    inp['ff_w_up'] = nrm((N_DENSE, D_MODEL, D_FF), D_MODEL ** -0.5)
    inp['ff_w_down'] = nrm((N_DENSE, D_FF, D_MODEL), D_FF ** -0.5)
    inp['moe_router'] = nrm((N_MOE, D_MODEL, N_EXPERTS), D_MODEL ** -0.5)
    inp['moe_w_gate'] = nrm((N_MOE, N_EXPERTS, D_MODEL, D_FF), D_MODEL ** -0.5)
    inp['moe_w_up'] = nrm((N_MOE, N_EXPERTS, D_MODEL, D_FF), D_MODEL ** -0.5)
    inp['moe_w_down'] = nrm((N_MOE, N_EXPERTS, D_FF, D_MODEL), D_FF ** -0.5)
    inp['norm_final'] = gain((D_MODEL,))
    return inp


def reference(x, norm_mix, w_in, w_out, s5_a_re, s5_a_im, s5_b_re, s5_b_im, s5_c_re, s5_c_im,
              s5_d, s5_log_step, s5_w_glu, s5_norm, ssd_conv_w, ssd_conv_b, ssd_dt_bias, ssd_a_log,
              ssd_d, ssd_norm, gdn_conv_w, gdn_a_log, gdn_dt_bias, gdn_norm, norm_ffn,
              ff_w_gate, ff_w_up, ff_w_down, moe_router, moe_w_gate, moe_w_up, moe_w_down, norm_final):
    for layer in range(DEPTH):
        hn = rms_norm(x, norm_mix[layer])
        proj = (hn @ w_in[layer]).astype(jnp.float32)
        u_s5, z_ssd, xbc_ssd, dt_ssd, qkv_gdn, z_gdn, b_gdn, a_gdn = jnp.split(proj, IN_SPLITS, axis=-1)
        y_s5 = s5_mixer(u_s5, s5_a_re[layer], s5_a_im[layer], s5_b_re[layer], s5_b_im[layer],
                        s5_c_re[layer], s5_c_im[layer], s5_d[layer], s5_log_step[layer],
                        s5_w_glu[layer], s5_norm[layer])
        y_ssd = ssd_mixer(z_ssd, xbc_ssd, dt_ssd, ssd_conv_w[layer], ssd_conv_b[layer],
                          ssd_dt_bias[layer], ssd_a_log[layer], ssd_d[layer], ssd_norm[layer])
        y_gdn = gdn_mixer(qkv_gdn, z_gdn, b_gdn, a_gdn, gdn_conv_w[layer], gdn_a_log[layer],
                          gdn_dt_bias[layer], gdn_norm[layer])
        mix = jnp.concatenate([y_s5, y_ssd, y_gdn], axis=-1).astype(x.dtype)
        x = x + mix @ w_out[layer]
        hn = rms_norm(x, norm_ffn[layer])
        i = layer // 2
        if layer % 2 == 0:
            x = x + swiglu(hn, ff_w_gate[i], ff_w_up[i], ff_w_down[i])
        else:
            x = x + moe_ffn(hn, moe_router[i], moe_w_gate[i], moe_w_up[i], moe_w_down[i])
    return rms_norm(x, norm_final)
```

```python
import contextlib
import numpy as np
import concourse.bass as bass
import concourse.mybir as mybir
from concourse.bass_utils import run_bass_kernel_spmd

F32 = mybir.dt.float32
BF16 = mybir.dt.bfloat16
I32 = mybir.dt.int32
ALU = mybir.AluOpType
AF = mybir.ActivationFunctionType
AX = mybir.AxisListType

ENGS = ("pe", "act", "dve", "pool", "sp")
NDMASEM = 8


class Op:
    __slots__ = ("eng", "fn", "deps", "sig", "is_dma", "sem", "semval", "barriered")

    def __init__(self, eng, fn, is_dma):
        self.eng = eng
        self.fn = fn
        self.deps = []
        self.sig = None
        self.is_dma = is_dma
        self.sem = None
        self.semval = None
        self.barriered = False


class Tile:
    _n = 0

    def __init__(self, t, name, psum=False):
        self.t = t
        self.name = name
        self.psum = psum
        Tile._n += 1
        self.id = Tile._n

    def __getitem__(self, k):
        return self.t[k]

    def __hash__(self):
        return self.id

    def __eq__(self, o):
        return self is o


class Prog:
    def __init__(self, nc, same_engine_sync=True):
        self.nc = nc
        self.ops = {e: [] for e in ENGS}
        self.lastw = {}
        self.readers = {}
        self.same_engine_sync = same_engine_sync
        self.ndma = {e: 0 for e in ENGS}
        self.dma_last = {}
        self.sb_off = 16640
        self.sb_max = 0
        self.nalloc = 0

    def sb(self, name, shape, dtype, align=64):
        nb = int(np.prod(shape[1:])) * mybir.dt.size(dtype)
        off = (self.sb_off + align - 1) // align * align
        self.nalloc += 1
        t = self.nc.alloc_sbuf_tensor_at(f"{name}_{self.nalloc}", list(shape), dtype, offset=off)
        self.sb_off = off + nb
        self.sb_max = max(self.sb_max, self.sb_off)
        assert self.sb_off <= 229376, f"SBUF overflow {self.sb_off} at {name}"
        return Tile(t, name)

    def mark(self):
        return self.sb_off

    def release(self, m):
        self.sb_off = m

    def op(self, eng, fn, r=(), w=(), is_dma=False):
        o = Op(eng, fn, is_dma)
        if any(isinstance(k, Tile) and k.psum for k in r):
            w = list(w) + [k for k in r if isinstance(k, Tile) and k.psum]
            r = [k for k in r if not (isinstance(k, Tile) and k.psum)]
        deps = {}
        for k in r:
            lw = self.lastw.get(k)
            if lw is not None:
                deps[id(lw)] = lw
        for k in w:
            lw = self.lastw.get(k)
            if lw is not None:
                deps[id(lw)] = lw
            for rd in self.readers.get(k, ()):
                deps[id(rd)] = rd
        if is_dma:
            slot = self.ndma[eng] % NDMASEM
            self.ndma[eng] += 1
            prev = self.dma_last.get((eng, slot))
            if prev is not None:
                deps[id(prev)] = prev
            self.dma_last[(eng, slot)] = o
            o.sem = (eng, slot)
        for d in deps.values():
            if d is o:
                continue
            if (not d.is_dma) and d.eng == eng:
                if eng == "pe" or not self.same_engine_sync:
                    continue
            o.deps.append(d)
        for k in r:
            lst = self.readers.setdefault(k, [])
            if not is_dma:
                lst[:] = [x for x in lst if x.is_dma or x.eng != eng]
            lst.append(o)
        for k in w:
            self.lastw[k] = o
            self.readers[k] = []
        self.ops[eng].append(o)
        return o

    def pe(self, fn, r=(), w=()):
        return self.op("pe", fn, r, w)

    def act(self, fn, r=(), w=()):
        return self.op("act", fn, r, w)

    def dve(self, fn, r=(), w=()):
        return self.op("dve", fn, r, w)

    def pool(self, fn, r=(), w=()):
        return self.op("pool", fn, r, w)

    def dma(self, out, in_, r=(), w=(), eng="sp", **kw):
        return self.op(eng, lambda e: e.dma_start(out, in_, **kw), r, w, is_dma=True)

    def barrier(self):
        tails = []
        for e in ENGS:
            if e == "sp":
                continue
            for o in reversed(self.ops[e]):
                if o.fn is not None and not o.is_dma:
                    tails.append(o)
                    break
        dmas = [o for e in ENGS for o in self.ops[e] if o.is_dma and not o.barriered]
        for o in dmas:
            o.barriered = True
        for e in ENGS:
            b = Op(e, None, False)
            b.deps = [t for t in tails if t.eng != e] + list(dmas)
            self.ops[e].append(b)
        self.lastw.clear()
        self.readers.clear()

    def emit(self, final_waits=()):
        nc = self.nc
        fin = Op("sp", None, False)
        fin.deps = list(final_waits)
        self.ops["sp"].append(fin)
        for e in ENGS:
            for o in self.ops[e]:
                for d in o.deps:
                    d.sig = True
        esem = {}
        dsem = {}
        with contextlib.ExitStack() as st:
            for e in ENGS:
                esem[e] = st.enter_context(nc.semaphore(f"s_{e}"))
            for e in ENGS:
                if self.ndma[e]:
                    for s in range(min(NDMASEM, self.ndma[e])):
                        dsem[(e, s)] = st.enter_context(nc.semaphore(f"d_{e}{s}"))
            dcount = {}
            for e in ENGS:
                c = 0
                for o in self.ops[e]:
                    if o.is_dma:
                        n = dcount.get(o.sem, 0) + 16
                        dcount[o.sem] = n
                        o.semval = n
                        o.sig = True
                    elif o.sig:
                        c += 1
                        o.semval = c
                        o.sem = e
                assert c < 60000, (e, c)
            maxd = max(dcount.values()) if dcount else 0
            assert maxd < 60000, maxd
            self.stats = {e: len(self.ops[e]) for e in ENGS}
            block = st.enter_context(nc.Block())
            engmap = {"pe": block.tensor, "act": block.scalar, "dve": block.vector,
                      "pool": block.gpsimd, "sp": block.sync}
            nw = [0]
            for e in ENGS:
                ops = self.ops[e]
                if not ops:
                    continue

                def body(eng, ops=ops, e=e):
                    seen = {}
                    for o in ops:
                        need = {}
                        for d in o.deps:
                            if d.semval is None:
                                continue
                            if seen.get(d.sem, 0) >= d.semval:
                                continue
                            if need.get(d.sem, 0) < d.semval:
                                need[d.sem] = d.semval
                        for s, v in need.items():
                            sh = esem[s] if isinstance(s, str) else dsem[s]
                            eng.wait_ge(sh, v)
                            seen[s] = v
                            nw[0] += 1
                        if o.fn is None:
                            continue
                        ins = o.fn(eng)
                        if o.is_dma:
                            ins.then_inc(dsem[o.sem], 16)
                        elif o.sig:
                            ins.then_inc(esem[e], 1)

                engmap[e](body)
            self.stats["waits"] = nw[0]
        return nc


D = 1024
DFF = 3584
NE = 8
EPS = 1e-6
NPROJ = 3200
KT = 8


class Ctx:
    pass


def setup_common(P, C):
    nc = P.nc
    C.ps = [Tile(nc.alloc_psum_tensor(f"psb{i}", [128, 512], F32), f"ps{i}", psum=True) for i in range(8)]
    C.ID = P.sb("ID", [128, 128], F32)
    C.IDb = P.sb("IDb", [128, 128], BF16)
    P.pool(lambda e: e.memset(C.ID[:], 1.0), w=[C.ID])
    P.pool(lambda e: e.affine_select(C.ID[:], C.ID[:], [[1, 128]], ALU.is_equal, 0.0, base=0,
                                     channel_multiplier=-1), r=[C.ID], w=[C.ID])
    P.dve(lambda e: e.tensor_copy(C.IDb[:], C.ID[:]), r=[C.ID], w=[C.IDb])
    C.ones = P.sb("ones", [128, 128], F32)
    P.pool(lambda e: e.memset(C.ones[:], 1.0), w=[C.ones])


def norm_tile(P, C, xt, nwbc, xn, ss, junk):
    P.act(lambda e: e.activation(junk[:], xt[:], AF.Square, accum_out=ss[:, 0:1]), r=[xt], w=[junk, ss])
    P.dve(lambda e: e.tensor_scalar(ss[:, 1:2], ss[:, 0:1], 1.0 / D, EPS, ALU.mult, ALU.add), r=[ss], w=[ss])
    P.act(lambda e: e.activation(ss[:, 2:3], ss[:, 1:2], AF.Sqrt), r=[ss], w=[ss])
    P.dve(lambda e: e.reciprocal(ss[:, 3:4], ss[:, 2:3]), r=[ss], w=[ss])
    P.dve(lambda e: e.scalar_tensor_tensor(xn[:], xt[:], ss[:, 3:4], nwbc[:], ALU.mult, ALU.mult),
          r=[xt, ss, nwbc], w=[xn])


def transpose_to_T(P, C, xn, banks, hnT, col0, hn32=None):
    for half in range(2):
        bk = banks[half]
        for j in range(4):
            k = half * 4 + j
            P.pe(lambda e, bk=bk, j=j, k=k: e.transpose(bk[:, j * 128:(j + 1) * 128], xn[:, k * 128:(k + 1) * 128], C.ID[:]),
                 r=[xn, C.ID], w=[bk])
        src = bk.t[:, :].rearrange("p (k t) -> p k t", k=4)
        dst = hnT.t[:, half * 4:half * 4 + 4, col0:col0 + 128]
        if half == 0:
            P.act(lambda e, dst=dst, src=src: e.copy(dst, src), r=[bk], w=[(hnT, col0, 0)])
        else:
            P.dve(lambda e, dst=dst, src=src: e.tensor_copy(dst, src), r=[bk], w=[(hnT, col0, 1)])
        if hn32 is not None:
            d32 = hn32.t[:, half * 4:half * 4 + 4, :]
            if half == 0:
                P.dve(lambda e, d32=d32, src=src: e.tensor_copy(d32, src), r=[bk], w=[(hn32, half)])
            else:
                P.act(lambda e, d32=d32, src=src: e.copy(d32, src), r=[bk], w=[(hn32, half)])


def phase_ffn(P, C, SEQ, xsrc, xdst, nw_d, wg_d, wu_d, wd_d, n_exp, wr_d=None, nfin_d=None, T=1024):
    m0 = P.mark()
    T = min(T, SEQ)
    NTB = T // 512
    moe = wr_d is not None
    FG = 512
    NFG = DFF // FG
    ps = C.ps
    nwbc = P.sb("nwbc", [128, D], F32)
    P.dma(nwbc[:], nw_d, w=[nwbc])
    if nfin_d is not None:
        nfbc = P.sb("nfbc", [128, D], F32)
        P.dma(nfbc[:], nfin_d, w=[nfbc])
    hnT = P.sb("hnT", [128, KT, T], BF16)
    acc = P.sb("acc", [128, KT, T], F32)
    hT = [P.sb(f"hT{i}", [128, 4, T], BF16) for i in range(2)]
    wg = [P.sb(f"wg{i}", [128, KT, FG], BF16) for i in range(2)]
    wu = [P.sb(f"wu{i}", [128, KT, FG], BF16) for i in range(2)]
    wd = [P.sb(f"wd{i}", [128, 4, D], BF16) for i in range(2)]
    sg = [P.sb(f"sg{i}", [128, 512], BF16) for i in range(2)]
    xt = [P.sb(f"xt{i}", [128, D], F32) for i in range(2)]
    xn = [P.sb(f"xn{i}", [128, D], F32) for i in range(2)]
    junk = P.sb("junk", [128, D], F32)
    ssb = [P.sb(f"ss{i}", [128, 8], F32) for i in range(2)]
    if moe:
        wr = P.sb("wr", [128, KT, NE], F32)
        P.dma(wr[:], wr_d.rearrange("(k p) e -> p k e", p=128), w=[wr])
        hn32 = [P.sb(f"hn32{i}", [128, KT, 128], F32) for i in range(2)]
        cbc = P.sb("cbc", [128, NE, T], BF16)
        rt = [P.sb(f"rt{i}", [128, 64], F32) for i in range(2)]
    nsb = SEQ // T
    wcount = 0
    for sbi in range(nsb):
        t0 = sbi * T
        for tt in range(T // 128):
            b = tt % 2
            P.dma(xt[b][:], xsrc[t0 + tt * 128:t0 + (tt + 1) * 128, :], w=[xt[b]])
            norm_tile(P, C, xt[b], nwbc, xn[b], ssb[b], junk)
            banks = (ps[4 + 2 * b], ps[5 + 2 * b])
            transpose_to_T(P, C, xn[b], banks, hnT, tt * 128, hn32[b] if moe else None)
            if moe:
                lg = ps[0] if b == 0 else ps[1]
                R = rt[b]
                for k in range(KT):
                    P.pe(lambda e, k=k, lg=lg, b=b: e.matmul(lg[:, 0:NE], hn32[b][:, k, :], wr[:, k, :], start=(k == 0), stop=(k == KT - 1)),
                         r=[(hn32[b], k // 4), wr], w=[lg])
                P.dve(lambda e, R=R, lg=lg: e.tensor_copy(R[:, 0:8], lg[:, 0:8]), r=[lg], w=[R])
                P.dve(lambda e, R=R: e.tensor_reduce(R[:, 8:9], R[:, 0:8], AX.X, ALU.max), r=[R], w=[R])
                P.dve(lambda e, R=R: e.tensor_scalar(R[:, 9:17], R[:, 0:8], R[:, 8:9], None, ALU.is_equal), r=[R], w=[R])
                P.dve(lambda e, R=R: e.scalar_tensor_tensor(R[:, 17:25], R[:, 9:17], -1e30, R[:, 0:8], ALU.mult, ALU.add), r=[R], w=[R])
                P.dve(lambda e, R=R: e.tensor_reduce(R[:, 25:26], R[:, 17:25], AX.X, ALU.max), r=[R], w=[R])
                P.dve(lambda e, R=R: e.tensor_scalar(R[:, 26:34], R[:, 17:25], R[:, 25:26], None, ALU.is_equal), r=[R], w=[R])
                P.dve(lambda e, R=R: e.tensor_tensor(R[:, 34:35], R[:, 25:26], R[:, 8:9], ALU.subtract), r=[R], w=[R])
                P.act(lambda e, R=R: e.activation(R[:, 35:36], R[:, 34:35], AF.Exp), r=[R], w=[R])
                P.dve(lambda e, R=R: e.tensor_scalar(R[:, 36:37], R[:, 35:36], 1.0, None, ALU.add), r=[R], w=[R])
                P.dve(lambda e, R=R: e.reciprocal(R[:, 37:38], R[:, 36:37]), r=[R], w=[R])
                P.dve(lambda e, R=R: e.tensor_tensor(R[:, 38:39], R[:, 35:36], R[:, 37:38], ALU.mult), r=[R], w=[R])
                P.dve(lambda e, R=R: e.tensor_scalar(R[:, 40:48], R[:, 9:17], R[:, 37:38], None, ALU.mult), r=[R], w=[R])
                P.dve(lambda e, R=R: e.scalar_tensor_tensor(R[:, 40:48], R[:, 26:34], R[:, 38:39], R[:, 40:48], ALU.mult, ALU.add), r=[R], w=[R])
                bb = ps[2] if b == 0 else ps[3]
                for half in range(2):
                    for j in range(4):
                        ex = half * 4 + j
                        P.pe(lambda e, ex=ex, j=j, R=R, bb=bb: e.matmul(bb[:, j * 128:(j + 1) * 128], R[:, 40 + ex:41 + ex].broadcast_to([128, 128]), C.ID[:], start=True, stop=True),
                             r=[R, C.ID], w=[bb])
                    src = bb.t[:, :].rearrange("p (k t) -> p k t", k=4)
                    dst = cbc.t[:, half * 4:half * 4 + 4, tt * 128:(tt + 1) * 128]
                    P.act(lambda e, dst=dst, src=src: e.copy(dst, src), r=[bb], w=[(cbc, tt)])
        nfg_total = n_exp * NFG

        def load_w(g, wb):
            ex, fg = divmod(g, NFG)
            f0 = fg * FG
            P.dma(wg[wb][:], wg_d[ex, :, f0:f0 + FG].rearrange("(k p) f -> p k f", p=128), w=[wg[wb]], eng="pool")
            P.dma(wu[wb][:], wu_d[ex, :, f0:f0 + FG].rearrange("(k p) f -> p k f", p=128), w=[wu[wb]], eng="pool")
            P.dma(wd[wb][:], wd_d[ex, f0:f0 + FG, :].rearrange("(c p) d -> p c d", p=128), w=[wd[wb]], eng="pool")
        for ex in range(n_exp):
            for fg in range(NFG):
                gi = ex * NFG + fg
                wb = wcount % 2
                wcount += 1
                f0 = fg * FG
                if gi == 0:
                    load_w(0, wb)
                if gi + 1 < nfg_total:
                    load_w(gi + 1, 1 - wb)
                hb = hT[wb]
                for fc in range(4):
                    for tb in range(NTB):
                        pg = ps[0 + (fc * NTB + tb) % 2]
                        pu = ps[2 + (fc * NTB + tb) % 2]
                        s = sg[(fc * NTB + tb) % 2]
                        rd = [(hnT, c * 128, h2) for c in range(tb * 4, tb * 4 + 4) for h2 in range(2)]
                        for k in range(KT):
                            P.pe(lambda e, k=k, pg=pg, wb=wb, fc=fc, tb=tb: e.matmul(pg[:], wg[wb][:, k, fc * 128:(fc + 1) * 128], hnT[:, k, tb * 512:(tb + 1) * 512], start=(k == 0), stop=(k == KT - 1)),
                                 r=[wg[wb]] + rd, w=[pg])
                        for k in range(KT):
                            P.pe(lambda e, k=k, pu=pu, wb=wb, fc=fc, tb=tb: e.matmul(pu[:], wu[wb][:, k, fc * 128:(fc + 1) * 128], hnT[:, k, tb * 512:(tb + 1) * 512], start=(k == 0), stop=(k == KT - 1)),
                                 r=[wu[wb]] + rd, w=[pu])
                        P.act(lambda e, s=s, pg=pg: e.activation(s[:], pg[:], AF.Silu), r=[pg], w=[s])
                        hdst = hb.t[:, fc, tb * 512:(tb + 1) * 512]
                        P.dve(lambda e, hdst=hdst, s=s, pu=pu: e.tensor_tensor(hdst, s[:], pu[:], ALU.mult), r=[s, pu], w=[(hb, fc, tb)])
                        if moe:
                            csrc = cbc.t[:, ex, tb * 512:(tb + 1) * 512]
                            P.dve(lambda e, hdst=hdst, csrc=csrc: e.tensor_tensor(hdst, hdst, csrc, ALU.mult),
                                   r=[(hb, fc, tb)] + [(cbc, c) for c in range(tb * 4, tb * 4 + 4)], w=[(hb, fc, tb)])
                for dc in range(KT):
                    for tb in range(NTB):
                        pd = ps[4 + (dc * NTB + tb) % 4]
                        for fc in range(4):
                            P.pe(lambda e, fc=fc, pd=pd, wb=wb, dc=dc, tb=tb, hb=hb: e.matmul(pd[:], wd[wb][:, fc, dc * 128:(dc + 1) * 128], hb[:, fc, tb * 512:(tb + 1) * 512], start=(fc == 0), stop=(fc == 3)),
                                 r=[wd[wb], (hb, fc, tb)], w=[pd])
                        adst = acc.t[:, dc, tb * 512:(tb + 1) * 512]
                        if gi == 0:
                            P.act(lambda e, adst=adst, pd=pd: e.copy(adst, pd[:]), r=[pd], w=[(acc, dc, tb)])
                        else:
                            P.dve(lambda e, adst=adst, pd=pd: e.tensor_tensor(adst, adst, pd[:], ALU.add), r=[pd, (acc, dc, tb)], w=[(acc, dc, tb)])
        for tt in range(T // 128):
            b = tt % 2
            tb = tt // 4
            P.dma(xt[b][:], xsrc[t0 + tt * 128:t0 + (tt + 1) * 128, :], w=[xt[b]])
            banks = (ps[0 + 2 * b], ps[1 + 2 * b])
            for half in range(2):
                bk = banks[half]
                for j in range(4):
                    k = half * 4 + j
                    P.pe(lambda e, bk=bk, j=j, k=k, tt=tt: e.transpose(bk[:, j * 128:(j + 1) * 128], acc[:, k, tt * 128:(tt + 1) * 128], C.ID[:]),
                         r=[(acc, k, tb), C.ID], w=[bk])
                P.dve(lambda e, bk=bk, half=half, b=b: e.tensor_tensor(xn[b][:, half * 512:(half + 1) * 512], xt[b][:, half * 512:(half + 1) * 512], bk[:], ALU.add),
                      r=[bk, xt[b]], w=[xn[b]] if half == 0 else [xn[b]])
            if nfin_d is not None:
                norm_tile(P, C, xn[b], nfbc, xt[b], ssb[b], junk)
                P.dma(xdst[t0 + tt * 128:t0 + (tt + 1) * 128, :], xt[b][:], r=[xt[b]], w=[("xdst", id(xdst), t0 + tt * 128)])
            else:
                P.dma(xdst[t0 + tt * 128:t0 + (tt + 1) * 128, :], xn[b][:], r=[xn[b]], w=[("xdst", id(xdst), t0 + tt * 128)])
    P.barrier()
    P.release(m0)


def phase_inproj(P, C, SEQ, xsrc, nw_d, win_d, proj_d):
    m0 = P.mark()
    ps = C.ps
    nwbc = P.sb("nwbc", [128, D], F32)
    P.dma(nwbc[:], nw_d, w=[nwbc])
    W = P.sb("Win", [128, KT, NPROJ], BF16)
    wv = win_d.rearrange("(k p) n -> p k n", p=128)
    for k in range(KT):
        for h in range(2):
            P.dma(W.t[:, k, h * 1600:(h + 1) * 1600], wv[:, k, h * 1600:(h + 1) * 1600], w=[(W, k, h)], eng="pool")
    wkeys = [(W, k, h) for k in range(KT) for h in range(2)]
    hnT = P.sb("hnT", [128, KT, SEQ], BF16)
    xt = [P.sb(f"xt{i}", [128, D], F32) for i in range(2)]
    xn = [P.sb(f"xn{i}", [128, D], F32) for i in range(2)]
    junk = P.sb("junk", [128, D], F32)
    ssb = [P.sb(f"ss{i}", [128, 8], F32) for i in range(2)]
    stg = [P.sb(f"stg{i}", [128, 512], F32) for i in range(4)]
    for tt in range(SEQ // 128):
        b = tt % 2
        P.dma(xt[b][:], xsrc[tt * 128:(tt + 1) * 128, :], w=[xt[b]])
        norm_tile(P, C, xt[b], nwbc, xn[b], ssb[b], junk)
        transpose_to_T(P, C, xn[b], (ps[4 + 2 * b], ps[5 + 2 * b]), hnT, tt * 128)
    n = 0
    for tb in range(SEQ // 512):
        rd = [(hnT, c * 128, h2) for c in range(tb * 4, tb * 4 + 4) for h2 in range(2)]
        for mc in range(NPROJ // 128):
            pb = ps[n % 4]
            s = stg[n % 4]
            for k in range(KT):
                P.pe(lambda e, k=k, pb=pb, mc=mc, tb=tb: e.matmul(pb[:], W[:, k, mc * 128:(mc + 1) * 128], hnT[:, k, tb * 512:(tb + 1) * 512], start=(k == 0), stop=(k == KT - 1)),
                     r=wkeys + rd, w=[pb])
            if n % 2 == 0:
                P.act(lambda e, s=s, pb=pb: e.copy(s[:], pb[:]), r=[pb], w=[s])
            else:
                P.dve(lambda e, s=s, pb=pb: e.tensor_copy(s[:], pb[:]), r=[pb], w=[s])
            P.dma(proj_d[mc * 128:(mc + 1) * 128, tb * 512:(tb + 1) * 512], s[:], r=[s], w=[("proj", mc, tb)])
            n += 1
    P.barrier()
    P.release(m0)


def phase_outproj(P, C, SEQ, xsrc, xdst, mix_d, wout_d):
    m0 = P.mark()
    ps = C.ps
    Wo = P.sb("Wo", [128, KT, D], BF16)
    P.dma(Wo[:], wout_d.rearrange("(k p) n -> p k n", p=128), w=[Wo], eng="pool")
    mT = P.sb("mT", [128, KT, SEQ], BF16)
    mv = mix_d.rearrange("(k p) t -> p k t", p=128)
    for k in range(KT):
        P.dma(mT.t[:, k, :], mv[:, k, :], w=[(mT, k)])
    mk = [(mT, k) for k in range(KT)]
    xt = [P.sb(f"xt{i}", [128, D], F32) for i in range(2)]
    xn = [P.sb(f"xn{i}", [128, D], F32) for i in range(2)]
    for tt in range(SEQ // 128):
        b = tt % 2
        P.dma(xt[b][:], xsrc[tt * 128:(tt + 1) * 128, :], w=[xt[b]])
        for half in range(2):
            pb = ps[(tt * 2 + half) % 4]
            for k in range(KT):
                P.pe(lambda e, k=k, pb=pb, half=half, tt=tt: e.matmul(pb[:], mT[:, k, tt * 128:(tt + 1) * 128], Wo[:, k, half * 512:(half + 1) * 512], start=(k == 0), stop=(k == KT - 1)),
                     r=mk + [Wo], w=[pb])
            P.dve(lambda e, pb=pb, half=half, b=b: e.tensor_tensor(xn[b][:, half * 512:(half + 1) * 512], xt[b][:, half * 512:(half + 1) * 512], pb[:], ALU.add),
                  r=[pb, xt[b]], w=[xn[b]])
        P.dma(xdst[tt * 128:(tt + 1) * 128, :], xn[b][:], r=[xn[b]], w=[("xo", tt)])
    P.barrier()
    P.release(m0)


TWO_PI = 6.283185307179586
CW1 = 6.28125
CW2 = TWO_PI - CW1
RMAGIC = 12582912.0
PI_SAFE = 3.1415925


def sincos(P, x, sin_o, cos_o, kt, ks, tmp, N):
    xk, sk, ck, kk, tk = ks
    for phase, o, ok in ((0.0, sin_o, sk), (0.25, cos_o, ck)):
        P.dve(lambda e, phase=phase: e.tensor_scalar(kt, x, 1.0 / TWO_PI, phase, ALU.mult, ALU.add), r=[xk], w=[kk])
        P.dve(lambda e: e.tensor_scalar(kt, kt, RMAGIC, RMAGIC, ALU.add, ALU.subtract), r=[kk], w=[kk])
        P.dve(lambda e: e.scalar_tensor_tensor(tmp, kt, -CW1, x, ALU.mult, ALU.add), r=[kk, xk], w=[tk])
        P.dve(lambda e: e.scalar_tensor_tensor(tmp, kt, -CW2, tmp, ALU.mult, ALU.add), r=[kk, tk], w=[tk])
        if phase:
            P.dve(lambda e: e.tensor_scalar(tmp, tmp, 0.25 * TWO_PI, PI_SAFE, ALU.add, ALU.min), r=[tk], w=[tk])
        else:
            P.dve(lambda e: e.tensor_scalar(tmp, tmp, PI_SAFE, None, ALU.min), r=[tk], w=[tk])
        P.dve(lambda e: e.tensor_scalar(tmp, tmp, -PI_SAFE, None, ALU.max), r=[tk], w=[tk])
        P.act(lambda e, o=o: e.activation(o, tmp, AF.Sin), r=[tk], w=[ok])


def mixer_s5(P, C, SEQ, proj_d, mix_d, W):
    m0 = P.mark()
    ps = C.ps
    Q = 512
    NCH = SEQ // Q
    prm = P.sb("s5prm", [128, 12, 8], F32)
    for i, nm in enumerate(("a_re_s", "a_im_s", "ls_s")):
        P.dma(prm.t[:, i, :], W[nm], w=[(prm, i)])
    pk = lambda i: (prm, i)
    P.act(lambda e: e.activation(prm.t[:, 3, :], prm.t[:, 2, :], AF.Exp), r=[pk(2)], w=[pk(3)])
    P.dve(lambda e: e.tensor_tensor(prm.t[:, 4, :], prm.t[:, 0, :], prm.t[:, 3, :], ALU.mult), r=[pk(0), pk(3)], w=[pk(4)])
    P.act(lambda e: e.activation(prm.t[:, 4, :], prm.t[:, 4, :], AF.Exp), r=[pk(4)], w=[pk(4)])
    P.dve(lambda e: e.tensor_tensor(prm.t[:, 5, :], prm.t[:, 1, :], prm.t[:, 3, :], ALU.mult), r=[pk(1), pk(3)], w=[pk(5)])
    QT = Q + 1
    cosT = P.sb("cosT", [128, 8, QT], F32)
    sinT = P.sb("sinT", [128, 8, QT], F32)
    io = P.sb("iota", [128, QT], F32)
    P.pool(lambda e: e.iota(io[:], [[1, QT]], base=0, channel_multiplier=0, allow_small_or_imprecise_dtypes=True), w=[io])
    xa = P.sb("xa", [128, QT], F32)
    kt_ = P.sb("kts", [128, QT], F32)
    tp_ = P.sb("tps", [128, QT], F32)
    for j in range(8):
        P.dve(lambda e, j=j: e.tensor_scalar(xa[:], io[:], prm.t[:, 5, j:j + 1], None, ALU.mult), r=[io, pk(5)], w=[xa])
        sincos(P, xa[:], sinT.t[:, j, :], cosT.t[:, j, :], kt_[:], (xa, (sinT, j), (cosT, j), kt_, tp_), tp_[:], QT)
    m1 = P.mark()
    rw = [P.sb(f"s5rw{i}", [128, 1024], F32) for i in range(10)]
    P.dma(rw[0][:], W["a_re_r"], w=[rw[0]])
    P.dma(rw[1][:], W["a_im_r"], w=[rw[1]])
    P.dma(rw[2][:], W["ls_r"], w=[rw[2]])
    P.act(lambda e: e.activation(rw[2][:], rw[2][:], AF.Exp), r=[rw[2]], w=[rw[2]])
    P.dve(lambda e: e.tensor_tensor(rw[3][:], rw[0][:], rw[2][:], ALU.mult), r=[rw[0], rw[2]], w=[rw[3]])
    P.act(lambda e: e.activation(rw[3][:], rw[3][:], AF.Exp), r=[rw[3]], w=[rw[3]])
    P.dve(lambda e: e.tensor_tensor(rw[4][:], rw[1][:], rw[2][:], ALU.mult), r=[rw[1], rw[2]], w=[rw[4]])
    sincos(P, rw[4][:], rw[5][:], rw[6][:], rw[7][:], (rw[4], rw[5], rw[6], rw[7], rw[8]), rw[8][:], 1024)
    P.dve(lambda e: e.tensor_tensor(rw[6][:], rw[6][:], rw[3][:], ALU.mult), r=[rw[6], rw[3]], w=[rw[6]])
    P.dve(lambda e: e.tensor_scalar(rw[6][:], rw[6][:], -1.0, None, ALU.add), r=[rw[6]], w=[rw[6]])
    P.dve(lambda e: e.tensor_tensor(rw[5][:], rw[5][:], rw[3][:], ALU.mult), r=[rw[5], rw[3]], w=[rw[5]])
    P.dve(lambda e: e.tensor_tensor(rw[9][:], rw[0][:], rw[0][:], ALU.mult), r=[rw[0]], w=[rw[9]])
    P.dve(lambda e: e.tensor_tensor(rw[7][:], rw[1][:], rw[1][:], ALU.mult), r=[rw[1]], w=[rw[7]])
    P.dve(lambda e: e.tensor_tensor(rw[9][:], rw[9][:], rw[7][:], ALU.add), r=[rw[9], rw[7]], w=[rw[9]])
    P.dve(lambda e: e.reciprocal(rw[9][:], rw[9][:]), r=[rw[9]], w=[rw[9]])
    P.dve(lambda e: e.tensor_tensor(rw[7][:], rw[6][:], rw[0][:], ALU.mult), r=[rw[6], rw[0]], w=[rw[7]])
    P.dve(lambda e: e.tensor_tensor(rw[8][:], rw[5][:], rw[1][:], ALU.mult), r=[rw[5], rw[1]], w=[rw[8]])
    P.dve(lambda e: e.tensor_tensor(rw[7][:], rw[7][:], rw[8][:], ALU.add), r=[rw[7], rw[8]], w=[rw[7]])
    P.dve(lambda e: e.tensor_tensor(rw[7][:], rw[7][:], rw[9][:], ALU.mult), r=[rw[7], rw[9]], w=[rw[7]])
    P.dve(lambda e: e.tensor_tensor(rw[8][:], rw[5][:], rw[0][:], ALU.mult), r=[rw[5], rw[0]], w=[rw[8]])
    P.dve(lambda e: e.tensor_tensor(rw[4][:], rw[6][:], rw[1][:], ALU.mult), r=[rw[6], rw[1]], w=[rw[4]])
    P.dve(lambda e: e.tensor_tensor(rw[8][:], rw[8][:], rw[4][:], ALU.subtract), r=[rw[8], rw[4]], w=[rw[8]])
    P.dve(lambda e: e.tensor_tensor(rw[8][:], rw[8][:], rw[9][:], ALU.mult), r=[rw[8], rw[9]], w=[rw[8]])
    P.dma(rw[0][:], W["wb_re"], w=[rw[0]])
    P.dma(rw[1][:], W["wb_im"], w=[rw[1]])
    Bb_re = P.sb("Bb_re", [128, 1024], BF16)
    Bb_im = P.sb("Bb_im", [128, 1024], BF16)
    P.dve(lambda e: e.tensor_tensor(rw[2][:], rw[7][:], rw[0][:], ALU.mult), r=[rw[7], rw[0]], w=[rw[2]])
    P.dve(lambda e: e.tensor_tensor(rw[3][:], rw[8][:], rw[1][:], ALU.mult), r=[rw[8], rw[1]], w=[rw[3]])
    P.dve(lambda e: e.tensor_tensor(Bb_re[:], rw[2][:], rw[3][:], ALU.subtract), r=[rw[2], rw[3]], w=[Bb_re])
    P.dve(lambda e: e.tensor_tensor(rw[2][:], rw[7][:], rw[1][:], ALU.mult), r=[rw[7], rw[1]], w=[rw[2]])
    P.dve(lambda e: e.tensor_tensor(rw[3][:], rw[8][:], rw[0][:], ALU.mult), r=[rw[8], rw[0]], w=[rw[3]])
    P.dve(lambda e: e.tensor_tensor(Bb_im[:], rw[2][:], rw[3][:], ALU.add), r=[rw[2], rw[3]], w=[Bb_im])
    Wc_re = P.sb("Wc_re", [128, 1024], F32)
    Wc_im = P.sb("Wc_im", [128, 1024], F32)
    P.dma(rw[4][:], W["wc_re"], w=[rw[4]])
    P.dma(rw[5][:], W["wc_im"], w=[rw[5]])
    P.act(lambda e: e.copy(Wc_re[:], rw[4][:]), r=[rw[4]], w=[Wc_re])
    P.act(lambda e: e.mul(Wc_im[:], rw[5][:], -1.0), r=[rw[5]], w=[Wc_im])
    wglu = P.sb("wglu", [128, 2, 256], BF16)
    P.dma(wglu[:], W["wglu"].rearrange("(k p) n -> p k n", p=128), w=[wglu], eng="pool")
    cols = P.sb("s5cols", [128, 4], F32)
    P.dma(cols[:, 0:2], W["dcol"], w=[(cols, 0)])
    P.dma(cols[:, 2:4], W["ncol"], w=[(cols, 1)])
    P.barrier()
    keep = P.mark()
    u_bf = P.sb("u_bf", [128, 2, SEQ], BF16)
    uv = proj_d[0:256, :].rearrange("(k p) t -> p k t", p=128)
    for k in range(2):
        for c in range(SEQ // 2048 if SEQ >= 2048 else 1):
            w_ = min(2048, SEQ)
            P.dma(u_bf.t[:, k, c * w_:(c + 1) * w_], uv[:, k, c * w_:(c + 1) * w_], w=[(u_bf, k, c)], eng="pool")
    ini = P.sb("ini", [128, 8, 2], F32)
    P.dve(lambda e: e.memset(ini[:], 0.0), w=[ini])
    ini2 = P.sb("ini2", [128, 8, 4], F32)
    t = [P.sb(f"s5t{i}", [128, Q], F32) for i in range(8)]
    Wr = [P.sb(f"Wr{i}", [128, Q], F32) for i in range(2)]
    Wi = [P.sb(f"Wi{i}", [128, Q], F32) for i in range(2)]
    S_re = P.sb("S_re", [128, 8, Q], F32)
    S_im = P.sb("S_im", [128, 8, Q], F32)
    u32 = P.sb("u32", [128, 2, Q], F32)
    pt = [P.sb(f"s5p{i}", [128, Q], F32) for i in range(4)]
    gl = P.sb("s5gl", [128, 2, Q], BF16)
    y2 = P.sb("s5y2", [128, 2, Q], F32)
    sq = P.sb("s5sq", [128, 2, Q], F32)
    ob = P.sb("s5ob", [128, 2, Q], BF16)
    for c in range(NCH):
        c0 = c * Q
        ukeys = [(u_bf, k, c0 // 2048) for k in range(2)]
        P.dma(u32[:], uv[:, :, c0:c0 + Q], w=[u32])
        for j in range(8):
            b = j % 2
            ut = j // 4
            pa, pb_ = ps[0 + b], ps[2 + b]
            P.pe(lambda e, j=j, pa=pa, ut=ut, c0=c0: e.matmul(pa[:], Bb_re[:, j * 128:(j + 1) * 128], u_bf[:, ut, c0:c0 + Q], start=True, stop=True), r=[Bb_re] + ukeys, w=[pa])
            P.pe(lambda e, j=j, pb_=pb_, ut=ut, c0=c0: e.matmul(pb_[:], Bb_im[:, j * 128:(j + 1) * 128], u_bf[:, ut, c0:c0 + Q], start=True, stop=True), r=[Bb_im] + ukeys, w=[pb_])
            cs, sn = cosT.t[:, j, 0:Q], sinT.t[:, j, 0:Q]
            T0, T1, T2, T3 = t[4 * b:4 * b + 4]
            P.dve(lambda e, T0=T0, pa=pa, cs=cs: e.tensor_tensor(T0[:], pa[:], cs, ALU.mult), r=[pa, (cosT, j)], w=[T0])
            P.dve(lambda e, T1=T1, pb_=pb_, sn=sn: e.tensor_tensor(T1[:], pb_[:], sn, ALU.mult), r=[pb_, (sinT, j)], w=[T1])
            P.dve(lambda e, T2=T2, pb_=pb_, cs=cs: e.tensor_tensor(T2[:], pb_[:], cs, ALU.mult), r=[pb_, (cosT, j)], w=[T2])
            P.dve(lambda e, T3=T3, pa=pa, sn=sn: e.tensor_tensor(T3[:], pa[:], sn, ALU.mult), r=[pa, (sinT, j)], w=[T3])
            P.pool(lambda e, T0=T0, T1=T1: e.tensor_tensor(T0[:], T0[:], T1[:], ALU.add), r=[T0, T1], w=[T0])
            P.pool(lambda e, T2=T2, T3=T3: e.tensor_tensor(T2[:], T2[:], T3[:], ALU.subtract), r=[T2, T3], w=[T2])
            rmag = prm.t[:, 4, j:j + 1].broadcast_to([128, Q])
            P.dve(lambda e, b=b, T0=T0, rmag=rmag, j=j: e.tensor_tensor_scan(Wr[b][:], rmag, T0[:], ini.t[:, j, 0:1], ALU.mult, ALU.add), r=[T0, pk(4), ini], w=[Wr[b]])
            P.dve(lambda e, b=b, T2=T2, rmag=rmag, j=j: e.tensor_tensor_scan(Wi[b][:], rmag, T2[:], ini.t[:, j, 1:2], ALU.mult, ALU.add), r=[T2, pk(4), ini], w=[Wi[b]])
            P.pool(lambda e, T0=T0, b=b, cs=cs: e.tensor_tensor(T0[:], Wr[b][:], cs, ALU.mult), r=[Wr[b], (cosT, j)], w=[T0])
            P.pool(lambda e, T1=T1, b=b, sn=sn: e.tensor_tensor(T1[:], Wi[b][:], sn, ALU.mult), r=[Wi[b], (sinT, j)], w=[T1])
            P.pool(lambda e, T0=T0, T1=T1, j=j: e.tensor_tensor(S_re.t[:, j, :], T0[:], T1[:], ALU.subtract), r=[T0, T1], w=[(S_re, j)])
            P.pool(lambda e, T2=T2, b=b, sn=sn: e.tensor_tensor(T2[:], Wr[b][:], sn, ALU.mult), r=[Wr[b], (sinT, j)], w=[T2])
            P.pool(lambda e, T3=T3, b=b, cs=cs: e.tensor_tensor(T3[:], Wi[b][:], cs, ALU.mult), r=[Wi[b], (cosT, j)], w=[T3])
            P.pool(lambda e, T2=T2, T3=T3, j=j: e.tensor_tensor(S_im.t[:, j, :], T2[:], T3[:], ALU.add), r=[T2, T3], w=[(S_im, j)])
            cq, sq_ = cosT.t[:, j, Q:Q + 1], sinT.t[:, j, Q:Q + 1]
            P.dve(lambda e, b=b, j=j, cq=cq: e.tensor_tensor(ini2.t[:, j, 0:1], Wr[b][:, Q - 1:Q], cq, ALU.mult), r=[Wr[b], (cosT, j)], w=[ini2])
            P.dve(lambda e, b=b, j=j, sq_=sq_: e.tensor_tensor(ini2.t[:, j, 1:2], Wi[b][:, Q - 1:Q], sq_, ALU.mult), r=[Wi[b], (sinT, j)], w=[ini2])
            P.dve(lambda e, b=b, j=j, sq_=sq_: e.tensor_tensor(ini2.t[:, j, 2:3], Wr[b][:, Q - 1:Q], sq_, ALU.mult), r=[Wr[b], (sinT, j)], w=[ini2])
            P.dve(lambda e, b=b, j=j, cq=cq: e.tensor_tensor(ini2.t[:, j, 3:4], Wi[b][:, Q - 1:Q], cq, ALU.mult), r=[Wi[b], (cosT, j)], w=[ini2])
            P.dve(lambda e, j=j: e.tensor_tensor(ini.t[:, j, 0:1], ini2.t[:, j, 0:1], ini2.t[:, j, 1:2], ALU.subtract), r=[ini2], w=[ini])
            P.dve(lambda e, j=j: e.tensor_tensor(ini.t[:, j, 1:2], ini2.t[:, j, 2:3], ini2.t[:, j, 3:4], ALU.add), r=[ini2], w=[ini])
        for ut in range(2):
            py = ps[4 + ut]
            n = 0
            for j in range(4 * ut, 4 * ut + 4):
                P.pe(lambda e, j=j, py=py, n=n: e.matmul(py[:], Wc_re[:, j * 128:(j + 1) * 128], S_re[:, j, :], start=(n == 0), stop=False), r=[Wc_re, (S_re, j)], w=[py])
                n += 1
                P.pe(lambda e, j=j, py=py, n=n: e.matmul(py[:], Wc_im[:, j * 128:(j + 1) * 128], S_im[:, j, :], start=False, stop=(n == 7)), r=[Wc_im, (S_im, j)], w=[py])
                n += 1
            Y, A1, A2, A3 = pt
            P.dve(lambda e, ut=ut, py=py: e.scalar_tensor_tensor(Y[:], u32[:, ut, :], cols[:, ut:ut + 1], py[:], ALU.mult, ALU.add), r=[u32, (cols, 0), py], w=[Y])
            P.pool(lambda e: e.tensor_tensor(A1[:], Y[:], Y[:], ALU.mult), r=[Y], w=[A1])
            P.pool(lambda e: e.tensor_scalar(A1[:], A1[:], 0.044715, 1.0, ALU.mult, ALU.add), r=[A1], w=[A1])
            P.pool(lambda e: e.tensor_tensor(A1[:], A1[:], Y[:], ALU.mult), r=[A1, Y], w=[A1])
            P.act(lambda e: e.activation(A2[:], A1[:], AF.Sigmoid, scale=1.5957691216057308), r=[A1], w=[A2])
            P.dve(lambda e, ut=ut: e.tensor_tensor(y2.t[:, ut, :], Y[:], A2[:], ALU.mult), r=[Y, A2], w=[(y2, ut)])
            P.act(lambda e, ut=ut: e.copy(gl.t[:, ut, :], y2.t[:, ut, :]), r=[(y2, ut)], w=[(gl, ut)])
        for mo in range(2):
            pz = ps[6 + mo]
            for k in range(2):
                P.pe(lambda e, k=k, mo=mo, pz=pz: e.matmul(pz[:], wglu[:, k, mo * 128:(mo + 1) * 128], gl[:, k, :], start=(k == 0), stop=(k == 1)), r=[wglu, (gl, 0), (gl, 1)], w=[pz])
            A1 = pt[1]
            P.act(lambda e, pz=pz: e.activation(A1[:], pz[:], AF.Sigmoid), r=[pz], w=[A1])
            P.dve(lambda e, mo=mo: e.tensor_tensor(y2.t[:, mo, :], y2.t[:, mo, :], A1[:], ALU.mult), r=[(y2, mo), A1], w=[(y2, mo)])
            P.pool(lambda e, mo=mo: e.tensor_tensor(sq.t[:, mo, :], y2.t[:, mo, :], y2.t[:, mo, :], ALU.mult), r=[(y2, mo)], w=[(sq, mo)])
        pss = ps[4]
        for k in range(2):
            P.pe(lambda e, k=k: e.matmul(pss[:], C.ones[:], sq[:, k, :], start=(k == 0), stop=(k == 1)), r=[C.ones, (sq, k)], w=[pss])
        R1, R2 = pt[2], pt[3]
        P.dve(lambda e: e.tensor_scalar(R1[:], pss[:], 1.0 / 256, EPS, ALU.mult, ALU.add), r=[pss], w=[R1])
        P.act(lambda e: e.activation(R1[:], R1[:], AF.Sqrt), r=[R1], w=[R1])
        P.dve(lambda e: e.reciprocal(R2[:], R1[:]), r=[R1], w=[R2])
        for mo in range(2):
            P.dve(lambda e, mo=mo: e.scalar_tensor_tensor(ob.t[:, mo, :], y2.t[:, mo, :], cols[:, 2 + mo:3 + mo], R2[:], ALU.mult, ALU.mult), r=[(y2, mo), (cols, 1), R2], w=[(ob, mo)])
            P.dma(mix_d[mo * 128:(mo + 1) * 128, c0:c0 + Q], ob.t[:, mo, :], r=[(ob, mo)], w=[("mix", mo, c)])
    P.barrier()
    P.release(m0)


def rep128(v):
    v = np.asarray(v, np.float32).reshape(1, -1)
    return np.ascontiguousarray(np.broadcast_to(v, (128, v.shape[1])))


def host_layer_inputs(inp, l):
    o = {}
    f = lambda a: np.ascontiguousarray(np.asarray(a, np.float32))
    win = f(inp["w_in"][l])
    wp = np.zeros((D, NPROJ), np.float32)
    wp[:, 0:1536] = win[:, 0:1536]
    wp[:, 1536:2688] = win[:, 1542:2694]
    wp[:, 2688:3072] = win[:, 2694:3078]
    wp[:, 3072:3078] = win[:, 1536:1542]
    wp[:, 3078:3084] = win[:, 3078:3084]
    wp[:, 3084:3090] = win[:, 3084:3090]
    o["win"] = wp
    o["wout"] = f(inp["w_out"][l])
    o["nmix"] = rep128(inp["norm_mix"][l])
    o["nffn"] = rep128(inp["norm_ffn"][l])
    def slay(a):
        return np.ascontiguousarray(f(a).reshape(8, 2, 64).transpose(1, 2, 0).reshape(128, 8))
    o["a_re_s"] = slay(inp["s5_a_re"][l])
    o["a_im_s"] = slay(inp["s5_a_im"][l])
    o["ls_s"] = slay(np.broadcast_to(f(inp["s5_log_step"][l])[:, None], (16, 64)))
    o["a_re_r"] = rep128(f(inp["s5_a_re"][l]).reshape(-1))
    o["a_im_r"] = rep128(f(inp["s5_a_im"][l]).reshape(-1))
    o["ls_r"] = rep128(np.broadcast_to(f(inp["s5_log_step"][l])[:, None], (16, 64)).reshape(-1))
    for nm, src in (("wb_re", inp["s5_b_re"][l]), ("wb_im", inp["s5_b_im"][l])):
        B = f(src)
        wb = np.zeros((128, 8, 128), np.float32)
        for g in range(16):
            j, two = divmod(g, 2)
            r0 = (g % 8) * 16
            wb[r0:r0 + 16, j, two * 64:(two + 1) * 64] = B[g].T
        o[nm] = wb.reshape(128, 1024)
    for nm, src in (("wc_re", inp["s5_c_re"][l]), ("wc_im", inp["s5_c_im"][l])):
        Cm = f(src)
        wc = np.zeros((128, 8, 128), np.float32)
        for g in range(16):
            j, two = divmod(g, 2)
            c0 = (g % 8) * 16
            wc[two * 64:(two + 1) * 64, j, c0:c0 + 16] = Cm[g].T
        o[nm] = wc.reshape(128, 1024)
    o["dcol"] = np.ascontiguousarray(f(inp["s5_d"][l]).reshape(2, 128).T)
    o["ncol"] = np.ascontiguousarray(f(inp["s5_norm"][l]).reshape(2, 128).T)
    o["wglu"] = f(inp["s5_w_glu"][l])
    o["ssd_cw"] = np.ascontiguousarray(f(inp["ssd_conv_w"][l]).reshape(4, 7, 128).transpose(2, 1, 0))
    o["ssd_cb"] = np.ascontiguousarray(f(inp["ssd_conv_b"][l]).reshape(7, 128).T)
    o["ssd_dtb"] = rep128(inp["ssd_dt_bias"][l])
    o["ssd_alog"] = rep128(inp["ssd_a_log"][l])
    o["ssd_drep"] = rep128(np.repeat(f(inp["ssd_d"][l]), 64))
    o["ssd_nw"] = rep128(inp["ssd_norm"][l])
    o["gdn_cw"] = np.ascontiguousarray(f(inp["gdn_conv_w"][l]).reshape(4, 9, 128).transpose(2, 1, 0))
    o["gdn_alog"] = rep128(inp["gdn_a_log"][l])
    o["gdn_dtb"] = rep128(inp["gdn_dt_bias"][l])
    o["gdn_nw"] = rep128(np.tile(f(inp["gdn_norm"][l]), 6))
    return o


def mm(P, out, lhsT, rhs, r, w, start=True, stop=True):
    P.pe(lambda e: e.matmul(out, lhsT, rhs, start=start, stop=stop), r=r, w=w)


def tr(P, C, out, in_, n, r, w):
    P.pe(lambda e: e.transpose(out, in_, C.ID[0:n, 0:n]), r=list(r) + [C.ID], w=w)


def tt(P, eng, out, a, b, op, r, w):
    P.op(eng, lambda e: e.tensor_tensor(out, a, b, op), r, w)


def ts(P, eng, out, a, s1, s2, op0, op1, r, w):
    if op1 is None:
        P.op(eng, lambda e: e.tensor_scalar(out, a, s1, None, op0), r, w)
    else:
        P.op(eng, lambda e: e.tensor_scalar(out, a, s1, s2, op0, op1), r, w)


def stt(P, out, a, s, b, op0, op1, r, w):
    P.dve(lambda e: e.scalar_tensor_tensor(out, a, s, b, op0, op1), r, w)


def actf(P, out, in_, func, r, w, **kw):
    P.act(lambda e: e.activation(out, in_, func, **kw), r, w)


def conv_silu(P, C, SEQ, proj_d, row0, ntile, cw, cb, dst, xin, acc):
    for i in range(ntile):
        P.dve(lambda e: e.memset(xin[:, 0:3], 0.0), w=[xin])
        P.dma(xin[:, 3:3 + SEQ], proj_d[row0 + i * 128:row0 + (i + 1) * 128, :], w=[xin])
        ts(P, "dve", acc[:], xin[:, 0:SEQ], cw[:, i, 0:1], None, ALU.mult, None, [xin, cw], [acc])
        for k in range(1, 4):
            stt(P, acc[:], xin[:, k:k + SEQ], cw[:, i, k:k + 1], acc[:], ALU.mult, ALU.add, [xin, cw, acc], [acc])
        if cb is not None:
            actf(P, dst[i][:], acc[:], AF.Silu, [acc, cb], [dst[i]], bias=cb[:, i:i + 1])
        else:
            actf(P, dst[i][:], acc[:], AF.Silu, [acc], [dst[i]])


def conv_blk(P, SEQ, proj_d, row0, ntile, cw, cb, dst, xin, acc, t0):
    for i in range(ntile):
        xi = xin[i % 2]
        if t0 == 0:
            P.dve(lambda e, xi=xi: e.memset(xi[:, 0:3], 0.0), w=[xi])
            P.dma(xi[:, 3:131], proj_d[row0 + i * 128:row0 + (i + 1) * 128, 0:128], w=[xi])
        else:
            P.dma(xi[:, 0:131], proj_d[row0 + i * 128:row0 + (i + 1) * 128, t0 - 3:t0 + 128], w=[xi])
        ts(P, "dve", acc[:], xi[:, 0:128], cw[:, i, 0:1], None, ALU.mult, None, [xi, cw], [acc])
        for k in range(1, 4):
            stt(P, acc[:], xi[:, k:k + 128], cw[:, i, k:k + 1], acc[:], ALU.mult, ALU.add, [xi, cw, acc], [acc])
        if cb is not None:
            actf(P, dst[i][:], acc[:], AF.Silu, [acc, cb], [dst[i]], bias=cb[:, i:i + 1])
        else:
            actf(P, dst[i][:], acc[:], AF.Silu, [acc], [dst[i]])


def make_masks(P, C):
    def tri(name, cmp_base, cm, step):
        t = P.sb(name, [128, 128], F32)
        P.pool(lambda e: e.memset(t[:], 1.0), w=[t])
        P.pool(lambda e: e.affine_select(t[:], t[:], [[step, 128]], ALU.is_ge, 0.0, base=cmp_base, channel_multiplier=cm), r=[t], w=[t])
        return t
    C.U = tri("U", 0, -1, 1)
    C.Ls = tri("Ls", -1, 1, -1)
    C.L = tri("L", 0, 1, -1)
    C.BD = P.sb("BD", [128, 128], F32)
    P.pool(lambda e: e.memset(C.BD[:], 0.0), w=[C.BD])
    P.pool(lambda e: e.memset(C.BD[0:64, 0:64], 1.0), r=[C.BD], w=[C.BD])
    P.pool(lambda e: e.memset(C.BD[64:128, 64:128], 1.0), r=[C.BD], w=[C.BD])
    C.SEL0 = P.sb("SEL0", [128, 128], F32)
    C.SEL1 = P.sb("SEL1", [128, 128], F32)
    P.pool(lambda e: e.memset(C.SEL0[:], 0.0), w=[C.SEL0])
    P.pool(lambda e: e.memset(C.SEL0[0:64, :], 1.0), r=[C.SEL0], w=[C.SEL0])
    P.pool(lambda e: e.memset(C.SEL1[:], 0.0), w=[C.SEL1])
    P.pool(lambda e: e.memset(C.SEL1[64:128, :], 1.0), r=[C.SEL1], w=[C.SEL1])
    def neg(name, m01, extra=None):
        t = P.sb(name, [128, 128], F32)
        if extra is not None:
            tt(P, "pool", t[:], m01[:], extra[:], ALU.mult, [m01, extra], [t])
            ts(P, "pool", t[:], t[:], 1e30, -1e30, ALU.mult, ALU.add, [t], [t])
        else:
            ts(P, "pool", t[:], m01[:], 1e30, -1e30, ALU.mult, ALU.add, [m01], [t])
        return t
    C.negU = neg("negU", C.U)
    C.U2 = P.sb("U2", [128, 128], F32)
    tt(P, "pool", C.U2[:], C.U[:], C.BD[:], ALU.mult, [C.U, C.BD], [C.U2])
    C.negU2 = neg("negU2", C.U2)
    C.negLs2 = neg("negLs2", C.Ls, C.BD)


def mixer_ssd(P, C, SEQ, proj_d, mix_d, W):
    m0 = P.mark()
    ps = C.ps
    NB = SEQ // 128
    cw = P.sb("ssd_cw", [128, 7, 4], F32)
    cb = P.sb("ssd_cb", [128, 7], F32)
    P.dma(cw[:], W["ssd_cw"], w=[cw])
    P.dma(cb[:], W["ssd_cb"], w=[cb])
    bc = P.sb("ssd_bc", [128, 4, 384], F32)
    P.dma(bc.t[:, 0, 0:6], W["ssd_dtb"], w=[(bc, 0)])
    P.dma(bc.t[:, 1, 0:6], W["ssd_alog"], w=[(bc, 1)])
    P.dma(bc.t[:, 2, :], W["ssd_drep"], w=[(bc, 2)])
    P.dma(bc.t[:, 3, :], W["ssd_nw"], w=[(bc, 3)])
    actf(P, bc.t[:, 1, 0:6], bc.t[:, 1, 0:6], AF.Exp, [(bc, 1)], [(bc, 1)])
    ts(P, "dve", bc.t[:, 1, 0:6], bc.t[:, 1, 0:6], -1.0, None, ALU.mult, None, [(bc, 1)], [(bc, 1)])
    xin = [P.sb(f"cv_in{i}", [128, 131], F32) for i in range(2)]
    acc = P.sb("cv_acc", [128, 128], F32)
    F = [P.sb(f"ssdF{i}", [128, 128], F32) for i in range(7)]
    zT = [P.sb(f"ssdz{i}", [128, 128], F32) for i in range(3)]
    sm = P.sb("ssd_sm", [6, SEQ], F32)
    P.dma(sm[:], proj_d[3072:3078, :], w=[sm])
    S = P.sb("ssdS", [128, 6, 64], F32)
    P.dve(lambda e: e.memset(S[:], 0.0), w=[S])
    v = P.sb("ssdv", [128, 64], F32)
    E = [P.sb(f"ssdE{i}", [128, 128], F32) for i in range(2)]
    Mt = [P.sb(f"ssdM{i}", [128, 128], F32) for i in range(2)]
    Xtm = P.sb("ssdXtm", [128, 384], F32)
    xdt = P.sb("ssdxdt", [128, 384], F32)
    xdd = P.sb("ssdxdd", [128, 384], F32)
    Btm = P.sb("ssdBtm", [128, 256], F32)
    yd = P.sb("ssdyd", [128, 384], F32)
    y = P.sb("ssdy", [128, 384], F32)
    zs = P.sb("ssdzs", [128, 384], F32)
    junk = P.sb("ssdjunk", [128, 192], F32)
    ssq = P.sb("ssdss", [128, 8], F32)
    ob = P.sb("ssdob", [128, 3, 128], BF16)
    for blk in range(NB):
        t0 = blk * 128
        cs = slice(t0, t0 + 128)
        conv_blk(P, SEQ, proj_d, 640, 7, cw, cb, F, xin, acc, t0)
        for i in range(3):
            P.dma(zT[i][:], proj_d[256 + i * 128:256 + (i + 1) * 128, cs], w=[zT[i]])
        tr(P, C, ps[0][:, 0:6], sm[0:6, cs], 6, [sm], [ps[0]])
        tt(P, "dve", v[:, 0:6], ps[0][:, 0:6], bc.t[:, 0, 0:6], ALU.add, [ps[0], (bc, 0)], [v])
        actf(P, v[:, 0:6], v[:, 0:6], AF.Exp, [v], [v])
        actf(P, v[:, 0:6], v[:, 0:6], AF.Ln, [v], [v], bias=C.ones[:, 0:1])
        tt(P, "dve", v[:, 6:12], v[:, 0:6], bc.t[:, 1, 0:6], ALU.mult, [v, (bc, 1)], [v])
        mm(P, ps[0][:, 8:14], C.U[:], v[:, 6:12], [C.U, v], [ps[0]])
        mm(P, ps[0][:, 16:22], C.ones[:], v[:, 6:12], [C.ones, v], [ps[0]])
        P.dve(lambda e: e.tensor_copy(v[:, 12:18], ps[0][:, 8:14]), r=[ps[0]], w=[v])
        ts(P, "dve", v[:, 18:24], v[:, 12:18], -1.0, None, ALU.mult, None, [v], [v])
        P.dve(lambda e: e.tensor_copy(v[:, 24:30], ps[0][:, 16:22]), r=[ps[0]], w=[v])
        tt(P, "dve", v[:, 30:36], v[:, 24:30], v[:, 12:18], ALU.subtract, [v], [v])
        actf(P, v[:, 30:36], v[:, 30:36], AF.Exp, [v], [v])
        tt(P, "dve", v[:, 30:36], v[:, 30:36], v[:, 0:6], ALU.mult, [v], [v])
        actf(P, v[:, 36:42], v[:, 12:18], AF.Exp, [v], [v])
        actf(P, v[:, 42:48], v[:, 24:30], AF.Exp, [v], [v])
        for i in range(3):
            tr(P, C, ps[1][:, i * 128:(i + 1) * 128], F[i][:], 128, [F[i]], [ps[1]])
        P.act(lambda e: e.copy(Xtm[:], ps[1][:, 0:384]), r=[ps[1]], w=[Xtm])
        for h in range(6):
            hs = slice(h * 64, (h + 1) * 64)
            actf(P, xdt[:, hs], Xtm[:, hs], AF.Copy, [Xtm, v], [(xdt, h)], scale=v[:, h:h + 1])
            actf(P, xdd[:, hs], Xtm[:, hs], AF.Copy, [Xtm, v], [(xdd, h)], scale=v[:, 30 + h:31 + h])
        for g in range(2):
            tr(P, C, ps[2][:, g * 128:(g + 1) * 128], F[3 + g][:], 128, [F[3 + g]], [ps[2]])
        P.dve(lambda e: e.tensor_copy(Btm[:], ps[2][:, 0:256]), r=[ps[2]], w=[Btm])
        for g in range(2):
            mm(P, ps[3][:, g * 128:(g + 1) * 128], F[3 + g][:], F[5 + g][:], [F[3 + g], F[5 + g]], [ps[3]])
        for h in range(6):
            g = h // 3
            b = h % 2
            pr_ = ps[4 + b]
            mm(P, pr_[:, 0:128], v[:, 6 + h:7 + h].broadcast_to([128, 128]), C.U[:], [v, C.U], [pr_])
            stt(P, E[b][:], pr_[:, 0:128], v[:, 18 + h:19 + h], C.negU[:], ALU.add, ALU.add, [pr_, v, C.negU], [E[b]])
            actf(P, E[b][:], E[b][:], AF.Exp, [E[b]], [E[b]])
            tt(P, "dve", Mt[b][:], ps[3][:, g * 128:(g + 1) * 128], E[b][:], ALU.mult, [ps[3], E[b]], [Mt[b]])
            hs = slice(h * 64, (h + 1) * 64)
            mm(P, ps[6][:, hs], Mt[b][:], xdt[:, hs], [Mt[b], (xdt, h)], [ps[6]])
            mm(P, ps[7][:, hs], F[5 + g][:], S[:, h, :], [F[5 + g], (S, h)], [ps[7]])
        P.act(lambda e: e.copy(yd[:], ps[6][:, 0:384]), r=[ps[6]], w=[yd])
        for h in range(6):
            hs = slice(h * 64, (h + 1) * 64)
            stt(P, y[:, hs], ps[7][:, hs], v[:, 36 + h:37 + h], yd[:, hs], ALU.mult, ALU.add, [ps[7], v, yd], [(y, h)])
        for g in range(2):
            mm(P, ps[2][:, 256 + 0:256 + 192] if False else ps[0][:, 64 + g * 192:64 + (g + 1) * 192], Btm[:, g * 128:(g + 1) * 128], xdd[:, g * 192:(g + 1) * 192],
               [Btm] + [(xdd, h) for h in range(3 * g, 3 * g + 3)], [ps[0]])
        for h in range(6):
            stt(P, S[:, h, :], S[:, h, :], v[:, 42 + h:43 + h], ps[0][:, 64 + h * 64:64 + (h + 1) * 64], ALU.mult, ALU.add, [(S, h), v, ps[0]], [(S, h)])
        yk = [(y, h) for h in range(6)]
        tt(P, "pool", xdt[:], Xtm[:], bc.t[:, 2, :], ALU.mult, [Xtm, (bc, 2)] + [(xdt, h) for h in range(6)], [(xdt, h) for h in range(6)])
        tt(P, "pool", y[:], y[:], xdt[:], ALU.add, yk + [(xdt, h) for h in range(6)], yk)
        for i in range(3):
            tr(P, C, ps[1][:, i * 128:(i + 1) * 128], zT[i][:], 128, [zT[i]], [ps[1]])
        actf(P, zs[:], ps[1][:, 0:384], AF.Silu, [ps[1]], [zs])
        tt(P, "dve", y[:], y[:], zs[:], ALU.mult, yk + [zs], yk)
        for g in range(2):
            gs = slice(g * 192, (g + 1) * 192)
            actf(P, junk[:], y[:, gs], AF.Square, yk, [junk, (ssq, g)], accum_out=ssq[:, g:g + 1])
            ts(P, "dve", ssq[:, 2 + g:3 + g], ssq[:, g:g + 1], 1.0 / 192, EPS, ALU.mult, ALU.add, [(ssq, g)], [(ssq, g)])
            actf(P, ssq[:, 4 + g:5 + g], ssq[:, 2 + g:3 + g], AF.Sqrt, [(ssq, g)], [(ssq, g)])
            P.dve(lambda e, g=g: e.reciprocal(ssq[:, 6 + g:7 + g], ssq[:, 4 + g:5 + g]), r=[(ssq, g)], w=[(ssq, g)])
            stt(P, y[:, gs], y[:, gs], ssq[:, 6 + g:7 + g], bc.t[:, 3, gs], ALU.mult, ALU.mult, yk + [(ssq, g), (bc, 3)], yk)
        for i in range(3):
            tr(P, C, ps[2][:, i * 128:(i + 1) * 128], y[:, i * 128:(i + 1) * 128], 128, yk, [ps[2]])
        P.act(lambda e: e.copy(ob[:], ps[2].t[:, 0:384].rearrange("p (k t) -> p k t", k=3)), r=[ps[2]], w=[ob])
        P.dma(mix_d[256:640, cs].rearrange("(k p) t -> p k t", p=128), ob[:], r=[ob], w=[("mixssd", blk)])
    P.barrier()
    P.release(m0)


def mixer_gdn(P, C, SEQ, proj_d, mix_d, W):
    m0 = P.mark()
    ps = C.ps
    NB = SEQ // 128
    cw = P.sb("gdn_cw", [128, 9, 4], F32)
    P.dma(cw[:], W["gdn_cw"], w=[cw])
    bc = P.sb("gdn_bc", [128, 3, 384], F32)
    P.dma(bc.t[:, 0, 0:6], W["gdn_alog"], w=[(bc, 0)])
    P.dma(bc.t[:, 1, 0:6], W["gdn_dtb"], w=[(bc, 1)])
    P.dma(bc.t[:, 2, :], W["gdn_nw"], w=[(bc, 2)])
    actf(P, bc.t[:, 0, 0:6], bc.t[:, 0, 0:6], AF.Exp, [(bc, 0)], [(bc, 0)])
    ts(P, "dve", bc.t[:, 0, 0:6], bc.t[:, 0, 0:6], -1.0, None, ALU.mult, None, [(bc, 0)], [(bc, 0)])
    xin = [P.sb(f"gcv_in{i}", [128, 131], F32) for i in range(2)]
    acc = P.sb("gcv_acc", [128, 128], F32)
    F = [P.sb(f"gdnF{i}", [128, 128], F32) for i in range(9)]
    zT = [P.sb(f"gdnz{i}", [128, 128], F32) for i in range(3)]
    sm = P.sb("gdn_sm", [12, SEQ], F32)
    P.dma(sm[:], proj_d[3078:3090, :], w=[sm])
    S = [P.sb(f"gdnS{h}", [64, 64], F32) for h in range(6)]
    for h in range(6):
        P.dve(lambda e, h=h: e.memset(S[h][:], 0.0), w=[S[h]])
    v = P.sb("gdnv", [128, 96], F32)
    big = {n: P.sb("gdn_" + n, [128, 384], F32) for n in ("q", "k", "v", "sq", "qn", "kn", "qd", "kbg", "kend", "vb", "o", "zs")}
    v3 = lambda t: t.t[:, :].rearrange("p (h d) -> p h d", h=6)
    hb = lambda c0: v[:, c0:c0 + 6].unsqueeze(2).broadcast_to([128, 6, 64])
    knT = [P.sb(f"knT{h}", [64, 128], F32) for h in range(6)]
    qnT = [P.sb(f"qnT{h}", [64, 128], F32) for h in range(6)]
    qdT = [P.sb(f"qdT{h}", [64, 128], F32) for h in range(6)]
    E = P.sb("gE", [128, 128], F32)
    DL = P.sb("gDL", [128, 128], F32)
    qkT = P.sb("gqkT", [128, 128], F32)
    Pk = [P.sb(f"gP{i}", [128, 128], F32) for i in range(2)]
    PkT = [P.sb(f"gPT{i}", [128, 128], F32) for i in range(2)]
    YT = P.sb("gYT", [128, 128], F32)
    WT = P.sb("gWT", [64, 128], F32)
    Us = P.sb("gU", [128, 64], F32)
    vn = P.sb("gvn", [128, 64], F32)
    ob = P.sb("gob", [128, 3, 128], BF16)
    allv = [v]
    for blk in range(NB):
        t0 = blk * 128
        cs = slice(t0, t0 + 128)
        conv_blk(P, SEQ, proj_d, 1536, 9, cw, None, F, xin, acc, t0)
        for i in range(3):
            P.dma(zT[i][:], proj_d[2688 + i * 128:2688 + (i + 1) * 128, cs], w=[zT[i]])
        tr(P, C, ps[0][:, 0:12], sm[0:12, cs], 12, [sm], [ps[0]])
        actf(P, v[:, 0:6], ps[0][:, 0:6], AF.Sigmoid, [ps[0]], [v])
        tt(P, "dve", v[:, 6:12], ps[0][:, 6:12], bc.t[:, 1, 0:6], ALU.add, [ps[0], (bc, 1)], [v])
        actf(P, v[:, 6:12], v[:, 6:12], AF.Exp, [v], [v])
        actf(P, v[:, 6:12], v[:, 6:12], AF.Ln, [v], [v], bias=C.ones[:, 0:1])
        tt(P, "dve", v[:, 6:12], v[:, 6:12], bc.t[:, 0, 0:6], ALU.mult, [v, (bc, 0)], [v])
        mm(P, ps[0][:, 16:22], C.U2[:], v[:, 6:12], [C.U2, v], [ps[0]])
        mm(P, ps[0][:, 24:30], C.BD[:], v[:, 6:12], [C.BD, v], [ps[0]])
        mm(P, ps[0][:, 32:38], C.SEL0[:], v[:, 6:12], [C.SEL0, v], [ps[0]])
        mm(P, ps[0][:, 40:46], C.SEL1[:], v[:, 6:12], [C.SEL1, v], [ps[0]])
        P.dve(lambda e: e.tensor_copy(v[:, 12:18], ps[0][:, 16:22]), r=[ps[0]], w=[v])
        ts(P, "dve", v[:, 18:24], v[:, 12:18], -1.0, None, ALU.mult, None, [v], [v])
        tt(P, "dve", v[:, 30:36], ps[0][:, 24:30], v[:, 12:18], ALU.subtract, [ps[0], v], [v])
        actf(P, v[:, 30:36], v[:, 30:36], AF.Exp, [v], [v])
        actf(P, v[:, 36:42], v[:, 12:18], AF.Exp, [v], [v])
        actf(P, v[:, 42:48], ps[0][:, 32:38], AF.Exp, [ps[0]], [v])
        actf(P, v[:, 48:54], ps[0][:, 40:46], AF.Exp, [ps[0]], [v])
        ts(P, "dve", v[:, 78:84], v[:, 0:6], -1.0, None, ALU.mult, None, [v], [v])
        for n_, base, bank in (("q", 0, 1), ("k", 3, 2), ("v", 6, 3)):
            for i in range(3):
                tr(P, C, ps[bank][:, i * 128:(i + 1) * 128], F[base + i][:], 128, [F[base + i]], [ps[bank]])
            P.act(lambda e, n_=n_, bank=bank: e.copy(big[n_][:], ps[bank][:, 0:384]), r=[ps[bank]], w=[big[n_]])
        for n_, c_ss, c_r, sc in (("q", 54, 60, 0.125), ("k", 66, 72, 1.0)):
            tt(P, "pool", big["sq"][:], big[n_][:], big[n_][:], ALU.mult, [big[n_]], [big["sq"]])
            P.dve(lambda e, c_ss=c_ss: e.tensor_reduce(v[:, c_ss:c_ss + 6], v3(big["sq"]), AX.X, ALU.add), r=[big["sq"]], w=[v])
            ts(P, "dve", v[:, c_ss:c_ss + 6], v[:, c_ss:c_ss + 6], EPS, None, ALU.add, None, [v], [v])
            actf(P, v[:, c_ss:c_ss + 6], v[:, c_ss:c_ss + 6], AF.Sqrt, [v], [v])
            P.dve(lambda e, c_ss=c_ss, c_r=c_r: e.reciprocal(v[:, c_r:c_r + 6], v[:, c_ss:c_ss + 6]), r=[v], w=[v])
            if sc != 1.0:
                ts(P, "dve", v[:, c_r:c_r + 6], v[:, c_r:c_r + 6], sc, None, ALU.mult, None, [v], [v])
        tt(P, "dve", v3(big["qn"]), v3(big["q"]), hb(60), ALU.mult, [big["q"], v], [big["qn"]])
        tt(P, "dve", v3(big["kn"]), v3(big["k"]), hb(72), ALU.mult, [big["k"], v], [big["kn"]])
        tt(P, "pool", v3(big["qd"]), v3(big["qn"]), hb(36), ALU.mult, [big["qn"], v], [big["qd"]])
        tt(P, "pool", v3(big["kend"]), v3(big["kn"]), hb(30), ALU.mult, [big["kn"], v], [big["kend"]])
        tt(P, "dve", v3(big["kbg"]), v3(big["kn"]), hb(0), ALU.mult, [big["kn"], v], [big["kbg"]])
        tt(P, "dve", v3(big["kbg"]), v3(big["kbg"]), hb(36), ALU.mult, [big["kbg"], v], [big["kbg"]])
        tt(P, "pool", v3(big["vb"]), v3(big["v"]), hb(0), ALU.mult, [big["v"], v], [big["vb"]])
        for h in range(6):
            hs = slice(h * 64, (h + 1) * 64)
            for src, dstl, col in ((big["kn"], knT, 0), (big["qn"], qnT, 128), (big["qd"], qdT, 256)):
                tr(P, C, ps[1][0:64, col:col + 128], src[:, hs], 128, [src], [ps[1]])
            P.act(lambda e, h=h: e.copy(knT[h][:], ps[1][0:64, 0:128]), r=[ps[1]], w=[knT[h]])
            P.dve(lambda e, h=h: e.tensor_copy(qnT[h][:], ps[1][0:64, 128:256]), r=[ps[1]], w=[qnT[h]])
            P.act(lambda e, h=h: e.copy(qdT[h][:], ps[1][0:64, 256:384]), r=[ps[1]], w=[qdT[h]])
        for h in range(6):
            hs = slice(h * 64, (h + 1) * 64)
            mm(P, ps[2][:, 0:128], v[:, 6 + h:7 + h].broadcast_to([128, 128]), C.U2[:], [v, C.U2], [ps[2]])
            stt(P, E[:], ps[2][:, 0:128], v[:, 18 + h:19 + h], C.negU2[:], ALU.add, ALU.add, [ps[2], v, C.negU2], [E])
            actf(P, E[:], E[:], AF.Exp, [E], [E])
            stt(P, DL[:], ps[2][:, 0:128], -1.0, C.negLs2[:], ALU.mult, ALU.add, [ps[2], C.negLs2], [DL])
            actf(P, DL[:], DL[:], AF.Exp, [DL, v], [DL], bias=v[:, 12 + h:13 + h])
            mm(P, ps[3][:, 0:128], knT[h][:], qnT[h][:], [knT[h], qnT[h]], [ps[3]])
            mm(P, ps[3][:, 128:256], knT[h][:], knT[h][:], [knT[h]], [ps[3]])
            tt(P, "dve", qkT[:], ps[3][:, 0:128], E[:], ALU.mult, [ps[3], E], [qkT])
            stt(P, Pk[0][:], ps[3][:, 128:256], v[:, 78 + h:79 + h], DL[:], ALU.mult, ALU.mult, [ps[3], v, DL], [Pk[0]])
            tr(P, C, ps[4][:, 0:128], Pk[0][:], 128, [Pk[0]], [ps[4]])
            P.act(lambda e: e.copy(PkT[0][:], ps[4][:, 0:128]), r=[ps[4]], w=[PkT[0]])
            tt(P, "dve", YT[:], ps[4][:, 0:128], C.ID[:], ALU.add, [ps[4], C.ID], [YT])
            cur = 0
            for lvl in range(1, 6):
                nxt = 1 - cur
                mm(P, ps[4][:, 128:256], PkT[cur][:], Pk[cur][:], [PkT[cur], Pk[cur]], [ps[4]])
                if lvl < 5:
                    mm(P, ps[4][:, 256:384], Pk[cur][:], PkT[cur][:], [PkT[cur], Pk[cur]], [ps[4]])
                P.act(lambda e, nxt=nxt: e.copy(Pk[nxt][:], ps[4][:, 128:256]), r=[ps[4]], w=[Pk[nxt]])
                if lvl < 5:
                    P.dve(lambda e, nxt=nxt: e.tensor_copy(PkT[nxt][:], ps[4][:, 256:384]), r=[ps[4]], w=[PkT[nxt]])
                mm(P, ps[5][:, 0:128], Pk[nxt][:], YT[:], [Pk[nxt], YT], [ps[5]])
                tt(P, "dve", YT[:], YT[:], ps[5][:, 0:128], ALU.add, [YT, ps[5]], [YT])
                cur = nxt
            mm(P, ps[6][:, 0:64], YT[:], big["vb"][:, hs], [YT, big["vb"]], [ps[6]])
            mm(P, ps[6][0:64, 384:512], big["kbg"][:, hs], YT[:], [YT, big["kbg"]], [ps[6]])
            P.act(lambda e: e.copy(Us[:], ps[6][:, 0:64]), r=[ps[6]], w=[Us])
            P.dve(lambda e: e.tensor_copy(WT[:], ps[6][0:64, 384:512]), r=[ps[6]], w=[WT])
            mm(P, ps[7][:, 0:64], WT[:], S[h][:], [WT, S[h]], [ps[7]])
            mm(P, ps[0][:, 128:192], qdT[h][:], S[h][:], [qdT[h], S[h]], [ps[0]], start=True, stop=False)
            tt(P, "dve", vn[0:64, :], Us[0:64, :], ps[7][0:64, 0:64], ALU.subtract, [Us, ps[7]], [vn])
            mm(P, ps[6][0:64, 64:128], big["kend"][0:64, hs], vn[0:64, :], [big["kend"], vn], [ps[6]])
            stt(P, S[h][:], S[h][:], v[0:64, 42 + h:43 + h], ps[6][0:64, 64:128], ALU.mult, ALU.add, [S[h], v, ps[6]], [S[h]])
            mm(P, ps[7][:, 64:128], WT[:], S[h][:], [WT, S[h]], [ps[7]])
            mm(P, ps[1][:, 384:448], qdT[h][:], S[h][:], [qdT[h], S[h]], [ps[1]], start=True, stop=False)
            tt(P, "dve", vn[64:128, :], Us[64:128, :], ps[7][64:128, 64:128], ALU.subtract, [Us, ps[7]], [vn])
            mm(P, ps[6][0:64, 128:192], big["kend"][64:128, hs], vn[64:128, :], [big["kend"], vn], [ps[6]])
            stt(P, S[h][:], S[h][:], v[0:64, 48 + h:49 + h], ps[6][0:64, 128:192], ALU.mult, ALU.add, [S[h], v, ps[6]], [S[h]])
            mm(P, ps[0][:, 128:192], qkT[:], vn[:], [qkT, vn], [ps[0]], start=False, stop=True)
            mm(P, ps[1][:, 384:448], qkT[:], vn[:], [qkT, vn], [ps[1]], start=False, stop=True)
            P.act(lambda e, hs=hs: e.copy(big["o"][0:64, hs], ps[0][0:64, 128:192]), r=[ps[0]], w=[(big["o"], h, 0)])
            P.dve(lambda e, hs=hs: e.tensor_copy(big["o"][64:128, hs], ps[1][64:128, 384:448]), r=[ps[1]], w=[(big["o"], h, 1)])
        ok = [(big["o"], h, k) for h in range(6) for k in range(2)]
        tt(P, "pool", big["sq"][:], big["o"][:], big["o"][:], ALU.mult, ok, [big["sq"]])
        P.dve(lambda e: e.tensor_reduce(v[:, 84:90], v3(big["sq"]), AX.X, ALU.add), r=[big["sq"]], w=[v])
        ts(P, "dve", v[:, 84:90], v[:, 84:90], 1.0 / 64, EPS, ALU.mult, ALU.add, [v], [v])
        actf(P, v[:, 84:90], v[:, 84:90], AF.Sqrt, [v], [v])
        P.dve(lambda e: e.reciprocal(v[:, 90:96], v[:, 84:90]), r=[v], w=[v])
        tt(P, "dve", v3(big["qn"]), v3(big["o"]), hb(90), ALU.mult, ok + [v, big["qn"]], [big["qn"]])
        tt(P, "pool", big["qn"][:], big["qn"][:], bc.t[:, 2, :], ALU.mult, [big["qn"], (bc, 2)], [big["qn"]])
        for i in range(3):
            tr(P, C, ps[1][:, i * 128:(i + 1) * 128], zT[i][:], 128, [zT[i]], [ps[1]])
        actf(P, big["zs"][:], ps[1][:, 0:384], AF.Silu, [ps[1]], [big["zs"]])
        tt(P, "dve", big["qn"][:], big["qn"][:], big["zs"][:], ALU.mult, [big["qn"], big["zs"]], [big["qn"]])
        for i in range(3):
            tr(P, C, ps[2][:, i * 128:(i + 1) * 128], big["qn"][:, i * 128:(i + 1) * 128], 128, [big["qn"]], [ps[2]])
        P.act(lambda e: e.copy(ob[:], ps[2].t[:, 0:384].rearrange("p (k t) -> p k t", k=3)), r=[ps[2]], w=[ob])
        P.dma(mix_d[640:1024, cs].rearrange("(k p) t -> p k t", p=128), ob[:], r=[ob], w=[("mixgdn", blk)])
    P.barrier()
    P.release(m0)


SEQ_FULL = 4096
N_CORES = 8
_LKEYS = None


def build_program(SEQ, layer_shapes):
    nc = bass.Bass("TRN2", target_bir_lowering=False)
    x_d = nc.dram_tensor("x", [SEQ, D], F32, kind="ExternalInput").ap()
    Ws = []
    for l in range(2):
        Ws.append({k: nc.dram_tensor(f"L{l}_{k}", list(shp), F32, kind="ExternalInput").ap() for k, shp in layer_shapes.items()})
    ffg = nc.dram_tensor("ff_g", [1, D, DFF], F32, kind="ExternalInput").ap()
    ffu = nc.dram_tensor("ff_u", [1, D, DFF], F32, kind="ExternalInput").ap()
    ffd = nc.dram_tensor("ff_d", [1, DFF, D], F32, kind="ExternalInput").ap()
    mog = nc.dram_tensor("moe_g", [NE, D, DFF], F32, kind="ExternalInput").ap()
    mou = nc.dram_tensor("moe_u", [NE, D, DFF], F32, kind="ExternalInput").ap()
    mod = nc.dram_tensor("moe_d", [NE, DFF, D], F32, kind="ExternalInput").ap()
    mor = nc.dram_tensor("moe_r", [D, NE], F32, kind="ExternalInput").ap()
    nfin = nc.dram_tensor("nfin", [128, D], F32, kind="ExternalInput").ap()
    out_d = nc.dram_tensor("out", [SEQ, D], F32, kind="ExternalOutput").ap()
    proj_d = nc.dram_tensor("proj_s", [NPROJ, SEQ], F32).ap()
    mix_d = nc.dram_tensor("mix_s", [D, SEQ], BF16).ap()
    xres = nc.dram_tensor("xres_s", [SEQ, D], F32).ap()
    P = Prog(nc)
    C = Ctx()
    setup_common(P, C)
    make_masks(P, C)
    P.barrier()
    for l in range(2):
        W = Ws[l]
        src = x_d if l == 0 else xres
        phase_inproj(P, C, SEQ, src, W["nmix"], W["win"], proj_d)
        mixer_s5(P, C, SEQ, proj_d, mix_d, W)
        mixer_ssd(P, C, SEQ, proj_d, mix_d, W)
        mixer_gdn(P, C, SEQ, proj_d, mix_d, W)
        phase_outproj(P, C, SEQ, src, xres, mix_d, W["wout"])
        if l == 0:
            phase_ffn(P, C, SEQ, xres, xres, W["nffn"], ffg, ffu, ffd, 1)
        else:
            phase_ffn(P, C, SEQ, xres, out_d, W["nffn"], mog, mou, mod, NE, wr_d=mor, nfin_d=nfin)
    finals = [o for e in ENGS for o in P.ops[e] if o.is_dma]
    P.emit(finals[-64:])
    return nc, P


def kernel(**inp):
    inp = {k: np.asarray(v) for k, v in inp.items()}
    x = np.ascontiguousarray(inp["x"], dtype=np.float32)
    B, SEQ, _ = x.shape
    layers = [host_layer_inputs(inp, l) for l in range(2)]
    shapes = {k: v.shape for k, v in layers[0].items()}
    nc, P = build_program(SEQ, shapes)
    f = lambda a: np.ascontiguousarray(np.asarray(a, np.float32))
    common = {}
    for l in range(2):
        for k, v in layers[l].items():
            common[f"L{l}_{k}"] = np.ascontiguousarray(v, dtype=np.float32)
    common["ff_g"] = f(inp["ff_w_gate"])
    common["ff_u"] = f(inp["ff_w_up"])
    common["ff_d"] = f(inp["ff_w_down"])
    common["moe_g"] = f(inp["moe_w_gate"][0])
    common["moe_u"] = f(inp["moe_w_up"][0])
    common["moe_d"] = f(inp["moe_w_down"][0])
    common["moe_r"] = f(inp["moe_router"][0])
    common["nfin"] = rep128(inp["norm_final"])
    in_maps = []
    for c in range(B):
        m = dict(common)
        m["x"] = np.ascontiguousarray(x[c])
        in_maps.append(m)
    res = run_bass_kernel_spmd(nc, in_maps, core_ids=list(range(B)))
    return np.stack([np.asarray(r["out"], dtype=np.float32) for r in res.results], axis=0)
```

```python
import contextlib
import numpy as np
import concourse.bass as bass
import concourse.mybir as mybir
from concourse.bass_utils import run_bass_kernel_spmd

F32 = mybir.dt.float32
BF16 = mybir.dt.bfloat16
I32 = mybir.dt.int32
ALU = mybir.AluOpType
AF = mybir.ActivationFunctionType
AX = mybir.AxisListType

ENGS = ("pe", "act", "dve", "pool", "sp")
NDMASEM = 8


class Op:
    __slots__ = ("eng", "fn", "deps", "sig", "is_dma", "sem", "semval", "barriered")

    def __init__(self, eng, fn, is_dma):
        self.eng = eng
        self.fn = fn
        self.deps = []
        self.sig = None
        self.is_dma = is_dma
        self.sem = None
        self.semval = None
        self.barriered = False


class Tile:
    _n = 0

    def __init__(self, t, name, psum=False):
        self.t = t
        self.name = name
        self.psum = psum
        Tile._n += 1
        self.id = Tile._n

    def __getitem__(self, k):
        return self.t[k]

    def __hash__(self):
        return self.id

    def __eq__(self, o):
        return self is o


class Prog:
    def __init__(self, nc, same_engine_sync=True):
        self.nc = nc
        self.ops = {e: [] for e in ENGS}
        self.lastw = {}
        self.readers = {}
        self.same_engine_sync = same_engine_sync
        self.ndma = {e: 0 for e in ENGS}
        self.dma_last = {}
        self.sb_off = 16640
        self.sb_max = 0
        self.nalloc = 0

    def sb(self, name, shape, dtype, align=64):
        nb = int(np.prod(shape[1:])) * mybir.dt.size(dtype)
        off = (self.sb_off + align - 1) // align * align
        self.nalloc += 1
        t = self.nc.alloc_sbuf_tensor_at(f"{name}_{self.nalloc}", list(shape), dtype, offset=off)
        self.sb_off = off + nb
        self.sb_max = max(self.sb_max, self.sb_off)
        assert self.sb_off <= 229376, f"SBUF overflow {self.sb_off} at {name}"
        return Tile(t, name)

    def mark(self):
        return self.sb_off

    def release(self, m):
        self.sb_off = m

    def op(self, eng, fn, r=(), w=(), is_dma=False):
        o = Op(eng, fn, is_dma)
        if any(isinstance(k, Tile) and k.psum for k in r):
            w = list(w) + [k for k in r if isinstance(k, Tile) and k.psum]
            r = [k for k in r if not (isinstance(k, Tile) and k.psum)]
        deps = {}
        for k in r:
            lw = self.lastw.get(k)
            if lw is not None:
                deps[id(lw)] = lw
        for k in w:
            lw = self.lastw.get(k)
            if lw is not None:
                deps[id(lw)] = lw
            for rd in self.readers.get(k, ()):
                deps[id(rd)] = rd
        if is_dma:
            slot = self.ndma[eng] % NDMASEM
            self.ndma[eng] += 1
            prev = self.dma_last.get((eng, slot))
            if prev is not None:
                deps[id(prev)] = prev
            self.dma_last[(eng, slot)] = o
            o.sem = (eng, slot)
        for d in deps.values():
            if d is o:
                continue
            if (not d.is_dma) and d.eng == eng:
                if eng == "pe" or not self.same_engine_sync:
                    continue
            o.deps.append(d)
        for k in r:
            lst = self.readers.setdefault(k, [])
            if not is_dma:
                lst[:] = [x for x in lst if x.is_dma or x.eng != eng]
            lst.append(o)
        for k in w:
            self.lastw[k] = o
            self.readers[k] = []
        self.ops[eng].append(o)
        return o

    def pe(self, fn, r=(), w=()):
        return self.op("pe", fn, r, w)

    def act(self, fn, r=(), w=()):
        return self.op("act", fn, r, w)

    def dve(self, fn, r=(), w=()):
        return self.op("dve", fn, r, w)

    def pool(self, fn, r=(), w=()):
        return self.op("pool", fn, r, w)

    def dma(self, out, in_, r=(), w=(), eng="sp", **kw):
        return self.op(eng, lambda e: e.dma_start(out, in_, **kw), r, w, is_dma=True)

    def barrier(self):
        tails = []
        for e in ENGS:
            if e == "sp":
                continue
            for o in reversed(self.ops[e]):
                if o.fn is not None and not o.is_dma:
                    tails.append(o)
                    break
        dmas = [o for e in ENGS for o in self.ops[e] if o.is_dma and not o.barriered]
        for o in dmas:
            o.barriered = True
        for e in ENGS:
            b = Op(e, None, False)
            b.deps = [t for t in tails if t.eng != e] + list(dmas)
            self.ops[e].append(b)
        self.lastw.clear()
        self.readers.clear()

    def emit(self, final_waits=()):
        nc = self.nc
        fin = Op("sp", None, False)
        fin.deps = list(final_waits)
        self.ops["sp"].append(fin)
        for e in ENGS:
            for o in self.ops[e]:
                for d in o.deps:
                    d.sig = True
        esem = {}
        dsem = {}
        with contextlib.ExitStack() as st:
            for e in ENGS:
                esem[e] = st.enter_context(nc.semaphore(f"s_{e}"))
            for e in ENGS:
                if self.ndma[e]:
                    for s in range(min(NDMASEM, self.ndma[e])):
                        dsem[(e, s)] = st.enter_context(nc.semaphore(f"d_{e}{s}"))
            dcount = {}
            for e in ENGS:
                c = 0
                for o in self.ops[e]:
                    if o.is_dma:
                        n = dcount.get(o.sem, 0) + 16
                        dcount[o.sem] = n
                        o.semval = n
                        o.sig = True
                    elif o.sig:
                        c += 1
                        o.semval = c
                        o.sem = e
                assert c < 60000, (e, c)
            maxd = max(dcount.values()) if dcount else 0
            assert maxd < 60000, maxd
            self.stats = {e: len(self.ops[e]) for e in ENGS}
            block = st.enter_context(nc.Block())
            engmap = {"pe": block.tensor, "act": block.scalar, "dve": block.vector,
                      "pool": block.gpsimd, "sp": block.sync}
            nw = [0]
            for e in ENGS:
                ops = self.ops[e]
                if not ops:
                    continue

                def body(eng, ops=ops, e=e):
                    seen = {}
                    for o in ops:
                        need = {}
                        for d in o.deps:
                            if d.semval is None:
                                continue
                            if seen.get(d.sem, 0) >= d.semval:
                                continue
                            if need.get(d.sem, 0) < d.semval:
                                need[d.sem] = d.semval
                        for s, v in need.items():
                            sh = esem[s] if isinstance(s, str) else dsem[s]
                            eng.wait_ge(sh, v)
                            seen[s] = v
                            nw[0] += 1
                        if o.fn is None:
                            continue
                        ins = o.fn(eng)
                        if o.is_dma:
                            ins.then_inc(dsem[o.sem], 16)
                        elif o.sig:
                            ins.then_inc(esem[e], 1)

                engmap[e](body)
            self.stats["waits"] = nw[0]
        return nc


D = 1024
DFF = 3584
NE = 8
EPS = 1e-6
NPROJ = 3200
KT = 8


class Ctx:
    pass


def setup_common(P, C):
    nc = P.nc
    C.ps = [Tile(nc.alloc_psum_tensor(f"psb{i}", [128, 512], F32), f"ps{i}", psum=True) for i in range(8)]
    C.ID = P.sb("ID", [128, 128], F32)
    C.IDb = P.sb("IDb", [128, 128], BF16)
    P.pool(lambda e: e.memset(C.ID[:], 1.0), w=[C.ID])
    P.pool(lambda e: e.affine_select(C.ID[:], C.ID[:], [[1, 128]], ALU.is_equal, 0.0, base=0,
                                     channel_multiplier=-1), r=[C.ID], w=[C.ID])
    P.dve(lambda e: e.tensor_copy(C.IDb[:], C.ID[:]), r=[C.ID], w=[C.IDb])
    C.ones = P.sb("ones", [128, 128], F32)
    P.pool(lambda e: e.memset(C.ones[:], 1.0), w=[C.ones])


def norm_tile(P, C, xt, nwbc, xn, ss, junk):
    P.act(lambda e: e.activation(junk[:], xt[:], AF.Square, accum_out=ss[:, 0:1]), r=[xt], w=[junk, ss])
    P.dve(lambda e: e.tensor_scalar(ss[:, 1:2], ss[:, 0:1], 1.0 / D, EPS, ALU.mult, ALU.add), r=[ss], w=[ss])
    P.act(lambda e: e.activation(ss[:, 2:3], ss[:, 1:2], AF.Sqrt), r=[ss], w=[ss])
    P.dve(lambda e: e.reciprocal(ss[:, 3:4], ss[:, 2:3]), r=[ss], w=[ss])
    P.dve(lambda e: e.scalar_tensor_tensor(xn[:], xt[:], ss[:, 3:4], nwbc[:], ALU.mult, ALU.mult),
          r=[xt, ss, nwbc], w=[xn])


def transpose_to_T(P, C, xn, banks, hnT, col0, hn32=None):
    for half in range(2):
        bk = banks[half]
        for j in range(4):
            k = half * 4 + j
            P.pe(lambda e, bk=bk, j=j, k=k: e.transpose(bk[:, j * 128:(j + 1) * 128], xn[:, k * 128:(k + 1) * 128], C.ID[:]),
                 r=[xn, C.ID], w=[bk])
        src = bk.t[:, :].rearrange("p (k t) -> p k t", k=4)
        dst = hnT.t[:, half * 4:half * 4 + 4, col0:col0 + 128]
        if half == 0:
            P.act(lambda e, dst=dst, src=src: e.copy(dst, src), r=[bk], w=[(hnT, col0, 0)])
        else:
            P.dve(lambda e, dst=dst, src=src: e.tensor_copy(dst, src), r=[bk], w=[(hnT, col0, 1)])
        if hn32 is not None:
            d32 = hn32.t[:, half * 4:half * 4 + 4, :]
            if half == 0:
                P.dve(lambda e, d32=d32, src=src: e.tensor_copy(d32, src), r=[bk], w=[(hn32, half)])
            else:
                P.act(lambda e, d32=d32, src=src: e.copy(d32, src), r=[bk], w=[(hn32, half)])


def phase_ffn(P, C, SEQ, xsrc, xdst, nw_d, wg_d, wu_d, wd_d, n_exp, wr_d=None, nfin_d=None, T=1024):
    m0 = P.mark()
    T = min(T, SEQ)
    NTB = T // 512
    moe = wr_d is not None
    FG = 512
    NFG = DFF // FG
    ps = C.ps
    nwbc = P.sb("nwbc", [128, D], F32)
    P.dma(nwbc[:], nw_d, w=[nwbc])
    if nfin_d is not None:
        nfbc = P.sb("nfbc", [128, D], F32)
        P.dma(nfbc[:], nfin_d, w=[nfbc])
    hnT = P.sb("hnT", [128, KT, T], BF16)
    acc = P.sb("acc", [128, KT, T], F32)
    hT = [P.sb(f"hT{i}", [128, 4, T], BF16) for i in range(2)]
    wg = [P.sb(f"wg{i}", [128, KT, FG], BF16) for i in range(2)]
    wu = [P.sb(f"wu{i}", [128, KT, FG], BF16) for i in range(2)]
    wd = [P.sb(f"wd{i}", [128, 4, D], BF16) for i in range(2)]
    sg = [P.sb(f"sg{i}", [128, 512], BF16) for i in range(2)]
    xt = [P.sb(f"xt{i}", [128, D], F32) for i in range(2)]
    xn = [P.sb(f"xn{i}", [128, D], F32) for i in range(2)]
    junk = P.sb("junk", [128, D], F32)
    ssb = [P.sb(f"ss{i}", [128, 8], F32) for i in range(2)]
    if moe:
        wr = P.sb("wr", [128, KT, NE], F32)
        P.dma(wr[:], wr_d.rearrange("(k p) e -> p k e", p=128), w=[wr])
        hn32 = [P.sb(f"hn32{i}", [128, KT, 128], F32) for i in range(2)]
        cbc = P.sb("cbc", [128, NE, T], BF16)
        rt = [P.sb(f"rt{i}", [128, 64], F32) for i in range(2)]
    nsb = SEQ // T
    wcount = 0
    for sbi in range(nsb):
        t0 = sbi * T
        for tt in range(T // 128):
            b = tt % 2
            P.dma(xt[b][:], xsrc[t0 + tt * 128:t0 + (tt + 1) * 128, :], w=[xt[b]])
            norm_tile(P, C, xt[b], nwbc, xn[b], ssb[b], junk)
            banks = (ps[4 + 2 * b], ps[5 + 2 * b])
            transpose_to_T(P, C, xn[b], banks, hnT, tt * 128, hn32[b] if moe else None)
            if moe:
                lg = ps[0] if b == 0 else ps[1]
                R = rt[b]
                for k in range(KT):
                    P.pe(lambda e, k=k, lg=lg, b=b: e.matmul(lg[:, 0:NE], hn32[b][:, k, :], wr[:, k, :], start=(k == 0), stop=(k == KT - 1)),
                         r=[(hn32[b], k // 4), wr], w=[lg])
                P.dve(lambda e, R=R, lg=lg: e.tensor_copy(R[:, 0:8], lg[:, 0:8]), r=[lg], w=[R])
                P.dve(lambda e, R=R: e.tensor_reduce(R[:, 8:9], R[:, 0:8], AX.X, ALU.max), r=[R], w=[R])
                P.dve(lambda e, R=R: e.tensor_scalar(R[:, 9:17], R[:, 0:8], R[:, 8:9], None, ALU.is_equal), r=[R], w=[R])
                P.dve(lambda e, R=R: e.scalar_tensor_tensor(R[:, 17:25], R[:, 9:17], -1e30, R[:, 0:8], ALU.mult, ALU.add), r=[R], w=[R])
                P.dve(lambda e, R=R: e.tensor_reduce(R[:, 25:26], R[:, 17:25], AX.X, ALU.max), r=[R], w=[R])
                P.dve(lambda e, R=R: e.tensor_scalar(R[:, 26:34], R[:, 17:25], R[:, 25:26], None, ALU.is_equal), r=[R], w=[R])
                P.dve(lambda e, R=R: e.tensor_tensor(R[:, 34:35], R[:, 25:26], R[:, 8:9], ALU.subtract), r=[R], w=[R])
                P.act(lambda e, R=R: e.activation(R[:, 35:36], R[:, 34:35], AF.Exp), r=[R], w=[R])
                P.dve(lambda e, R=R: e.tensor_scalar(R[:, 36:37], R[:, 35:36], 1.0, None, ALU.add), r=[R], w=[R])
                P.dve(lambda e, R=R: e.reciprocal(R[:, 37:38], R[:, 36:37]), r=[R], w=[R])
                P.dve(lambda e, R=R: e.tensor_tensor(R[:, 38:39], R[:, 35:36], R[:, 37:38], ALU.mult), r=[R], w=[R])
                P.dve(lambda e, R=R: e.tensor_scalar(R[:, 40:48], R[:, 9:17], R[:, 37:38], None, ALU.mult), r=[R], w=[R])
                P.dve(lambda e, R=R: e.scalar_tensor_tensor(R[:, 40:48], R[:, 26:34], R[:, 38:39], R[:, 40:48], ALU.mult, ALU.add), r=[R], w=[R])
                bb = ps[2] if b == 0 else ps[3]
                for half in range(2):
                    for j in range(4):
                        ex = half * 4 + j
                        P.pe(lambda e, ex=ex, j=j, R=R, bb=bb: e.matmul(bb[:, j * 128:(j + 1) * 128], R[:, 40 + ex:41 + ex].broadcast_to([128, 128]), C.ID[:], start=True, stop=True),
                             r=[R, C.ID], w=[bb])
                    src = bb.t[:, :].rearrange("p (k t) -> p k t", k=4)
                    dst = cbc.t[:, half * 4:half * 4 + 4, tt * 128:(tt + 1) * 128]
                    P.act(lambda e, dst=dst, src=src: e.copy(dst, src), r=[bb], w=[(cbc, tt)])
        nfg_total = n_exp * NFG

        def load_w(g, wb):
            ex, fg = divmod(g, NFG)
            f0 = fg * FG
            P.dma(wg[wb][:], wg_d[ex, :, f0:f0 + FG].rearrange("(k p) f -> p k f", p=128), w=[wg[wb]], eng="pool")
            P.dma(wu[wb][:], wu_d[ex, :, f0:f0 + FG].rearrange("(k p) f -> p k f", p=128), w=[wu[wb]], eng="pool")
            P.dma(wd[wb][:], wd_d[ex, f0:f0 + FG, :].rearrange("(c p) d -> p c d", p=128), w=[wd[wb]], eng="pool")
        for ex in range(n_exp):
            for fg in range(NFG):
                gi = ex * NFG + fg
                wb = wcount % 2
                wcount += 1
                f0 = fg * FG
                if gi == 0:
                    load_w(0, wb)
                if gi + 1 < nfg_total:
                    load_w(gi + 1, 1 - wb)
                hb = hT[wb]
                for fc in range(4):
                    for tb in range(NTB):
                        pg = ps[0 + (fc * NTB + tb) % 2]
                        pu = ps[2 + (fc * NTB + tb) % 2]
                        s = sg[(fc * NTB + tb) % 2]
                        rd = [(hnT, c * 128, h2) for c in range(tb * 4, tb * 4 + 4) for h2 in range(2)]
                        for k in range(KT):
                            P.pe(lambda e, k=k, pg=pg, wb=wb, fc=fc, tb=tb: e.matmul(pg[:], wg[wb][:, k, fc * 128:(fc + 1) * 128], hnT[:, k, tb * 512:(tb + 1) * 512], start=(k == 0), stop=(k == KT - 1)),
                                 r=[wg[wb]] + rd, w=[pg])
                        for k in range(KT):
                            P.pe(lambda e, k=k, pu=pu, wb=wb, fc=fc, tb=tb: e.matmul(pu[:], wu[wb][:, k, fc * 128:(fc + 1) * 128], hnT[:, k, tb * 512:(tb + 1) * 512], start=(k == 0), stop=(k == KT - 1)),
                                 r=[wu[wb]] + rd, w=[pu])
                        P.act(lambda e, s=s, pg=pg: e.activation(s[:], pg[:], AF.Silu), r=[pg], w=[s])
                        hdst = hb.t[:, fc, tb * 512:(tb + 1) * 512]
                        P.dve(lambda e, hdst=hdst, s=s, pu=pu: e.tensor_tensor(hdst, s[:], pu[:], ALU.mult), r=[s, pu], w=[(hb, fc, tb)])
                        if moe:
                            csrc = cbc.t[:, ex, tb * 512:(tb + 1) * 512]
                            P.dve(lambda e, hdst=hdst, csrc=csrc: e.tensor_tensor(hdst, hdst, csrc, ALU.mult),
                                   r=[(hb, fc, tb)] + [(cbc, c) for c in range(tb * 4, tb * 4 + 4)], w=[(hb, fc, tb)])
                for dc in range(KT):
                    for tb in range(NTB):
                        pd = ps[4 + (dc * NTB + tb) % 4]
                        for fc in range(4):
                            P.pe(lambda e, fc=fc, pd=pd, wb=wb, dc=dc, tb=tb, hb=hb: e.matmul(pd[:], wd[wb][:, fc, dc * 128:(dc + 1) * 128], hb[:, fc, tb * 512:(tb + 1) * 512], start=(fc == 0), stop=(fc == 3)),
                                 r=[wd[wb], (hb, fc, tb)], w=[pd])
                        adst = acc.t[:, dc, tb * 512:(tb + 1) * 512]
                        if gi == 0:
                            P.act(lambda e, adst=adst, pd=pd: e.copy(adst, pd[:]), r=[pd], w=[(acc, dc, tb)])
                        else:
                            P.dve(lambda e, adst=adst, pd=pd: e.tensor_tensor(adst, adst, pd[:], ALU.add), r=[pd, (acc, dc, tb)], w=[(acc, dc, tb)])
        for tt in range(T // 128):
            b = tt % 2
            tb = tt // 4
            P.dma(xt[b][:], xsrc[t0 + tt * 128:t0 + (tt + 1) * 128, :], w=[xt[b]])
            banks = (ps[0 + 2 * b], ps[1 + 2 * b])
            for half in range(2):
                bk = banks[half]
                for j in range(4):
                    k = half * 4 + j
                    P.pe(lambda e, bk=bk, j=j, k=k, tt=tt: e.transpose(bk[:, j * 128:(j + 1) * 128], acc[:, k, tt * 128:(tt + 1) * 128], C.ID[:]),
                         r=[(acc, k, tb), C.ID], w=[bk])
                P.dve(lambda e, bk=bk, half=half, b=b: e.tensor_tensor(xn[b][:, half * 512:(half + 1) * 512], xt[b][:, half * 512:(half + 1) * 512], bk[:], ALU.add),
                      r=[bk, xt[b]], w=[xn[b]] if half == 0 else [xn[b]])
            if nfin_d is not None:
                norm_tile(P, C, xn[b], nfbc, xt[b], ssb[b], junk)
                P.dma(xdst[t0 + tt * 128:t0 + (tt + 1) * 128, :], xt[b][:], r=[xt[b]], w=[("xdst", id(xdst), t0 + tt * 128)])
            else:
                P.dma(xdst[t0 + tt * 128:t0 + (tt + 1) * 128, :], xn[b][:], r=[xn[b]], w=[("xdst", id(xdst), t0 + tt * 128)])
    P.barrier()
    P.release(m0)


def phase_inproj(P, C, SEQ, xsrc, nw_d, win_d, proj_d):
    m0 = P.mark()
    ps = C.ps
    nwbc = P.sb("nwbc", [128, D], F32)
    P.dma(nwbc[:], nw_d, w=[nwbc])
    W = P.sb("Win", [128, KT, NPROJ], BF16)
    wv = win_d.rearrange("(k p) n -> p k n", p=128)
    for k in range(KT):
        for h in range(2):
            P.dma(W.t[:, k, h * 1600:(h + 1) * 1600], wv[:, k, h * 1600:(h + 1) * 1600], w=[(W, k, h)], eng="pool")
    wkeys = [(W, k, h) for k in range(KT) for h in range(2)]
    hnT = P.sb("hnT", [128, KT, SEQ], BF16)
    xt = [P.sb(f"xt{i}", [128, D], F32) for i in range(2)]
    xn = [P.sb(f"xn{i}", [128, D], F32) for i in range(2)]
    junk = P.sb("junk", [128, D], F32)
    ssb = [P.sb(f"ss{i}", [128, 8], F32) for i in range(2)]
    stg = [P.sb(f"stg{i}", [128, 512], F32) for i in range(4)]
    for tt in range(SEQ // 128):
        b = tt % 2
        P.dma(xt[b][:], xsrc[tt * 128:(tt + 1) * 128, :], w=[xt[b]])
        norm_tile(P, C, xt[b], nwbc, xn[b], ssb[b], junk)
        transpose_to_T(P, C, xn[b], (ps[4 + 2 * b], ps[5 + 2 * b]), hnT, tt * 128)
    n = 0
    for tb in range(SEQ // 512):
        rd = [(hnT, c * 128, h2) for c in range(tb * 4, tb * 4 + 4) for h2 in range(2)]
        for mc in range(NPROJ // 128):
            pb = ps[n % 4]
            s = stg[n % 4]
            for k in range(KT):
                P.pe(lambda e, k=k, pb=pb, mc=mc, tb=tb: e.matmul(pb[:], W[:, k, mc * 128:(mc + 1) * 128], hnT[:, k, tb * 512:(tb + 1) * 512], start=(k == 0), stop=(k == KT - 1)),
                     r=wkeys + rd, w=[pb])
            if n % 2 == 0:
                P.act(lambda e, s=s, pb=pb: e.copy(s[:], pb[:]), r=[pb], w=[s])
            else:
                P.dve(lambda e, s=s, pb=pb: e.tensor_copy(s[:], pb[:]), r=[pb], w=[s])
            P.dma(proj_d[mc * 128:(mc + 1) * 128, tb * 512:(tb + 1) * 512], s[:], r=[s], w=[("proj", mc, tb)])
            n += 1
    P.barrier()
    P.release(m0)


def phase_outproj(P, C, SEQ, xsrc, xdst, mix_d, wout_d):
    m0 = P.mark()
    ps = C.ps
    Wo = P.sb("Wo", [128, KT, D], BF16)
    P.dma(Wo[:], wout_d.rearrange("(k p) n -> p k n", p=128), w=[Wo], eng="pool")
    mT = P.sb("mT", [128, KT, SEQ], BF16)
    mv = mix_d.rearrange("(k p) t -> p k t", p=128)
    for k in range(KT):
        P.dma(mT.t[:, k, :], mv[:, k, :], w=[(mT, k)])
    mk = [(mT, k) for k in range(KT)]
    xt = [P.sb(f"xt{i}", [128, D], F32) for i in range(2)]
    xn = [P.sb(f"xn{i}", [128, D], F32) for i in range(2)]
    for tt in range(SEQ // 128):
        b = tt % 2
        P.dma(xt[b][:], xsrc[tt * 128:(tt + 1) * 128, :], w=[xt[b]])
        for half in range(2):
            pb = ps[(tt * 2 + half) % 4]
            for k in range(KT):
                P.pe(lambda e, k=k, pb=pb, half=half, tt=tt: e.matmul(pb[:], mT[:, k, tt * 128:(tt + 1) * 128], Wo[:, k, half * 512:(half + 1) * 512], start=(k == 0), stop=(k == KT - 1)),
                     r=mk + [Wo], w=[pb])
            P.dve(lambda e, pb=pb, half=half, b=b: e.tensor_tensor(xn[b][:, half * 512:(half + 1) * 512], xt[b][:, half * 512:(half + 1) * 512], pb[:], ALU.add),
                  r=[pb, xt[b]], w=[xn[b]])
        P.dma(xdst[tt * 128:(tt + 1) * 128, :], xn[b][:], r=[xn[b]], w=[("xo", tt)])
    P.barrier()
    P.release(m0)


TWO_PI = 6.283185307179586
CW1 = 6.28125
CW2 = TWO_PI - CW1
RMAGIC = 12582912.0
PI_SAFE = 3.1415925


def sincos(P, x, sin_o, cos_o, kt, ks, tmp, N):
    xk, sk, ck, kk, tk = ks
    for phase, o, ok in ((0.0, sin_o, sk), (0.25, cos_o, ck)):
        P.dve(lambda e, phase=phase: e.tensor_scalar(kt, x, 1.0 / TWO_PI, phase, ALU.mult, ALU.add), r=[xk], w=[kk])
        P.dve(lambda e: e.tensor_scalar(kt, kt, RMAGIC, RMAGIC, ALU.add, ALU.subtract), r=[kk], w=[kk])
        P.dve(lambda e: e.scalar_tensor_tensor(tmp, kt, -CW1, x, ALU.mult, ALU.add), r=[kk, xk], w=[tk])
        P.dve(lambda e: e.scalar_tensor_tensor(tmp, kt, -CW2, tmp, ALU.mult, ALU.add), r=[kk, tk], w=[tk])
        if phase:
            P.dve(lambda e: e.tensor_scalar(tmp, tmp, 0.25 * TWO_PI, PI_SAFE, ALU.add, ALU.min), r=[tk], w=[tk])
        else:
            P.dve(lambda e: e.tensor_scalar(tmp, tmp, PI_SAFE, None, ALU.min), r=[tk], w=[tk])
        P.dve(lambda e: e.tensor_scalar(tmp, tmp, -PI_SAFE, None, ALU.max), r=[tk], w=[tk])
        P.act(lambda e, o=o: e.activation(o, tmp, AF.Sin), r=[tk], w=[ok])


def mixer_s5(P, C, SEQ, proj_d, mix_d, W):
    m0 = P.mark()
    ps = C.ps
    Q = 512
    NCH = SEQ // Q
    prm = P.sb("s5prm", [128, 12, 8], F32)
    for i, nm in enumerate(("a_re_s", "a_im_s", "ls_s")):
        P.dma(prm.t[:, i, :], W[nm], w=[(prm, i)])
    pk = lambda i: (prm, i)
    P.act(lambda e: e.activation(prm.t[:, 3, :], prm.t[:, 2, :], AF.Exp), r=[pk(2)], w=[pk(3)])
    P.dve(lambda e: e.tensor_tensor(prm.t[:, 4, :], prm.t[:, 0, :], prm.t[:, 3, :], ALU.mult), r=[pk(0), pk(3)], w=[pk(4)])
    P.act(lambda e: e.activation(prm.t[:, 4, :], prm.t[:, 4, :], AF.Exp), r=[pk(4)], w=[pk(4)])
    P.dve(lambda e: e.tensor_tensor(prm.t[:, 5, :], prm.t[:, 1, :], prm.t[:, 3, :], ALU.mult), r=[pk(1), pk(3)], w=[pk(5)])
    QT = Q + 1
    cosT = P.sb("cosT", [128, 8, QT], F32)
    sinT = P.sb("sinT", [128, 8, QT], F32)
    io = P.sb("iota", [128, QT], F32)
    P.pool(lambda e: e.iota(io[:], [[1, QT]], base=0, channel_multiplier=0, allow_small_or_imprecise_dtypes=True), w=[io])
    xa = P.sb("xa", [128, QT], F32)
    kt_ = P.sb("kts", [128, QT], F32)
    tp_ = P.sb("tps", [128, QT], F32)
    for j in range(8):
        P.dve(lambda e, j=j: e.tensor_scalar(xa[:], io[:], prm.t[:, 5, j:j + 1], None, ALU.mult), r=[io, pk(5)], w=[xa])
        sincos(P, xa[:], sinT.t[:, j, :], cosT.t[:, j, :], kt_[:], (xa, (sinT, j), (cosT, j), kt_, tp_), tp_[:], QT)
    m1 = P.mark()
    rw = [P.sb(f"s5rw{i}", [128, 1024], F32) for i in range(10)]
    P.dma(rw[0][:], W["a_re_r"], w=[rw[0]])
    P.dma(rw[1][:], W["a_im_r"], w=[rw[1]])
    P.dma(rw[2][:], W["ls_r"], w=[rw[2]])
    P.act(lambda e: e.activation(rw[2][:], rw[2][:], AF.Exp), r=[rw[2]], w=[rw[2]])
    P.dve(lambda e: e.tensor_tensor(rw[3][:], rw[0][:], rw[2][:], ALU.mult), r=[rw[0], rw[2]], w=[rw[3]])
    P.act(lambda e: e.activation(rw[3][:], rw[3][:], AF.Exp), r=[rw[3]], w=[rw[3]])
    P.dve(lambda e: e.tensor_tensor(rw[4][:], rw[1][:], rw[2][:], ALU.mult), r=[rw[1], rw[2]], w=[rw[4]])
    sincos(P, rw[4][:], rw[5][:], rw[6][:], rw[7][:], (rw[4], rw[5], rw[6], rw[7], rw[8]), rw[8][:], 1024)
    P.dve(lambda e: e.tensor_tensor(rw[6][:], rw[6][:], rw[3][:], ALU.mult), r=[rw[6], rw[3]], w=[rw[6]])
    P.dve(lambda e: e.tensor_scalar(rw[6][:], rw[6][:], -1.0, None, ALU.add), r=[rw[6]], w=[rw[6]])
    P.dve(lambda e: e.tensor_tensor(rw[5][:], rw[5][:], rw[3][:], ALU.mult), r=[rw[5], rw[3]], w=[rw[5]])
    P.dve(lambda e: e.tensor_tensor(rw[9][:], rw[0][:], rw[0][:], ALU.mult), r=[rw[0]], w=[rw[9]])
    P.dve(lambda e: e.tensor_tensor(rw[7][:], rw[1][:], rw[1][:], ALU.mult), r=[rw[1]], w=[rw[7]])
    P.dve(lambda e: e.tensor_tensor(rw[9][:], rw[9][:], rw[7][:], ALU.add), r=[rw[9], rw[7]], w=[rw[9]])
    P.dve(lambda e: e.reciprocal(rw[9][:], rw[9][:]), r=[rw[9]], w=[rw[9]])
    P.dve(lambda e: e.tensor_tensor(rw[7][:], rw[6][:], rw[0][:], ALU.mult), r=[rw[6], rw[0]], w=[rw[7]])
    P.dve(lambda e: e.tensor_tensor(rw[8][:], rw[5][:], rw[1][:], ALU.mult), r=[rw[5], rw[1]], w=[rw[8]])
    P.dve(lambda e: e.tensor_tensor(rw[7][:], rw[7][:], rw[8][:], ALU.add), r=[rw[7], rw[8]], w=[rw[7]])
    P.dve(lambda e: e.tensor_tensor(rw[7][:], rw[7][:], rw[9][:], ALU.mult), r=[rw[7], rw[9]], w=[rw[7]])
    P.dve(lambda e: e.tensor_tensor(rw[8][:], rw[5][:], rw[0][:], ALU.mult), r=[rw[5], rw[0]], w=[rw[8]])
    P.dve(lambda e: e.tensor_tensor(rw[4][:], rw[6][:], rw[1][:], ALU.mult), r=[rw[6], rw[1]], w=[rw[4]])
    P.dve(lambda e: e.tensor_tensor(rw[8][:], rw[8][:], rw[4][:], ALU.subtract), r=[rw[8], rw[4]], w=[rw[8]])
    P.dve(lambda e: e.tensor_tensor(rw[8][:], rw[8][:], rw[9][:], ALU.mult), r=[rw[8], rw[9]], w=[rw[8]])
    P.dma(rw[0][:], W["wb_re"], w=[rw[0]])
    P.dma(rw[1][:], W["wb_im"], w=[rw[1]])
    Bb_re = P.sb("Bb_re", [128, 1024], BF16)
    Bb_im = P.sb("Bb_im", [128, 1024], BF16)
    P.dve(lambda e: e.tensor_tensor(rw[2][:], rw[7][:], rw[0][:], ALU.mult), r=[rw[7], rw[0]], w=[rw[2]])
    P.dve(lambda e: e.tensor_tensor(rw[3][:], rw[8][:], rw[1][:], ALU.mult), r=[rw[8], rw[1]], w=[rw[3]])
    P.dve(lambda e: e.tensor_tensor(Bb_re[:], rw[2][:], rw[3][:], ALU.subtract), r=[rw[2], rw[3]], w=[Bb_re])
    P.dve(lambda e: e.tensor_tensor(rw[2][:], rw[7][:], rw[1][:], ALU.mult), r=[rw[7], rw[1]], w=[rw[2]])
    P.dve(lambda e: e.tensor_tensor(rw[3][:], rw[8][:], rw[0][:], ALU.mult), r=[rw[8], rw[0]], w=[rw[3]])
    P.dve(lambda e: e.tensor_tensor(Bb_im[:], rw[2][:], rw[3][:], ALU.add), r=[rw[2], rw[3]], w=[Bb_im])
    Wc_re = P.sb("Wc_re", [128, 1024], F32)
    Wc_im = P.sb("Wc_im", [128, 1024], F32)
    P.dma(rw[4][:], W["wc_re"], w=[rw[4]])
    P.dma(rw[5][:], W["wc_im"], w=[rw[5]])
    P.act(lambda e: e.copy(Wc_re[:], rw[4][:]), r=[rw[4]], w=[Wc_re])
    P.act(lambda e: e.mul(Wc_im[:], rw[5][:], -1.0), r=[rw[5]], w=[Wc_im])
    wglu = P.sb("wglu", [128, 2, 256], BF16)
    P.dma(wglu[:], W["wglu"].rearrange("(k p) n -> p k n", p=128), w=[wglu], eng="pool")
    cols = P.sb("s5cols", [128, 4], F32)
    P.dma(cols[:, 0:2], W["dcol"], w=[(cols, 0)])
    P.dma(cols[:, 2:4], W["ncol"], w=[(cols, 1)])
    P.barrier()
    keep = P.mark()
    u_bf = P.sb("u_bf", [128, 2, SEQ], BF16)
    uv = proj_d[0:256, :].rearrange("(k p) t -> p k t", p=128)
    for k in range(2):
        for c in range(SEQ // 2048 if SEQ >= 2048 else 1):
            w_ = min(2048, SEQ)
            P.dma(u_bf.t[:, k, c * w_:(c + 1) * w_], uv[:, k, c * w_:(c + 1) * w_], w=[(u_bf, k, c)], eng="pool")
    ini = P.sb("ini", [128, 8, 2], F32)
    P.dve(lambda e: e.memset(ini[:], 0.0), w=[ini])
    ini2 = P.sb("ini2", [128, 8, 4], F32)
    t = [P.sb(f"s5t{i}", [128, Q], F32) for i in range(8)]
    Wr = [P.sb(f"Wr{i}", [128, Q], F32) for i in range(2)]
    Wi = [P.sb(f"Wi{i}", [128, Q], F32) for i in range(2)]
    S_re = P.sb("S_re", [128, 8, Q], F32)
    S_im = P.sb("S_im", [128, 8, Q], F32)
    u32 = P.sb("u32", [128, 2, Q], F32)
    pt = [P.sb(f"s5p{i}", [128, Q], F32) for i in range(4)]
    gl = P.sb("s5gl", [128, 2, Q], BF16)
    y2 = P.sb("s5y2", [128, 2, Q], F32)
    sq = P.sb("s5sq", [128, 2, Q], F32)
    ob = P.sb("s5ob", [128, 2, Q], BF16)
    for c in range(NCH):
        c0 = c * Q
        ukeys = [(u_bf, k, c0 // 2048) for k in range(2)]
        P.dma(u32[:], uv[:, :, c0:c0 + Q], w=[u32])
        for j in range(8):
            b = j % 2
            ut = j // 4
            pa, pb_ = ps[0 + b], ps[2 + b]
            P.pe(lambda e, j=j, pa=pa, ut=ut, c0=c0: e.matmul(pa[:], Bb_re[:, j * 128:(j + 1) * 128], u_bf[:, ut, c0:c0 + Q], start=True, stop=True), r=[Bb_re] + ukeys, w=[pa])
            P.pe(lambda e, j=j, pb_=pb_, ut=ut, c0=c0: e.matmul(pb_[:], Bb_im[:, j * 128:(j + 1) * 128], u_bf[:, ut, c0:c0 + Q], start=True, stop=True), r=[Bb_im] + ukeys, w=[pb_])
            cs, sn = cosT.t[:, j, 0:Q], sinT.t[:, j, 0:Q]
            T0, T1, T2, T3 = t[4 * b:4 * b + 4]
            P.dve(lambda e, T0=T0, pa=pa, cs=cs: e.tensor_tensor(T0[:], pa[:], cs, ALU.mult), r=[pa, (cosT, j)], w=[T0])
            P.dve(lambda e, T1=T1, pb_=pb_, sn=sn: e.tensor_tensor(T1[:], pb_[:], sn, ALU.mult), r=[pb_, (sinT, j)], w=[T1])
            P.dve(lambda e, T2=T2, pb_=pb_, cs=cs: e.tensor_tensor(T2[:], pb_[:], cs, ALU.mult), r=[pb_, (cosT, j)], w=[T2])
            P.dve(lambda e, T3=T3, pa=pa, sn=sn: e.tensor_tensor(T3[:], pa[:], sn, ALU.mult), r=[pa, (sinT, j)], w=[T3])
            P.pool(lambda e, T0=T0, T1=T1: e.tensor_tensor(T0[:], T0[:], T1[:], ALU.add), r=[T0, T1], w=[T0])
            P.pool(lambda e, T2=T2, T3=T3: e.tensor_tensor(T2[:], T2[:], T3[:], ALU.subtract), r=[T2, T3], w=[T2])
            rmag = prm.t[:, 4, j:j + 1].broadcast_to([128, Q])
            P.dve(lambda e, b=b, T0=T0, rmag=rmag, j=j: e.tensor_tensor_scan(Wr[b][:], rmag, T0[:], ini.t[:, j, 0:1], ALU.mult, ALU.add), r=[T0, pk(4), ini], w=[Wr[b]])
            P.dve(lambda e, b=b, T2=T2, rmag=rmag, j=j: e.tensor_tensor_scan(Wi[b][:], rmag, T2[:], ini.t[:, j, 1:2], ALU.mult, ALU.add), r=[T2, pk(4), ini], w=[Wi[b]])
            P.pool(lambda e, T0=T0, b=b, cs=cs: e.tensor_tensor(T0[:], Wr[b][:], cs, ALU.mult), r=[Wr[b], (cosT, j)], w=[T0])
            P.pool(lambda e, T1=T1, b=b, sn=sn: e.tensor_tensor(T1[:], Wi[b][:], sn, ALU.mult), r=[Wi[b], (sinT, j)], w=[T1])
            P.pool(lambda e, T0=T0, T1=T1, j=j: e.tensor_tensor(S_re.t[:, j, :], T0[:], T1[:], ALU.subtract), r=[T0, T1], w=[(S_re, j)])
            P.pool(lambda e, T2=T2, b=b, sn=sn: e.tensor_tensor(T2[:], Wr[b][:], sn, ALU.mult), r=[Wr[b], (sinT, j)], w=[T2])
            P.pool(lambda e, T3=T3, b=b, cs=cs: e.tensor_tensor(T3[:], Wi[b][:], cs, ALU.mult), r=[Wi[b], (cosT, j)], w=[T3])
            P.pool(lambda e, T2=T2, T3=T3, j=j: e.tensor_tensor(S_im.t[:, j, :], T2[:], T3[:], ALU.add), r=[T2, T3], w=[(S_im, j)])
            cq, sq_ = cosT.t[:, j, Q:Q + 1], sinT.t[:, j, Q:Q + 1]
            P.dve(lambda e, b=b, j=j, cq=cq: e.tensor_tensor(ini2.t[:, j, 0:1], Wr[b][:, Q - 1:Q], cq, ALU.mult), r=[Wr[b], (cosT, j)], w=[ini2])
            P.dve(lambda e, b=b, j=j, sq_=sq_: e.tensor_tensor(ini2.t[:, j, 1:2], Wi[b][:, Q - 1:Q], sq_, ALU.mult), r=[Wi[b], (sinT, j)], w=[ini2])
            P.dve(lambda e, b=b, j=j, sq_=sq_: e.tensor_tensor(ini2.t[:, j, 2:3], Wr[b][:, Q - 1:Q], sq_, ALU.mult), r=[Wr[b], (sinT, j)], w=[ini2])
            P.dve(lambda e, b=b, j=j, cq=cq: e.tensor_tensor(ini2.t[:, j, 3:4], Wi[b][:, Q - 1:Q], cq, ALU.mult), r=[Wi[b], (cosT, j)], w=[ini2])
            P.dve(lambda e, j=j: e.tensor_tensor(ini.t[:, j, 0:1], ini2.t[:, j, 0:1], ini2.t[:, j, 1:2], ALU.subtract), r=[ini2], w=[ini])
            P.dve(lambda e, j=j: e.tensor_tensor(ini.t[:, j, 1:2], ini2.t[:, j, 2:3], ini2.t[:, j, 3:4], ALU.add), r=[ini2], w=[ini])
        for ut in range(2):
            py = ps[4 + ut]
            n = 0
            for j in range(4 * ut, 4 * ut + 4):
                P.pe(lambda e, j=j, py=py, n=n: e.matmul(py[:], Wc_re[:, j * 128:(j + 1) * 128], S_re[:, j, :], start=(n == 0), stop=False), r=[Wc_re, (S_re, j)], w=[py])
                n += 1
                P.pe(lambda e, j=j, py=py, n=n: e.matmul(py[:], Wc_im[:, j * 128:(j + 1) * 128], S_im[:, j, :], start=False, stop=(n == 7)), r=[Wc_im, (S_im, j)], w=[py])
                n += 1
            Y, A1, A2, A3 = pt
            P.dve(lambda e, ut=ut, py=py: e.scalar_tensor_tensor(Y[:], u32[:, ut, :], cols[:, ut:ut + 1], py[:], ALU.mult, ALU.add), r=[u32, (cols, 0), py], w=[Y])
            P.pool(lambda e: e.tensor_tensor(A1[:], Y[:], Y[:], ALU.mult), r=[Y], w=[A1])
            P.pool(lambda e: e.tensor_scalar(A1[:], A1[:], 0.044715, 1.0, ALU.mult, ALU.add), r=[A1], w=[A1])
            P.pool(lambda e: e.tensor_tensor(A1[:], A1[:], Y[:], ALU.mult), r=[A1, Y], w=[A1])
            P.act(lambda e: e.activation(A2[:], A1[:], AF.Sigmoid, scale=1.5957691216057308), r=[A1], w=[A2])
            P.dve(lambda e, ut=ut: e.tensor_tensor(y2.t[:, ut, :], Y[:], A2[:], ALU.mult), r=[Y, A2], w=[(y2, ut)])
            P.act(lambda e, ut=ut: e.copy(gl.t[:, ut, :], y2.t[:, ut, :]), r=[(y2, ut)], w=[(gl, ut)])
        for mo in range(2):
            pz = ps[6 + mo]
            for k in range(2):
                P.pe(lambda e, k=k, mo=mo, pz=pz: e.matmul(pz[:], wglu[:, k, mo * 128:(mo + 1) * 128], gl[:, k, :], start=(k == 0), stop=(k == 1)), r=[wglu, (gl, 0), (gl, 1)], w=[pz])
            A1 = pt[1]
            P.act(lambda e, pz=pz: e.activation(A1[:], pz[:], AF.Sigmoid), r=[pz], w=[A1])
            P.dve(lambda e, mo=mo: e.tensor_tensor(y2.t[:, mo, :], y2.t[:, mo, :], A1[:], ALU.mult), r=[(y2, mo), A1], w=[(y2, mo)])
            P.pool(lambda e, mo=mo: e.tensor_tensor(sq.t[:, mo, :], y2.t[:, mo, :], y2.t[:, mo, :], ALU.mult), r=[(y2, mo)], w=[(sq, mo)])
        pss = ps[4]
        for k in range(2):
            P.pe(lambda e, k=k: e.matmul(pss[:], C.ones[:], sq[:, k, :], start=(k == 0), stop=(k == 1)), r=[C.ones, (sq, k)], w=[pss])
        R1, R2 = pt[2], pt[3]
        P.dve(lambda e: e.tensor_scalar(R1[:], pss[:], 1.0 / 256, EPS, ALU.mult, ALU.add), r=[pss], w=[R1])
        P.act(lambda e: e.activation(R1[:], R1[:], AF.Sqrt), r=[R1], w=[R1])
        P.dve(lambda e: e.reciprocal(R2[:], R1[:]), r=[R1], w=[R2])
        for mo in range(2):
            P.dve(lambda e, mo=mo: e.scalar_tensor_tensor(ob.t[:, mo, :], y2.t[:, mo, :], cols[:, 2 + mo:3 + mo], R2[:], ALU.mult, ALU.mult), r=[(y2, mo), (cols, 1), R2], w=[(ob, mo)])
            P.dma(mix_d[mo * 128:(mo + 1) * 128, c0:c0 + Q], ob.t[:, mo, :], r=[(ob, mo)], w=[("mix", mo, c)])
    P.barrier()
    P.release(m0)


def rep128(v):
    v = np.asarray(v, np.float32).reshape(1, -1)
    return np.ascontiguousarray(np.broadcast_to(v, (128, v.shape[1])))


def host_layer_inputs(inp, l):
    o = {}
    f = lambda a: np.ascontiguousarray(np.asarray(a, np.float32))
    win = f(inp["w_in"][l])
    wp = np.zeros((D, NPROJ), np.float32)
    wp[:, 0:1536] = win[:, 0:1536]
    wp[:, 1536:2688] = win[:, 1542:2694]
    wp[:, 2688:3072] = win[:, 2694:3078]
    wp[:, 3072:3078] = win[:, 1536:1542]
    wp[:, 3078:3084] = win[:, 3078:3084]
    wp[:, 3084:3090] = win[:, 3084:3090]
    o["win"] = wp
    o["wout"] = f(inp["w_out"][l])
    o["nmix"] = rep128(inp["norm_mix"][l])
    o["nffn"] = rep128(inp["norm_ffn"][l])
    def slay(a):
        return np.ascontiguousarray(f(a).reshape(8, 2, 64).transpose(1, 2, 0).reshape(128, 8))
    o["a_re_s"] = slay(inp["s5_a_re"][l])
    o["a_im_s"] = slay(inp["s5_a_im"][l])
    o["ls_s"] = slay(np.broadcast_to(f(inp["s5_log_step"][l])[:, None], (16, 64)))
    o["a_re_r"] = rep128(f(inp["s5_a_re"][l]).reshape(-1))
    o["a_im_r"] = rep128(f(inp["s5_a_im"][l]).reshape(-1))
    o["ls_r"] = rep128(np.broadcast_to(f(inp["s5_log_step"][l])[:, None], (16, 64)).reshape(-1))
    for nm, src in (("wb_re", inp["s5_b_re"][l]), ("wb_im", inp["s5_b_im"][l])):
        B = f(src)
        wb = np.zeros((128, 8, 128), np.float32)
        for g in range(16):
            j, two = divmod(g, 2)
            r0 = (g % 8) * 16
            wb[r0:r0 + 16, j, two * 64:(two + 1) * 64] = B[g].T
        o[nm] = wb.reshape(128, 1024)
    for nm, src in (("wc_re", inp["s5_c_re"][l]), ("wc_im", inp["s5_c_im"][l])):
        Cm = f(src)
        wc = np.zeros((128, 8, 128), np.float32)
        for g in range(16):
            j, two = divmod(g, 2)
            c0 = (g % 8) * 16
            wc[two * 64:(two + 1) * 64, j, c0:c0 + 16] = Cm[g].T
        o[nm] = wc.reshape(128, 1024)
    o["dcol"] = np.ascontiguousarray(f(inp["s5_d"][l]).reshape(2, 128).T)
    o["ncol"] = np.ascontiguousarray(f(inp["s5_norm"][l]).reshape(2, 128).T)
    o["wglu"] = f(inp["s5_w_glu"][l])
    o["ssd_cw"] = np.ascontiguousarray(f(inp["ssd_conv_w"][l]).reshape(4, 7, 128).transpose(2, 1, 0))
    o["ssd_cb"] = np.ascontiguousarray(f(inp["ssd_conv_b"][l]).reshape(7, 128).T)
    o["ssd_dtb"] = rep128(inp["ssd_dt_bias"][l])
    o["ssd_alog"] = rep128(inp["ssd_a_log"][l])
    o["ssd_drep"] = rep128(np.repeat(f(inp["ssd_d"][l]), 64))
    o["ssd_nw"] = rep128(inp["ssd_norm"][l])
    o["gdn_cw"] = np.ascontiguousarray(f(inp["gdn_conv_w"][l]).reshape(4, 9, 128).transpose(2, 1, 0))
    o["gdn_alog"] = rep128(inp["gdn_a_log"][l])
    o["gdn_dtb"] = rep128(inp["gdn_dt_bias"][l])
    o["gdn_nw"] = rep128(np.tile(f(inp["gdn_norm"][l]), 6))
    return o


F32R = mybir.dt.float32r
USE_F32R = False


def mm(P, out, lhsT, rhs, r, w, start=True, stop=True, exact=False):
    if USE_F32R and not exact and lhsT.dtype == F32 and rhs.dtype == F32:
        lhsT = lhsT.bitcast(F32R)
        rhs = rhs.bitcast(F32R)
    P.pe(lambda e: e.matmul(out, lhsT, rhs, start=start, stop=stop), r=r, w=w)


def tr(P, C, out, in_, n, r, w):
    P.pe(lambda e: e.transpose(out, in_, C.ID[0:n, 0:n]), r=list(r) + [C.ID], w=w)


def tt(P, eng, out, a, b, op, r, w):
    P.op(eng, lambda e: e.tensor_tensor(out, a, b, op), r, w)


def ts(P, eng, out, a, s1, s2, op0, op1, r, w):
    if op1 is None:
        P.op(eng, lambda e: e.tensor_scalar(out, a, s1, None, op0), r, w)
    else:
        P.op(eng, lambda e: e.tensor_scalar(out, a, s1, s2, op0, op1), r, w)


def stt(P, out, a, s, b, op0, op1, r, w):
    P.dve(lambda e: e.scalar_tensor_tensor(out, a, s, b, op0, op1), r, w)


def actf(P, out, in_, func, r, w, **kw):
    P.act(lambda e: e.activation(out, in_, func, **kw), r, w)


def conv_silu(P, C, SEQ, proj_d, row0, ntile, cw, cb, dst, xin, acc):
    for i in range(ntile):
        P.dve(lambda e: e.memset(xin[:, 0:3], 0.0), w=[xin])
        P.dma(xin[:, 3:3 + SEQ], proj_d[row0 + i * 128:row0 + (i + 1) * 128, :], w=[xin])
        ts(P, "dve", acc[:], xin[:, 0:SEQ], cw[:, i, 0:1], None, ALU.mult, None, [xin, cw], [acc])
        for k in range(1, 4):
            stt(P, acc[:], xin[:, k:k + SEQ], cw[:, i, k:k + 1], acc[:], ALU.mult, ALU.add, [xin, cw, acc], [acc])
        if cb is not None:
            actf(P, dst[i][:], acc[:], AF.Silu, [acc, cb], [dst[i]], bias=cb[:, i:i + 1])
        else:
            actf(P, dst[i][:], acc[:], AF.Silu, [acc], [dst[i]])


def conv_blk(P, SEQ, proj_d, row0, ntile, cw, cb, dst, xin, acc, t0):
    for i in range(ntile):
        xi = xin[i % 2]
        if t0 == 0:
            P.dve(lambda e, xi=xi: e.memset(xi[:, 0:3], 0.0), w=[xi])
            P.dma(xi[:, 3:131], proj_d[row0 + i * 128:row0 + (i + 1) * 128, 0:128], w=[xi])
        else:
            P.dma(xi[:, 0:131], proj_d[row0 + i * 128:row0 + (i + 1) * 128, t0 - 3:t0 + 128], w=[xi])
        ts(P, "dve", acc[:], xi[:, 0:128], cw[:, i, 0:1], None, ALU.mult, None, [xi, cw], [acc])
        for k in range(1, 4):
            stt(P, acc[:], xi[:, k:k + 128], cw[:, i, k:k + 1], acc[:], ALU.mult, ALU.add, [xi, cw, acc], [acc])
        if cb is not None:
            actf(P, dst[i][:], acc[:], AF.Silu, [acc, cb], [dst[i]], bias=cb[:, i:i + 1])
        else:
            actf(P, dst[i][:], acc[:], AF.Silu, [acc], [dst[i]])


def make_masks(P, C):
    def tri(name, cmp_base, cm, step):
        t = P.sb(name, [128, 128], F32)
        P.pool(lambda e: e.memset(t[:], 1.0), w=[t])
        P.pool(lambda e: e.affine_select(t[:], t[:], [[step, 128]], ALU.is_ge, 0.0, base=cmp_base, channel_multiplier=cm), r=[t], w=[t])
        return t
    C.U = tri("U", 0, -1, 1)
    C.Ls = tri("Ls", -1, 1, -1)
    C.L = tri("L", 0, 1, -1)
    C.BD = P.sb("BD", [128, 128], F32)
    P.pool(lambda e: e.memset(C.BD[:], 0.0), w=[C.BD])
    P.pool(lambda e: e.memset(C.BD[0:64, 0:64], 1.0), r=[C.BD], w=[C.BD])
    P.pool(lambda e: e.memset(C.BD[64:128, 64:128], 1.0), r=[C.BD], w=[C.BD])
    C.SEL0 = P.sb("SEL0", [128, 128], F32)
    C.SEL1 = P.sb("SEL1", [128, 128], F32)
    P.pool(lambda e: e.memset(C.SEL0[:], 0.0), w=[C.SEL0])
    P.pool(lambda e: e.memset(C.SEL0[0:64, :], 1.0), r=[C.SEL0], w=[C.SEL0])
    P.pool(lambda e: e.memset(C.SEL1[:], 0.0), w=[C.SEL1])
    P.pool(lambda e: e.memset(C.SEL1[64:128, :], 1.0), r=[C.SEL1], w=[C.SEL1])
    def neg(name, m01, extra=None):
        t = P.sb(name, [128, 128], F32)
        if extra is not None:
            tt(P, "pool", t[:], m01[:], extra[:], ALU.mult, [m01, extra], [t])
            ts(P, "pool", t[:], t[:], 1e30, -1e30, ALU.mult, ALU.add, [t], [t])
        else:
            ts(P, "pool", t[:], m01[:], 1e30, -1e30, ALU.mult, ALU.add, [m01], [t])
        return t
    C.negU = neg("negU", C.U)
    C.U2 = P.sb("U2", [128, 128], F32)
    tt(P, "pool", C.U2[:], C.U[:], C.BD[:], ALU.mult, [C.U, C.BD], [C.U2])
    C.negU2 = neg("negU2", C.U2)
    C.negLs2 = neg("negLs2", C.Ls, C.BD)


def mixer_ssd(P, C, SEQ, proj_d, mix_d, W):
    m0 = P.mark()
    ps = C.ps
    NB = SEQ // 128
    cw = P.sb("ssd_cw", [128, 7, 4], F32)
    cb = P.sb("ssd_cb", [128, 7], F32)
    P.dma(cw[:], W["ssd_cw"], w=[cw])
    P.dma(cb[:], W["ssd_cb"], w=[cb])
    bc = P.sb("ssd_bc", [128, 4, 384], F32)
    P.dma(bc.t[:, 0, 0:6], W["ssd_dtb"], w=[(bc, 0)])
    P.dma(bc.t[:, 1, 0:6], W["ssd_alog"], w=[(bc, 1)])
    P.dma(bc.t[:, 2, :], W["ssd_drep"], w=[(bc, 2)])
    P.dma(bc.t[:, 3, :], W["ssd_nw"], w=[(bc, 3)])
    actf(P, bc.t[:, 1, 0:6], bc.t[:, 1, 0:6], AF.Exp, [(bc, 1)], [(bc, 1)])
    ts(P, "dve", bc.t[:, 1, 0:6], bc.t[:, 1, 0:6], -1.0, None, ALU.mult, None, [(bc, 1)], [(bc, 1)])
    xin = [P.sb(f"cv_in{i}", [128, 131], F32) for i in range(2)]
    acc = P.sb("cv_acc", [128, 128], F32)
    F = [P.sb(f"ssdF{i}", [128, 128], F32) for i in range(7)]
    zT = [P.sb(f"ssdz{i}", [128, 128], F32) for i in range(3)]
    sm = P.sb("ssd_sm", [6, SEQ], F32)
    P.dma(sm[:], proj_d[3072:3078, :], w=[sm])
    S = P.sb("ssdS", [128, 6, 64], F32)
    P.dve(lambda e: e.memset(S[:], 0.0), w=[S])
    v = P.sb("ssdv", [128, 64], F32)
    E = [P.sb(f"ssdE{i}", [128, 128], F32) for i in range(2)]
    Mt = [P.sb(f"ssdM{i}", [128, 128], F32) for i in range(2)]
    Xtm = P.sb("ssdXtm", [128, 384], F32)
    xdt = P.sb("ssdxdt", [128, 384], F32)
    xdd = P.sb("ssdxdd", [128, 384], F32)
    Btm = P.sb("ssdBtm", [128, 256], F32)
    yd = P.sb("ssdyd", [128, 384], F32)
    y = P.sb("ssdy", [128, 384], F32)
    zs = P.sb("ssdzs", [128, 384], F32)
    junk = P.sb("ssdjunk", [128, 192], F32)
    ssq = P.sb("ssdss", [128, 8], F32)
    ob = P.sb("ssdob", [128, 3, 128], BF16)
    for blk in range(NB):
        t0 = blk * 128
        cs = slice(t0, t0 + 128)
        conv_blk(P, SEQ, proj_d, 640, 7, cw, cb, F, xin, acc, t0)
        for i in range(3):
            P.dma(zT[i][:], proj_d[256 + i * 128:256 + (i + 1) * 128, cs], w=[zT[i]])
        tr(P, C, ps[0][:, 0:6], sm[0:6, cs], 6, [sm], [ps[0]])
        tt(P, "dve", v[:, 0:6], ps[0][:, 0:6], bc.t[:, 0, 0:6], ALU.add, [ps[0], (bc, 0)], [v])
        actf(P, v[:, 0:6], v[:, 0:6], AF.Exp, [v], [v])
        actf(P, v[:, 0:6], v[:, 0:6], AF.Ln, [v], [v], bias=C.ones[:, 0:1])
        tt(P, "dve", v[:, 6:12], v[:, 0:6], bc.t[:, 1, 0:6], ALU.mult, [v, (bc, 1)], [v])
        mm(P, ps[0][:, 8:14], C.U[:], v[:, 6:12], [C.U, v], [ps[0]])
        mm(P, ps[0][:, 16:22], C.ones[:], v[:, 6:12], [C.ones, v], [ps[0]])
        P.dve(lambda e: e.tensor_copy(v[:, 12:18], ps[0][:, 8:14]), r=[ps[0]], w=[v])
        ts(P, "dve", v[:, 18:24], v[:, 12:18], -1.0, None, ALU.mult, None, [v], [v])
        P.dve(lambda e: e.tensor_copy(v[:, 24:30], ps[0][:, 16:22]), r=[ps[0]], w=[v])
        tt(P, "dve", v[:, 30:36], v[:, 24:30], v[:, 12:18], ALU.subtract, [v], [v])
        actf(P, v[:, 30:36], v[:, 30:36], AF.Exp, [v], [v])
        tt(P, "dve", v[:, 30:36], v[:, 30:36], v[:, 0:6], ALU.mult, [v], [v])
        actf(P, v[:, 36:42], v[:, 12:18], AF.Exp, [v], [v])
        actf(P, v[:, 42:48], v[:, 24:30], AF.Exp, [v], [v])
        for i in range(3):
            tr(P, C, ps[1][:, i * 128:(i + 1) * 128], F[i][:], 128, [F[i]], [ps[1]])
        P.act(lambda e: e.copy(Xtm[:], ps[1][:, 0:384]), r=[ps[1]], w=[Xtm])
        for h in range(6):
            hs = slice(h * 64, (h + 1) * 64)
            actf(P, xdt[:, hs], Xtm[:, hs], AF.Copy, [Xtm, v], [(xdt, h)], scale=v[:, h:h + 1])
            actf(P, xdd[:, hs], Xtm[:, hs], AF.Copy, [Xtm, v], [(xdd, h)], scale=v[:, 30 + h:31 + h])
        for g in range(2):
            tr(P, C, ps[2][:, g * 128:(g + 1) * 128], F[3 + g][:], 128, [F[3 + g]], [ps[2]])
        P.dve(lambda e: e.tensor_copy(Btm[:], ps[2][:, 0:256]), r=[ps[2]], w=[Btm])
        for g in range(2):
            mm(P, ps[3][:, g * 128:(g + 1) * 128], F[3 + g][:], F[5 + g][:], [F[3 + g], F[5 + g]], [ps[3]])
        for h in range(6):
            g = h // 3
            b = h % 2
            pr_ = ps[4 + b]
            mm(P, pr_[:, 0:128], v[:, 6 + h:7 + h].broadcast_to([128, 128]), C.U[:], [v, C.U], [pr_])
            stt(P, E[b][:], pr_[:, 0:128], v[:, 18 + h:19 + h], C.negU[:], ALU.add, ALU.add, [pr_, v, C.negU], [E[b]])
            actf(P, E[b][:], E[b][:], AF.Exp, [E[b]], [E[b]])
            tt(P, "dve", Mt[b][:], ps[3][:, g * 128:(g + 1) * 128], E[b][:], ALU.mult, [ps[3], E[b]], [Mt[b]])
            hs = slice(h * 64, (h + 1) * 64)
            mm(P, ps[6][:, hs], Mt[b][:], xdt[:, hs], [Mt[b], (xdt, h)], [ps[6]])
            mm(P, ps[7][:, hs], F[5 + g][:], S[:, h, :], [F[5 + g], (S, h)], [ps[7]])
        P.act(lambda e: e.copy(yd[:], ps[6][:, 0:384]), r=[ps[6]], w=[yd])
        for h in range(6):
            hs = slice(h * 64, (h + 1) * 64)
            stt(P, y[:, hs], ps[7][:, hs], v[:, 36 + h:37 + h], yd[:, hs], ALU.mult, ALU.add, [ps[7], v, yd], [(y, h)])
        for g in range(2):
            mm(P, ps[2][:, 256 + 0:256 + 192] if False else ps[0][:, 64 + g * 192:64 + (g + 1) * 192], Btm[:, g * 128:(g + 1) * 128], xdd[:, g * 192:(g + 1) * 192],
               [Btm] + [(xdd, h) for h in range(3 * g, 3 * g + 3)], [ps[0]])
        for h in range(6):
            stt(P, S[:, h, :], S[:, h, :], v[:, 42 + h:43 + h], ps[0][:, 64 + h * 64:64 + (h + 1) * 64], ALU.mult, ALU.add, [(S, h), v, ps[0]], [(S, h)])
        yk = [(y, h) for h in range(6)]
        tt(P, "pool", xdt[:], Xtm[:], bc.t[:, 2, :], ALU.mult, [Xtm, (bc, 2)] + [(xdt, h) for h in range(6)], [(xdt, h) for h in range(6)])
        tt(P, "pool", y[:], y[:], xdt[:], ALU.add, yk + [(xdt, h) for h in range(6)], yk)
        for i in range(3):
            tr(P, C, ps[1][:, i * 128:(i + 1) * 128], zT[i][:], 128, [zT[i]], [ps[1]])
        actf(P, zs[:], ps[1][:, 0:384], AF.Silu, [ps[1]], [zs])
        tt(P, "dve", y[:], y[:], zs[:], ALU.mult, yk + [zs], yk)
        for g in range(2):
            gs = slice(g * 192, (g + 1) * 192)
            actf(P, junk[:], y[:, gs], AF.Square, yk, [junk, (ssq, g)], accum_out=ssq[:, g:g + 1])
            ts(P, "dve", ssq[:, 2 + g:3 + g], ssq[:, g:g + 1], 1.0 / 192, EPS, ALU.mult, ALU.add, [(ssq, g)], [(ssq, g)])
            actf(P, ssq[:, 4 + g:5 + g], ssq[:, 2 + g:3 + g], AF.Sqrt, [(ssq, g)], [(ssq, g)])
            P.dve(lambda e, g=g: e.reciprocal(ssq[:, 6 + g:7 + g], ssq[:, 4 + g:5 + g]), r=[(ssq, g)], w=[(ssq, g)])
            stt(P, y[:, gs], y[:, gs], ssq[:, 6 + g:7 + g], bc.t[:, 3, gs], ALU.mult, ALU.mult, yk + [(ssq, g), (bc, 3)], yk)
        for i in range(3):
            tr(P, C, ps[2][:, i * 128:(i + 1) * 128], y[:, i * 128:(i + 1) * 128], 128, yk, [ps[2]])
        P.act(lambda e: e.copy(ob[:], ps[2].t[:, 0:384].rearrange("p (k t) -> p k t", k=3)), r=[ps[2]], w=[ob])
        P.dma(mix_d[256:640, cs].rearrange("(k p) t -> p k t", p=128), ob[:], r=[ob], w=[("mixssd", blk)])
    P.barrier()
    P.release(m0)


def mixer_gdn(P, C, SEQ, proj_d, mix_d, W):
    m0 = P.mark()
    ps = C.ps
    NB = SEQ // 128
    cw = P.sb("gdn_cw", [128, 9, 4], F32)
    P.dma(cw[:], W["gdn_cw"], w=[cw])
    bc = P.sb("gdn_bc", [128, 3, 384], F32)
    P.dma(bc.t[:, 0, 0:6], W["gdn_alog"], w=[(bc, 0)])
    P.dma(bc.t[:, 1, 0:6], W["gdn_dtb"], w=[(bc, 1)])
    P.dma(bc.t[:, 2, :], W["gdn_nw"], w=[(bc, 2)])
    actf(P, bc.t[:, 0, 0:6], bc.t[:, 0, 0:6], AF.Exp, [(bc, 0)], [(bc, 0)])
    ts(P, "dve", bc.t[:, 0, 0:6], bc.t[:, 0, 0:6], -1.0, None, ALU.mult, None, [(bc, 0)], [(bc, 0)])
    xin = [P.sb(f"gcv_in{i}", [128, 131], F32) for i in range(2)]
    acc = P.sb("gcv_acc", [128, 128], F32)
    F = [P.sb(f"gdnF{i}", [128, 128], F32) for i in range(9)]
    zT = [P.sb(f"gdnz{i}", [128, 128], F32) for i in range(3)]
    sm = P.sb("gdn_sm", [12, SEQ], F32)
    P.dma(sm[:], proj_d[3078:3090, :], w=[sm])
    S = [P.sb(f"gdnS{h}", [64, 64], F32) for h in range(6)]
    for h in range(6):
        P.dve(lambda e, h=h: e.memset(S[h][:], 0.0), w=[S[h]])
    v = P.sb("gdnv", [128, 96], F32)
    big = {n: P.sb("gdn_" + n, [128, 384], F32) for n in ("q", "k", "v", "sq", "qn", "kn", "qd", "kbg", "kend", "vb", "o", "zs")}
    v3 = lambda t: t.t[:, :].rearrange("p (h d) -> p h d", h=6)
    hb = lambda c0: v[:, c0:c0 + 6].unsqueeze(2).broadcast_to([128, 6, 64])
    knT = [P.sb(f"knT{h}", [64, 128], F32) for h in range(6)]
    qnT = [P.sb(f"qnT{h}", [64, 128], F32) for h in range(6)]
    qdT = [P.sb(f"qdT{h}", [64, 128], F32) for h in range(6)]
    E = [P.sb(f"gE{h}", [128, 128], F32) for h in range(6)]
    DL = [P.sb(f"gDL{h}", [128, 128], F32) for h in range(6)]
    qkT = [P.sb(f"gqkT{h}", [128, 128], F32) for h in range(6)]
    Pk = [[P.sb(f"gP{h}_{i}", [128, 128], F32) for i in range(2)] for h in range(6)]
    PkT = [[P.sb(f"gPT{h}_{i}", [128, 128], F32) for i in range(2)] for h in range(6)]
    YT = [P.sb(f"gYT{h}", [128, 128], F32) for h in range(6)]
    WT = [P.sb(f"gWT{h}", [64, 128], F32) for h in range(6)]
    Us = [P.sb(f"gU{h}", [128, 64], F32) for h in range(6)]
    vn = [P.sb(f"gvn{h}", [128, 64], F32) for h in range(6)]
    oq = [P.sb(f"goq{h}", [128, 64], F32) for h in range(6)]
    ob = P.sb("gob", [128, 3, 128], BF16)
    allv = [v]
    for blk in range(NB):
        t0 = blk * 128
        cs = slice(t0, t0 + 128)
        conv_blk(P, SEQ, proj_d, 1536, 9, cw, None, F, xin, acc, t0)
        for i in range(3):
            P.dma(zT[i][:], proj_d[2688 + i * 128:2688 + (i + 1) * 128, cs], w=[zT[i]])
        tr(P, C, ps[0][:, 0:12], sm[0:12, cs], 12, [sm], [ps[0]])
        actf(P, v[:, 0:6], ps[0][:, 0:6], AF.Sigmoid, [ps[0]], [v])
        tt(P, "dve", v[:, 6:12], ps[0][:, 6:12], bc.t[:, 1, 0:6], ALU.add, [ps[0], (bc, 1)], [v])
        actf(P, v[:, 6:12], v[:, 6:12], AF.Exp, [v], [v])
        actf(P, v[:, 6:12], v[:, 6:12], AF.Ln, [v], [v], bias=C.ones[:, 0:1])
        tt(P, "dve", v[:, 6:12], v[:, 6:12], bc.t[:, 0, 0:6], ALU.mult, [v, (bc, 0)], [v])
        mm(P, ps[0][:, 16:22], C.U2[:], v[:, 6:12], [C.U2, v], [ps[0]])
        mm(P, ps[0][:, 24:30], C.BD[:], v[:, 6:12], [C.BD, v], [ps[0]])
        mm(P, ps[0][:, 32:38], C.SEL0[:], v[:, 6:12], [C.SEL0, v], [ps[0]])
        mm(P, ps[0][:, 40:46], C.SEL1[:], v[:, 6:12], [C.SEL1, v], [ps[0]])
        P.dve(lambda e: e.tensor_copy(v[:, 12:18], ps[0][:, 16:22]), r=[ps[0]], w=[v])
        ts(P, "dve", v[:, 18:24], v[:, 12:18], -1.0, None, ALU.mult, None, [v], [v])
        tt(P, "dve", v[:, 30:36], ps[0][:, 24:30], v[:, 12:18], ALU.subtract, [ps[0], v], [v])
        actf(P, v[:, 30:36], v[:, 30:36], AF.Exp, [v], [v])
        actf(P, v[:, 36:42], v[:, 12:18], AF.Exp, [v], [v])
        actf(P, v[:, 42:48], ps[0][:, 32:38], AF.Exp, [ps[0]], [v])
        actf(P, v[:, 48:54], ps[0][:, 40:46], AF.Exp, [ps[0]], [v])
        ts(P, "dve", v[:, 78:84], v[:, 0:6], -1.0, None, ALU.mult, None, [v], [v])
        for n_, base, bank in (("q", 0, 1), ("k", 3, 2), ("v", 6, 3)):
            for i in range(3):
                tr(P, C, ps[bank][:, i * 128:(i + 1) * 128], F[base + i][:], 128, [F[base + i]], [ps[bank]])
            P.act(lambda e, n_=n_, bank=bank: e.copy(big[n_][:], ps[bank][:, 0:384]), r=[ps[bank]], w=[big[n_]])
        for n_, c_ss, c_r, sc in (("q", 54, 60, 0.125), ("k", 66, 72, 1.0)):
            tt(P, "pool", big["sq"][:], big[n_][:], big[n_][:], ALU.mult, [big[n_]], [big["sq"]])
            P.dve(lambda e, c_ss=c_ss: e.tensor_reduce(v[:, c_ss:c_ss + 6], v3(big["sq"]), AX.X, ALU.add), r=[big["sq"]], w=[v])
            ts(P, "dve", v[:, c_ss:c_ss + 6], v[:, c_ss:c_ss + 6], EPS, None, ALU.add, None, [v], [v])
            actf(P, v[:, c_ss:c_ss + 6], v[:, c_ss:c_ss + 6], AF.Sqrt, [v], [v])
            P.dve(lambda e, c_ss=c_ss, c_r=c_r: e.reciprocal(v[:, c_r:c_r + 6], v[:, c_ss:c_ss + 6]), r=[v], w=[v])
            if sc != 1.0:
                ts(P, "dve", v[:, c_r:c_r + 6], v[:, c_r:c_r + 6], sc, None, ALU.mult, None, [v], [v])
        tt(P, "dve", v3(big["qn"]), v3(big["q"]), hb(60), ALU.mult, [big["q"], v], [big["qn"]])
        tt(P, "dve", v3(big["kn"]), v3(big["k"]), hb(72), ALU.mult, [big["k"], v], [big["kn"]])
        tt(P, "pool", v3(big["qd"]), v3(big["qn"]), hb(36), ALU.mult, [big["qn"], v], [big["qd"]])
        tt(P, "pool", v3(big["kend"]), v3(big["kn"]), hb(30), ALU.mult, [big["kn"], v], [big["kend"]])
        tt(P, "dve", v3(big["kbg"]), v3(big["kn"]), hb(0), ALU.mult, [big["kn"], v], [big["kbg"]])
        tt(P, "dve", v3(big["kbg"]), v3(big["kbg"]), hb(36), ALU.mult, [big["kbg"], v], [big["kbg"]])
        tt(P, "pool", v3(big["vb"]), v3(big["v"]), hb(0), ALU.mult, [big["v"], v], [big["vb"]])
        for h in range(6):
            hs = slice(h * 64, (h + 1) * 64)
            for src, dstl, col in ((big["kn"], knT, 0), (big["qn"], qnT, 128), (big["qd"], qdT, 256)):
                tr(P, C, ps[1][0:64, col:col + 128], src[:, hs], 128, [src], [ps[1]])
            P.act(lambda e, h=h: e.copy(knT[h][:], ps[1][0:64, 0:128]), r=[ps[1]], w=[knT[h]])
            P.dve(lambda e, h=h: e.tensor_copy(qnT[h][:], ps[1][0:64, 128:256]), r=[ps[1]], w=[qnT[h]])
            P.act(lambda e, h=h: e.copy(qdT[h][:], ps[1][0:64, 256:384]), r=[ps[1]], w=[qdT[h]])
        H6 = range(6)
        hsl = [slice(h * 64, (h + 1) * 64) for h in H6]
        bk = lambda stage, h: ps[(2 * stage + (h % 2)) % 8]
        for h in H6:
            pb = bk(1, h)
            mm(P, pb[:, 0:128], v[:, 6 + h:7 + h].broadcast_to([128, 128]), C.U2[:], [v, C.U2], [pb])
            stt(P, E[h][:], pb[:, 0:128], v[:, 18 + h:19 + h], C.negU2[:], ALU.add, ALU.add, [pb, v, C.negU2], [E[h]])
            actf(P, E[h][:], E[h][:], AF.Exp, [E[h]], [E[h]])
            stt(P, DL[h][:], pb[:, 0:128], -1.0, C.negLs2[:], ALU.mult, ALU.add, [pb, C.negLs2], [DL[h]])
            actf(P, DL[h][:], DL[h][:], AF.Exp, [DL[h], v], [DL[h]], bias=v[:, 12 + h:13 + h])
        for h in H6:
            pb = bk(2, h)
            mm(P, pb[:, 0:128], knT[h][:], qnT[h][:], [knT[h], qnT[h]], [pb])
            mm(P, pb[:, 128:256], knT[h][:], knT[h][:], [knT[h]], [pb])
            tt(P, "dve", qkT[h][:], pb[:, 0:128], E[h][:], ALU.mult, [pb, E[h]], [qkT[h]])
            stt(P, Pk[h][0][:], pb[:, 128:256], v[:, 78 + h:79 + h], DL[h][:], ALU.mult, ALU.mult, [pb, v, DL[h]], [Pk[h][0]])
        for h in H6:
            pb = bk(3, h)
            tr(P, C, pb[:, 0:128], Pk[h][0][:], 128, [Pk[h][0]], [pb])
            P.act(lambda e, h=h, pb=pb: e.copy(PkT[h][0][:], pb[:, 0:128]), r=[pb], w=[PkT[h][0]])
            tt(P, "dve", YT[h][:], pb[:, 0:128], C.ID[:], ALU.add, [pb, C.ID], [YT[h]])
        cur = 0
        for lvl in range(1, 6):
            nxt = 1 - cur
            for h in H6:
                pb = bk(4 + 2 * lvl, h)
                mm(P, pb[:, 0:128], PkT[h][cur][:], Pk[h][cur][:], [PkT[h][cur], Pk[h][cur]], [pb])
                if lvl < 5:
                    mm(P, pb[:, 128:256], Pk[h][cur][:], PkT[h][cur][:], [PkT[h][cur], Pk[h][cur]], [pb])
                P.act(lambda e, h=h, pb=pb, nxt=nxt: e.copy(Pk[h][nxt][:], pb[:, 0:128]), r=[pb], w=[Pk[h][nxt]])
                if lvl < 5:
                    P.dve(lambda e, h=h, pb=pb, nxt=nxt: e.tensor_copy(PkT[h][nxt][:], pb[:, 128:256]), r=[pb], w=[PkT[h][nxt]])
            for h in H6:
                pb = bk(5 + 2 * lvl, h)
                mm(P, pb[:, 0:128], Pk[h][nxt][:], YT[h][:], [Pk[h][nxt], YT[h]], [pb])
                tt(P, "dve", YT[h][:], YT[h][:], pb[:, 0:128], ALU.add, [YT[h], pb], [YT[h]])
            cur = nxt
        for h in H6:
            pb = bk(0, h)
            mm(P, pb[:, 0:64], YT[h][:], big["vb"][:, hsl[h]], [YT[h], big["vb"]], [pb])
            mm(P, pb[0:64, 128:256], big["kbg"][:, hsl[h]], YT[h][:], [YT[h], big["kbg"]], [pb])
            P.act(lambda e, h=h, pb=pb: e.copy(Us[h][:], pb[:, 0:64]), r=[pb], w=[Us[h]])
            P.dve(lambda e, h=h, pb=pb: e.tensor_copy(WT[h][:], pb[0:64, 128:256]), r=[pb], w=[WT[h]])
        for half in range(2):
            r0 = 64 * half
            rs = slice(r0, r0 + 64)
            for h in H6:
                pb = bk(1, h)
                mm(P, pb[:, 0:64], WT[h][:], S[h][:], [WT[h], S[h]], [pb])
                mm(P, pb[:, 64:128], qdT[h][:], S[h][:], [qdT[h], S[h]], [pb])
                tt(P, "dve", vn[h][rs, :], Us[h][rs, :], pb[rs, 0:64], ALU.subtract, [Us[h], pb], [(vn[h], half)])
                P.act(lambda e, h=h, pb=pb, rs=rs: e.copy(oq[h][rs, :], pb[rs, 64:128]), r=[pb], w=[(oq[h], half)])
            for h in H6:
                pb = bk(2, h)
                mm(P, pb[0:64, 0:64], big["kend"][rs, hsl[h]], vn[h][rs, :], [big["kend"], (vn[h], half)], [pb])
                stt(P, S[h][:], S[h][:], v[0:64, 42 + 6 * half + h:43 + 6 * half + h], pb[0:64, 0:64], ALU.mult, ALU.add, [S[h], v, pb], [S[h]])
        for h in H6:
            pb = bk(3, h)
            mm(P, pb[:, 0:64], qkT[h][:], vn[h][:], [qkT[h], (vn[h], 0), (vn[h], 1)], [pb])
            tt(P, "dve", big["o"][:, hsl[h]], oq[h][:], pb[:, 0:64], ALU.add, [(oq[h], 0), (oq[h], 1), pb], [(big["o"], h, 0), (big["o"], h, 1)])
        ok = [(big["o"], h, k) for h in range(6) for k in range(2)]
        tt(P, "pool", big["sq"][:], big["o"][:], big["o"][:], ALU.mult, ok, [big["sq"]])
        P.dve(lambda e: e.tensor_reduce(v[:, 84:90], v3(big["sq"]), AX.X, ALU.add), r=[big["sq"]], w=[v])
        ts(P, "dve", v[:, 84:90], v[:, 84:90], 1.0 / 64, EPS, ALU.mult, ALU.add, [v], [v])
        actf(P, v[:, 84:90], v[:, 84:90], AF.Sqrt, [v], [v])
        P.dve(lambda e: e.reciprocal(v[:, 90:96], v[:, 84:90]), r=[v], w=[v])
        tt(P, "dve", v3(big["qn"]), v3(big["o"]), hb(90), ALU.mult, ok + [v, big["qn"]], [big["qn"]])
        tt(P, "pool", big["qn"][:], big["qn"][:], bc.t[:, 2, :], ALU.mult, [big["qn"], (bc, 2)], [big["qn"]])
        for i in range(3):
            tr(P, C, ps[1][:, i * 128:(i + 1) * 128], zT[i][:], 128, [zT[i]], [ps[1]])
        actf(P, big["zs"][:], ps[1][:, 0:384], AF.Silu, [ps[1]], [big["zs"]])
        tt(P, "dve", big["qn"][:], big["qn"][:], big["zs"][:], ALU.mult, [big["qn"], big["zs"]], [big["qn"]])
        for i in range(3):
            tr(P, C, ps[2][:, i * 128:(i + 1) * 128], big["qn"][:, i * 128:(i + 1) * 128], 128, [big["qn"]], [ps[2]])
        P.act(lambda e: e.copy(ob[:], ps[2].t[:, 0:384].rearrange("p (k t) -> p k t", k=3)), r=[ps[2]], w=[ob])
        P.dma(mix_d[640:1024, cs].rearrange("(k p) t -> p k t", p=128), ob[:], r=[ob], w=[("mixgdn", blk)])
    P.barrier()
    P.release(m0)


SEQ_FULL = 4096
N_CORES = 8
_LKEYS = None


def build_program(SEQ, layer_shapes):
    nc = bass.Bass("TRN2", target_bir_lowering=False)
    x_d = nc.dram_tensor("x", [SEQ, D], F32, kind="ExternalInput").ap()
    Ws = []
    for l in range(2):
        Ws.append({k: nc.dram_tensor(f"L{l}_{k}", list(shp), F32, kind="ExternalInput").ap() for k, shp in layer_shapes.items()})
    ffg = nc.dram_tensor("ff_g", [1, D, DFF], F32, kind="ExternalInput").ap()
    ffu = nc.dram_tensor("ff_u", [1, D, DFF], F32, kind="ExternalInput").ap()
    ffd = nc.dram_tensor("ff_d", [1, DFF, D], F32, kind="ExternalInput").ap()
    mog = nc.dram_tensor("moe_g", [NE, D, DFF], F32, kind="ExternalInput").ap()
    mou = nc.dram_tensor("moe_u", [NE, D, DFF], F32, kind="ExternalInput").ap()
    mod = nc.dram_tensor("moe_d", [NE, DFF, D], F32, kind="ExternalInput").ap()
    mor = nc.dram_tensor("moe_r", [D, NE], F32, kind="ExternalInput").ap()
    nfin = nc.dram_tensor("nfin", [128, D], F32, kind="ExternalInput").ap()
    out_d = nc.dram_tensor("out", [SEQ, D], F32, kind="ExternalOutput").ap()
    proj_d = nc.dram_tensor("proj_s", [NPROJ, SEQ], F32).ap()
    mix_d = nc.dram_tensor("mix_s", [D, SEQ], BF16).ap()
    xres = nc.dram_tensor("xres_s", [SEQ, D], F32).ap()
    P = Prog(nc)
    C = Ctx()
    setup_common(P, C)
    make_masks(P, C)
    P.barrier()
    for l in range(2):
        W = Ws[l]
        src = x_d if l == 0 else xres
        phase_inproj(P, C, SEQ, src, W["nmix"], W["win"], proj_d)
        mixer_s5(P, C, SEQ, proj_d, mix_d, W)
        mixer_ssd(P, C, SEQ, proj_d, mix_d, W)
        mixer_gdn(P, C, SEQ, proj_d, mix_d, W)
        phase_outproj(P, C, SEQ, src, xres, mix_d, W["wout"])
        if l == 0:
            phase_ffn(P, C, SEQ, xres, xres, W["nffn"], ffg, ffu, ffd, 1)
        else:
            phase_ffn(P, C, SEQ, xres, out_d, W["nffn"], mog, mou, mod, NE, wr_d=mor, nfin_d=nfin)
    finals = [o for e in ENGS for o in P.ops[e] if o.is_dma]
    P.emit(finals[-64:])
    return nc, P


def kernel(**inp):
    inp = {k: np.asarray(v) for k, v in inp.items()}
    x = np.ascontiguousarray(inp["x"], dtype=np.float32)
    B, SEQ, _ = x.shape
    layers = [host_layer_inputs(inp, l) for l in range(2)]
    shapes = {k: v.shape for k, v in layers[0].items()}
    nc, P = build_program(SEQ, shapes)
    f = lambda a: np.ascontiguousarray(np.asarray(a, np.float32))
    common = {}
    for l in range(2):
        for k, v in layers[l].items():
            common[f"L{l}_{k}"] = np.ascontiguousarray(v, dtype=np.float32)
    common["ff_g"] = f(inp["ff_w_gate"])
    common["ff_u"] = f(inp["ff_w_up"])
    common["ff_d"] = f(inp["ff_w_down"])
    common["moe_g"] = f(inp["moe_w_gate"][0])
    common["moe_u"] = f(inp["moe_w_up"][0])
    common["moe_d"] = f(inp["moe_w_down"][0])
    common["moe_r"] = f(inp["moe_router"][0])
    common["nfin"] = rep128(inp["norm_final"])
    in_maps = []
    for c in range(B):
        m = dict(common)
        m["x"] = np.ascontiguousarray(x[c])
        in_maps.append(m)
    res = run_bass_kernel_spmd(nc, in_maps, core_ids=list(range(B)))
    return np.stack([np.asarray(r["out"], dtype=np.float32) for r in res.results], axis=0)
```

```python
import contextlib
import numpy as np
import concourse.bass as bass
import concourse.mybir as mybir
from concourse.bass_utils import run_bass_kernel_spmd

F32 = mybir.dt.float32
BF16 = mybir.dt.bfloat16
I32 = mybir.dt.int32
ALU = mybir.AluOpType
AF = mybir.ActivationFunctionType
AX = mybir.AxisListType

ENGS = ("pe", "act", "dve", "pool", "sp")
NDMASEM = 8


class Op:
    __slots__ = ("eng", "fn", "deps", "sig", "is_dma", "sem", "semval", "barriered")

    def __init__(self, eng, fn, is_dma):
        self.eng = eng
        self.fn = fn
        self.deps = []
        self.sig = None
        self.is_dma = is_dma
        self.sem = None
        self.semval = None
        self.barriered = False


class Tile:
    _n = 0

    def __init__(self, t, name, psum=False):
        self.t = t
        self.name = name
        self.psum = psum
        Tile._n += 1
        self.id = Tile._n

    def __getitem__(self, k):
        return self.t[k]

    def __hash__(self):
        return self.id

    def __eq__(self, o):
        return self is o


class Prog:
    def __init__(self, nc, same_engine_sync=True):
        self.nc = nc
        self.ops = {e: [] for e in ENGS}
        self.lastw = {}
        self.readers = {}
        self.same_engine_sync = same_engine_sync
        self.ndma = {e: 0 for e in ENGS}
        self.dma_last = {}
        self.sb_off = 16640
        self.sb_max = 0
        self.nalloc = 0
        self.cur_stream = None

    def sb(self, name, shape, dtype, align=64):
        nb = int(np.prod(shape[1:])) * mybir.dt.size(dtype)
        off = (self.sb_off + align - 1) // align * align
        self.nalloc += 1
        t = self.nc.alloc_sbuf_tensor_at(f"{name}_{self.nalloc}", list(shape), dtype, offset=off)
        self.sb_off = off + nb
        self.sb_max = max(self.sb_max, self.sb_off)
        assert self.sb_off <= 229376, f"SBUF overflow {self.sb_off} at {name}"
        return Tile(t, name)

    def mark(self):
        return self.sb_off

    def release(self, m):
        self.sb_off = m

    def op(self, eng, fn, r=(), w=(), is_dma=False):
        o = Op(eng, fn, is_dma)
        if any(isinstance(k, Tile) and k.psum for k in r):
            w = list(w) + [k for k in r if isinstance(k, Tile) and k.psum]
            r = [k for k in r if not (isinstance(k, Tile) and k.psum)]
        deps = {}
        for k in r:
            lw = self.lastw.get(k)
            if lw is not None:
                deps[id(lw)] = lw
        for k in w:
            lw = self.lastw.get(k)
            if lw is not None:
                deps[id(lw)] = lw
            for rd in self.readers.get(k, ()):
                deps[id(rd)] = rd
        if is_dma:
            self.ndma[eng] += 1
        for d in deps.values():
            if d is o:
                continue
            if (not d.is_dma) and d.eng == eng:
                if eng == "pe" or not self.same_engine_sync:
                    continue
            o.deps.append(d)
        for k in r:
            lst = self.readers.setdefault(k, [])
            if not is_dma:
                lst[:] = [x for x in lst if x.is_dma or x.eng != eng]
            lst.append(o)
        for k in w:
            self.lastw[k] = o
            self.readers[k] = []
        if self.cur_stream is not None:
            self.cur_stream.append(o)
        else:
            self.ops[eng].append(o)
        return o

    def stream_begin(self):
        assert self.cur_stream is None
        self.cur_stream = []

    def stream_end(self):
        st, self.cur_stream = self.cur_stream, None
        return st

    def merge(self, streams):
        pos = [0] * len(streams)
        tot = [max(1, len(st)) for st in streams]
        while True:
            best = None
            for i, st in enumerate(streams):
                if pos[i] < len(st):
                    f = pos[i] / tot[i]
                    if best is None or f < best[0]:
                        best = (f, i)
            if best is None:
                break
            i = best[1]
            o = streams[i][pos[i]]
            pos[i] += 1
            self.ops[o.eng].append(o)

    def pe(self, fn, r=(), w=()):
        return self.op("pe", fn, r, w)

    def act(self, fn, r=(), w=()):
        return self.op("act", fn, r, w)

    def dve(self, fn, r=(), w=()):
        return self.op("dve", fn, r, w)

    def pool(self, fn, r=(), w=()):
        return self.op("pool", fn, r, w)

    def dma(self, out, in_, r=(), w=(), eng="sp", **kw):
        return self.op(eng, lambda e: e.dma_start(out, in_, **kw), r, w, is_dma=True)

    def barrier(self):
        tails = []
        for e in ENGS:
            if e == "sp":
                continue
            for o in reversed(self.ops[e]):
                if o.fn is not None and not o.is_dma:
                    tails.append(o)
                    break
        dmas = [o for e in ENGS for o in self.ops[e] if o.is_dma and not o.barriered]
        for o in dmas:
            o.barriered = True
        for e in ENGS:
            b = Op(e, None, False)
            b.deps = [t for t in tails if t.eng != e] + list(dmas)
            self.ops[e].append(b)
        self.lastw.clear()
        self.readers.clear()

    def emit(self, final_waits=()):
        nc = self.nc
        fin = Op("sp", None, False)
        fin.deps = list(final_waits)
        self.ops["sp"].append(fin)
        for e in ENGS:
            n = 0
            last = {}
            for o in self.ops[e]:
                if o.is_dma:
                    slot = n % NDMASEM
                    n += 1
                    prev = last.get(slot)
                    if prev is not None and all(d is not prev for d in o.deps):
                        o.deps.append(prev)
                    last[slot] = o
                    o.sem = (e, slot)
        for e in ENGS:
            for o in self.ops[e]:
                for d in o.deps:
                    d.sig = True
        esem = {}
        dsem = {}
        with contextlib.ExitStack() as st:
            for e in ENGS:
                esem[e] = st.enter_context(nc.semaphore(f"s_{e}"))
            for e in ENGS:
                if self.ndma[e]:
                    for s in range(min(NDMASEM, self.ndma[e])):
                        dsem[(e, s)] = st.enter_context(nc.semaphore(f"d_{e}{s}"))
            dcount = {}
            for e in ENGS:
                c = 0
                for o in self.ops[e]:
                    if o.is_dma:
                        n = dcount.get(o.sem, 0) + 16
                        dcount[o.sem] = n
                        o.semval = n
                        o.sig = True
                    elif o.sig:
                        c += 1
                        o.semval = c
                        o.sem = e
                assert c < 60000, (e, c)
            maxd = max(dcount.values()) if dcount else 0
            assert maxd < 60000, maxd
            self.stats = {e: len(self.ops[e]) for e in ENGS}
            block = st.enter_context(nc.Block())
            engmap = {"pe": block.tensor, "act": block.scalar, "dve": block.vector,
                      "pool": block.gpsimd, "sp": block.sync}
            nw = [0]
            for e in ENGS:
                ops = self.ops[e]
                if not ops:
                    continue

                def body(eng, ops=ops, e=e):
                    seen = {}
                    for o in ops:
                        need = {}
                        for d in o.deps:
                            if d.semval is None:
                                continue
                            if seen.get(d.sem, 0) >= d.semval:
                                continue
                            if need.get(d.sem, 0) < d.semval:
                                need[d.sem] = d.semval
                        for s, v in need.items():
                            sh = esem[s] if isinstance(s, str) else dsem[s]
                            eng.wait_ge(sh, v)
                            seen[s] = v
                            nw[0] += 1
                        if o.fn is None:
                            continue
                        ins = o.fn(eng)
                        if o.is_dma:
                            ins.then_inc(dsem[o.sem], 16)
                        elif o.sig:
                            ins.then_inc(esem[e], 1)

                engmap[e](body)
            self.stats["waits"] = nw[0]
        return nc


D = 1024
DFF = 3584
NE = 8
EPS = 1e-6
NPROJ = 3200
KT = 8


class Ctx:
    pass


def setup_common(P, C):
    nc = P.nc
    C.ps = [Tile(nc.alloc_psum_tensor(f"psb{i}", [128, 512], F32), f"ps{i}", psum=True) for i in range(8)]
    C.ID = P.sb("ID", [128, 128], F32)
    C.IDb = P.sb("IDb", [128, 128], BF16)
    P.pool(lambda e: e.memset(C.ID[:], 1.0), w=[C.ID])
    P.pool(lambda e: e.affine_select(C.ID[:], C.ID[:], [[1, 128]], ALU.is_equal, 0.0, base=0,
                                     channel_multiplier=-1), r=[C.ID], w=[C.ID])
    P.dve(lambda e: e.tensor_copy(C.IDb[:], C.ID[:]), r=[C.ID], w=[C.IDb])
    C.ones = P.sb("ones", [128, 128], F32)
    P.pool(lambda e: e.memset(C.ones[:], 1.0), w=[C.ones])


def norm_tile(P, C, xt, nwbc, xn, ss, junk):
    P.act(lambda e: e.activation(junk[:], xt[:], AF.Square, accum_out=ss[:, 0:1]), r=[xt], w=[junk, ss])
    P.dve(lambda e: e.tensor_scalar(ss[:, 1:2], ss[:, 0:1], 1.0 / D, EPS, ALU.mult, ALU.add), r=[ss], w=[ss])
    P.act(lambda e: e.activation(ss[:, 2:3], ss[:, 1:2], AF.Sqrt), r=[ss], w=[ss])
    P.dve(lambda e: e.reciprocal(ss[:, 3:4], ss[:, 2:3]), r=[ss], w=[ss])
    P.dve(lambda e: e.scalar_tensor_tensor(xn[:], xt[:], ss[:, 3:4], nwbc[:], ALU.mult, ALU.mult),
          r=[xt, ss, nwbc], w=[xn])


def transpose_to_T(P, C, xn, banks, hnT, col0, hn32=None):
    for half in range(2):
        bk = banks[half]
        for j in range(4):
            k = half * 4 + j
            P.pe(lambda e, bk=bk, j=j, k=k: e.transpose(bk[:, j * 128:(j + 1) * 128], xn[:, k * 128:(k + 1) * 128], C.ID[:]),
                 r=[xn, C.ID], w=[bk])
        src = bk.t[:, :].rearrange("p (k t) -> p k t", k=4)
        dst = hnT.t[:, half * 4:half * 4 + 4, col0:col0 + 128]
        if half == 0:
            P.act(lambda e, dst=dst, src=src: e.copy(dst, src), r=[bk], w=[(hnT, col0, 0)])
        else:
            P.dve(lambda e, dst=dst, src=src: e.tensor_copy(dst, src), r=[bk], w=[(hnT, col0, 1)])
        if hn32 is not None:
            d32 = hn32.t[:, half * 4:half * 4 + 4, :]
            if half == 0:
                P.dve(lambda e, d32=d32, src=src: e.tensor_copy(d32, src), r=[bk], w=[(hn32, half)])
            else:
                P.act(lambda e, d32=d32, src=src: e.copy(d32, src), r=[bk], w=[(hn32, half)])


def phase_ffn(P, C, SEQ, xsrc, xdst, nw_d, wg_d, wu_d, wd_d, n_exp, wr_d=None, nfin_d=None, T=1024):
    m0 = P.mark()
    T = min(T, SEQ)
    NTB = T // 512
    moe = wr_d is not None
    FG = 512
    NFG = DFF // FG
    ps = C.ps
    nwbc = P.sb("nwbc", [128, D], F32)
    P.dma(nwbc[:], nw_d, w=[nwbc])
    if nfin_d is not None:
        nfbc = P.sb("nfbc", [128, D], F32)
        P.dma(nfbc[:], nfin_d, w=[nfbc])
    hnT = P.sb("hnT", [128, KT, T], BF16)
    acc = P.sb("acc", [128, KT, T], F32)
    hT = [P.sb(f"hT{i}", [128, 4, T], BF16) for i in range(2)]
    wg = [P.sb(f"wg{i}", [128, KT, FG], BF16) for i in range(2)]
    wu = [P.sb(f"wu{i}", [128, KT, FG], BF16) for i in range(2)]
    wd = [P.sb(f"wd{i}", [128, 4, D], BF16) for i in range(2)]
    sg = [P.sb(f"sg{i}", [128, 512], BF16) for i in range(2)]
    xt = [P.sb(f"xt{i}", [128, D], F32) for i in range(2)]
    xn = [P.sb(f"xn{i}", [128, D], F32) for i in range(2)]
    junk = P.sb("junk", [128, D], F32)
    ssb = [P.sb(f"ss{i}", [128, 8], F32) for i in range(2)]
    if moe:
        wr = P.sb("wr", [128, KT, NE], F32)
        P.dma(wr[:], wr_d.rearrange("(k p) e -> p k e", p=128), w=[wr])
        hn32 = [P.sb(f"hn32{i}", [128, KT, 128], F32) for i in range(2)]
        cbc = P.sb("cbc", [128, NE, T], BF16)
        rt = [P.sb(f"rt{i}", [128, 64], F32) for i in range(2)]
    nsb = SEQ // T
    wcount = 0
    for sbi in range(nsb):
        t0 = sbi * T
        for tt in range(T // 128):
            b = tt % 2
            P.dma(xt[b][:], xsrc[t0 + tt * 128:t0 + (tt + 1) * 128, :], w=[xt[b]])
            norm_tile(P, C, xt[b], nwbc, xn[b], ssb[b], junk)
            banks = (ps[4 + 2 * b], ps[5 + 2 * b])
            transpose_to_T(P, C, xn[b], banks, hnT, tt * 128, hn32[b] if moe else None)
            if moe:
                lg = ps[0] if b == 0 else ps[1]
                R = rt[b]
                for k in range(KT):
                    P.pe(lambda e, k=k, lg=lg, b=b: e.matmul(lg[:, 0:NE], hn32[b][:, k, :], wr[:, k, :], start=(k == 0), stop=(k == KT - 1)),
                         r=[(hn32[b], k // 4), wr], w=[lg])
                P.dve(lambda e, R=R, lg=lg: e.tensor_copy(R[:, 0:8], lg[:, 0:8]), r=[lg], w=[R])
                P.dve(lambda e, R=R: e.tensor_reduce(R[:, 8:9], R[:, 0:8], AX.X, ALU.max), r=[R], w=[R])
                P.dve(lambda e, R=R: e.tensor_scalar(R[:, 9:17], R[:, 0:8], R[:, 8:9], None, ALU.is_equal), r=[R], w=[R])
                P.dve(lambda e, R=R: e.scalar_tensor_tensor(R[:, 17:25], R[:, 9:17], -1e30, R[:, 0:8], ALU.mult, ALU.add), r=[R], w=[R])
                P.dve(lambda e, R=R: e.tensor_reduce(R[:, 25:26], R[:, 17:25], AX.X, ALU.max), r=[R], w=[R])
                P.dve(lambda e, R=R: e.tensor_scalar(R[:, 26:34], R[:, 17:25], R[:, 25:26], None, ALU.is_equal), r=[R], w=[R])
                P.dve(lambda e, R=R: e.tensor_tensor(R[:, 34:35], R[:, 25:26], R[:, 8:9], ALU.subtract), r=[R], w=[R])
                P.act(lambda e, R=R: e.activation(R[:, 35:36], R[:, 34:35], AF.Exp), r=[R], w=[R])
                P.dve(lambda e, R=R: e.tensor_scalar(R[:, 36:37], R[:, 35:36], 1.0, None, ALU.add), r=[R], w=[R])
                P.dve(lambda e, R=R: e.reciprocal(R[:, 37:38], R[:, 36:37]), r=[R], w=[R])
                P.dve(lambda e, R=R: e.tensor_tensor(R[:, 38:39], R[:, 35:36], R[:, 37:38], ALU.mult), r=[R], w=[R])
                P.dve(lambda e, R=R: e.tensor_scalar(R[:, 40:48], R[:, 9:17], R[:, 37:38], None, ALU.mult), r=[R], w=[R])
                P.dve(lambda e, R=R: e.scalar_tensor_tensor(R[:, 40:48], R[:, 26:34], R[:, 38:39], R[:, 40:48], ALU.mult, ALU.add), r=[R], w=[R])
                bb = ps[2] if b == 0 else ps[3]
                for half in range(2):
                    for j in range(4):
                        ex = half * 4 + j
                        P.pe(lambda e, ex=ex, j=j, R=R, bb=bb: e.matmul(bb[:, j * 128:(j + 1) * 128], R[:, 40 + ex:41 + ex].broadcast_to([128, 128]), C.ID[:], start=True, stop=True),
                             r=[R, C.ID], w=[bb])
                    src = bb.t[:, :].rearrange("p (k t) -> p k t", k=4)
                    dst = cbc.t[:, half * 4:half * 4 + 4, tt * 128:(tt + 1) * 128]
                    P.act(lambda e, dst=dst, src=src: e.copy(dst, src), r=[bb], w=[(cbc, tt)])
        nfg_total = n_exp * NFG

        def load_w(g, wb):
            ex, fg = divmod(g, NFG)
            f0 = fg * FG
            P.dma(wg[wb][:], wg_d[ex, :, f0:f0 + FG].rearrange("(k p) f -> p k f", p=128), w=[wg[wb]], eng="pool")
            P.dma(wu[wb][:], wu_d[ex, :, f0:f0 + FG].rearrange("(k p) f -> p k f", p=128), w=[wu[wb]], eng="pool")
            P.dma(wd[wb][:], wd_d[ex, f0:f0 + FG, :].rearrange("(c p) d -> p c d", p=128), w=[wd[wb]], eng="pool")
        for ex in range(n_exp):
            for fg in range(NFG):
                gi = ex * NFG + fg
                wb = wcount % 2
                wcount += 1
                f0 = fg * FG
                if gi == 0:
                    load_w(0, wb)
                if gi + 1 < nfg_total:
                    load_w(gi + 1, 1 - wb)
                hb = hT[wb]
                for fc in range(4):
                    for tb in range(NTB):
                        pg = ps[0 + (fc * NTB + tb) % 2]
                        pu = ps[2 + (fc * NTB + tb) % 2]
                        s = sg[(fc * NTB + tb) % 2]
                        rd = [(hnT, c * 128, h2) for c in range(tb * 4, tb * 4 + 4) for h2 in range(2)]
                        for k in range(KT):
                            P.pe(lambda e, k=k, pg=pg, wb=wb, fc=fc, tb=tb: e.matmul(pg[:], wg[wb][:, k, fc * 128:(fc + 1) * 128], hnT[:, k, tb * 512:(tb + 1) * 512], start=(k == 0), stop=(k == KT - 1)),
                                 r=[wg[wb]] + rd, w=[pg])
                        for k in range(KT):
                            P.pe(lambda e, k=k, pu=pu, wb=wb, fc=fc, tb=tb: e.matmul(pu[:], wu[wb][:, k, fc * 128:(fc + 1) * 128], hnT[:, k, tb * 512:(tb + 1) * 512], start=(k == 0), stop=(k == KT - 1)),
                                 r=[wu[wb]] + rd, w=[pu])
                        P.act(lambda e, s=s, pg=pg: e.activation(s[:], pg[:], AF.Silu), r=[pg], w=[s])
                        hdst = hb.t[:, fc, tb * 512:(tb + 1) * 512]
                        P.dve(lambda e, hdst=hdst, s=s, pu=pu: e.tensor_tensor(hdst, s[:], pu[:], ALU.mult), r=[s, pu], w=[(hb, fc, tb)])
                        if moe:
                            csrc = cbc.t[:, ex, tb * 512:(tb + 1) * 512]
                            P.dve(lambda e, hdst=hdst, csrc=csrc: e.tensor_tensor(hdst, hdst, csrc, ALU.mult),
                                   r=[(hb, fc, tb)] + [(cbc, c) for c in range(tb * 4, tb * 4 + 4)], w=[(hb, fc, tb)])
                for dc in range(KT):
                    for tb in range(NTB):
                        pd = ps[4 + (dc * NTB + tb) % 4]
                        for fc in range(4):
                            P.pe(lambda e, fc=fc, pd=pd, wb=wb, dc=dc, tb=tb, hb=hb: e.matmul(pd[:], wd[wb][:, fc, dc * 128:(dc + 1) * 128], hb[:, fc, tb * 512:(tb + 1) * 512], start=(fc == 0), stop=(fc == 3)),
                                 r=[wd[wb], (hb, fc, tb)], w=[pd])
                        adst = acc.t[:, dc, tb * 512:(tb + 1) * 512]
                        if gi == 0:
                            P.act(lambda e, adst=adst, pd=pd: e.copy(adst, pd[:]), r=[pd], w=[(acc, dc, tb)])
                        else:
                            P.dve(lambda e, adst=adst, pd=pd: e.tensor_tensor(adst, adst, pd[:], ALU.add), r=[pd, (acc, dc, tb)], w=[(acc, dc, tb)])
        for tt in range(T // 128):
            b = tt % 2
            tb = tt // 4
            P.dma(xt[b][:], xsrc[t0 + tt * 128:t0 + (tt + 1) * 128, :], w=[xt[b]])
            banks = (ps[0 + 2 * b], ps[1 + 2 * b])
            for half in range(2):
                bk = banks[half]
                for j in range(4):
                    k = half * 4 + j
                    P.pe(lambda e, bk=bk, j=j, k=k, tt=tt: e.transpose(bk[:, j * 128:(j + 1) * 128], acc[:, k, tt * 128:(tt + 1) * 128], C.ID[:]),
                         r=[(acc, k, tb), C.ID], w=[bk])
                P.dve(lambda e, bk=bk, half=half, b=b: e.tensor_tensor(xn[b][:, half * 512:(half + 1) * 512], xt[b][:, half * 512:(half + 1) * 512], bk[:], ALU.add),
                      r=[bk, xt[b]], w=[xn[b]] if half == 0 else [xn[b]])
            if nfin_d is not None:
                norm_tile(P, C, xn[b], nfbc, xt[b], ssb[b], junk)
                P.dma(xdst[t0 + tt * 128:t0 + (tt + 1) * 128, :], xt[b][:], r=[xt[b]], w=[("xdst", id(xdst), t0 + tt * 128)])
            else:
                P.dma(xdst[t0 + tt * 128:t0 + (tt + 1) * 128, :], xn[b][:], r=[xn[b]], w=[("xdst", id(xdst), t0 + tt * 128)])
    P.barrier()
    P.release(m0)


def phase_inproj(P, C, SEQ, xsrc, nw_d, win_d, proj_d):
    m0 = P.mark()
    ps = C.ps
    nwbc = P.sb("nwbc", [128, D], F32)
    P.dma(nwbc[:], nw_d, w=[nwbc])
    W = P.sb("Win", [128, KT, NPROJ], BF16)
    wv = win_d.rearrange("(k p) n -> p k n", p=128)
    for k in range(KT):
        for h in range(2):
            P.dma(W.t[:, k, h * 1600:(h + 1) * 1600], wv[:, k, h * 1600:(h + 1) * 1600], w=[(W, k, h)], eng="pool")
    wkeys = [(W, k, h) for k in range(KT) for h in range(2)]
    hnT = P.sb("hnT", [128, KT, SEQ], BF16)
    xt = [P.sb(f"xt{i}", [128, D], F32) for i in range(2)]
    xn = [P.sb(f"xn{i}", [128, D], F32) for i in range(2)]
    junk = P.sb("junk", [128, D], F32)
    ssb = [P.sb(f"ss{i}", [128, 8], F32) for i in range(2)]
    stg = [P.sb(f"stg{i}", [128, 512], F32) for i in range(4)]
    for tt in range(SEQ // 128):
        b = tt % 2
        P.dma(xt[b][:], xsrc[tt * 128:(tt + 1) * 128, :], w=[xt[b]])
        norm_tile(P, C, xt[b], nwbc, xn[b], ssb[b], junk)
        transpose_to_T(P, C, xn[b], (ps[4 + 2 * b], ps[5 + 2 * b]), hnT, tt * 128)
    n = 0
    for tb in range(SEQ // 512):
        rd = [(hnT, c * 128, h2) for c in range(tb * 4, tb * 4 + 4) for h2 in range(2)]
        for mc in range(NPROJ // 128):
            pb = ps[n % 4]
            s = stg[n % 4]
            for k in range(KT):
                P.pe(lambda e, k=k, pb=pb, mc=mc, tb=tb: e.matmul(pb[:], W[:, k, mc * 128:(mc + 1) * 128], hnT[:, k, tb * 512:(tb + 1) * 512], start=(k == 0), stop=(k == KT - 1)),
                     r=wkeys + rd, w=[pb])
            if n % 2 == 0:
                P.act(lambda e, s=s, pb=pb: e.copy(s[:], pb[:]), r=[pb], w=[s])
            else:
                P.dve(lambda e, s=s, pb=pb: e.tensor_copy(s[:], pb[:]), r=[pb], w=[s])
            P.dma(proj_d[mc * 128:(mc + 1) * 128, tb * 512:(tb + 1) * 512], s[:], r=[s], w=[("proj", mc, tb)])
            n += 1
    P.barrier()
    P.release(m0)


def phase_outproj(P, C, SEQ, xsrc, xdst, mix_d, wout_d):
    m0 = P.mark()
    ps = C.ps
    Wo = P.sb("Wo", [128, KT, D], BF16)
    P.dma(Wo[:], wout_d.rearrange("(k p) n -> p k n", p=128), w=[Wo], eng="pool")
    mT = P.sb("mT", [128, KT, SEQ], BF16)
    mv = mix_d.rearrange("(k p) t -> p k t", p=128)
    for k in range(KT):
        P.dma(mT.t[:, k, :], mv[:, k, :], w=[(mT, k)])
    mk = [(mT, k) for k in range(KT)]
    xt = [P.sb(f"xt{i}", [128, D], F32) for i in range(2)]
    xn = [P.sb(f"xn{i}", [128, D], F32) for i in range(2)]
    for tt in range(SEQ // 128):
        b = tt % 2
        P.dma(xt[b][:], xsrc[tt * 128:(tt + 1) * 128, :], w=[xt[b]])
        for half in range(2):
            pb = ps[(tt * 2 + half) % 4]
            for k in range(KT):
                P.pe(lambda e, k=k, pb=pb, half=half, tt=tt: e.matmul(pb[:], mT[:, k, tt * 128:(tt + 1) * 128], Wo[:, k, half * 512:(half + 1) * 512], start=(k == 0), stop=(k == KT - 1)),
                     r=mk + [Wo], w=[pb])
            P.dve(lambda e, pb=pb, half=half, b=b: e.tensor_tensor(xn[b][:, half * 512:(half + 1) * 512], xt[b][:, half * 512:(half + 1) * 512], pb[:], ALU.add),
                  r=[pb, xt[b]], w=[xn[b]])
        P.dma(xdst[tt * 128:(tt + 1) * 128, :], xn[b][:], r=[xn[b]], w=[("xo", tt)])
    P.barrier()
    P.release(m0)


TWO_PI = 6.283185307179586
CW1 = 6.28125
CW2 = TWO_PI - CW1
RMAGIC = 12582912.0
PI_SAFE = 3.1415925


def sincos(P, x, sin_o, cos_o, kt, ks, tmp, N):
    xk, sk, ck, kk, tk = ks
    for phase, o, ok in ((0.0, sin_o, sk), (0.25, cos_o, ck)):
        P.dve(lambda e, phase=phase: e.tensor_scalar(kt, x, 1.0 / TWO_PI, phase, ALU.mult, ALU.add), r=[xk], w=[kk])
        P.dve(lambda e: e.tensor_scalar(kt, kt, RMAGIC, RMAGIC, ALU.add, ALU.subtract), r=[kk], w=[kk])
        P.dve(lambda e: e.scalar_tensor_tensor(tmp, kt, -CW1, x, ALU.mult, ALU.add), r=[kk, xk], w=[tk])
        P.dve(lambda e: e.scalar_tensor_tensor(tmp, kt, -CW2, tmp, ALU.mult, ALU.add), r=[kk, tk], w=[tk])
        if phase:
            P.dve(lambda e: e.tensor_scalar(tmp, tmp, 0.25 * TWO_PI, PI_SAFE, ALU.add, ALU.min), r=[tk], w=[tk])
        else:
            P.dve(lambda e: e.tensor_scalar(tmp, tmp, PI_SAFE, None, ALU.min), r=[tk], w=[tk])
        P.dve(lambda e: e.tensor_scalar(tmp, tmp, -PI_SAFE, None, ALU.max), r=[tk], w=[tk])
        P.act(lambda e, o=o: e.activation(o, tmp, AF.Sin), r=[tk], w=[ok])


def mixer_s5(P, C, SEQ, proj_d, mix_d, W, concurrent=False, banks=None):
    m0 = P.mark()
    ps = C.ps
    Q = 256 if concurrent else 512
    NCH = SEQ // Q
    prm = P.sb("s5prm", [128, 12, 8], F32)
    for i, nm in enumerate(("a_re_s", "a_im_s", "ls_s")):
        P.dma(prm.t[:, i, :], W[nm], w=[(prm, i)])
    pk = lambda i: (prm, i)
    P.act(lambda e: e.activation(prm.t[:, 3, :], prm.t[:, 2, :], AF.Exp), r=[pk(2)], w=[pk(3)])
    P.dve(lambda e: e.tensor_tensor(prm.t[:, 4, :], prm.t[:, 0, :], prm.t[:, 3, :], ALU.mult), r=[pk(0), pk(3)], w=[pk(4)])
    P.act(lambda e: e.activation(prm.t[:, 4, :], prm.t[:, 4, :], AF.Exp), r=[pk(4)], w=[pk(4)])
    P.dve(lambda e: e.tensor_tensor(prm.t[:, 5, :], prm.t[:, 1, :], prm.t[:, 3, :], ALU.mult), r=[pk(1), pk(3)], w=[pk(5)])
    QT = Q + 1
    cosT = P.sb("cosT", [128, 8, QT], F32)
    sinT = P.sb("sinT", [128, 8, QT], F32)
    Bb_re = P.sb("Bb_re", [128, 1024], BF16)
    Bb_im = P.sb("Bb_im", [128, 1024], BF16)
    Wc_re = P.sb("Wc_re", [128, 1024], F32)
    Wc_im = P.sb("Wc_im", [128, 1024], F32)
    wglu = P.sb("wglu", [128, 2, 256], BF16)
    cols = P.sb("s5cols", [128, 4], F32)
    mscr = P.mark()
    io = P.sb("iota", [128, QT], F32)
    P.pool(lambda e: e.iota(io[:], [[1, QT]], base=0, channel_multiplier=0, allow_small_or_imprecise_dtypes=True), w=[io])
    xa = P.sb("xa", [128, QT], F32)
    kt_ = P.sb("kts", [128, QT], F32)
    tp_ = P.sb("tps", [128, QT], F32)
    for j in range(8):
        P.dve(lambda e, j=j: e.tensor_scalar(xa[:], io[:], prm.t[:, 5, j:j + 1], None, ALU.mult), r=[io, pk(5)], w=[xa])
        sincos(P, xa[:], sinT.t[:, j, :], cosT.t[:, j, :], kt_[:], (xa, (sinT, j), (cosT, j), kt_, tp_), tp_[:], QT)
    m1 = P.mark()
    rw = [P.sb(f"s5rw{i}", [128, 1024], F32) for i in range(10)]
    P.dma(rw[0][:], W["a_re_r"], w=[rw[0]])
    P.dma(rw[1][:], W["a_im_r"], w=[rw[1]])
    P.dma(rw[2][:], W["ls_r"], w=[rw[2]])
    P.act(lambda e: e.activation(rw[2][:], rw[2][:], AF.Exp), r=[rw[2]], w=[rw[2]])
    P.dve(lambda e: e.tensor_tensor(rw[3][:], rw[0][:], rw[2][:], ALU.mult), r=[rw[0], rw[2]], w=[rw[3]])
    P.act(lambda e: e.activation(rw[3][:], rw[3][:], AF.Exp), r=[rw[3]], w=[rw[3]])
    P.dve(lambda e: e.tensor_tensor(rw[4][:], rw[1][:], rw[2][:], ALU.mult), r=[rw[1], rw[2]], w=[rw[4]])
    sincos(P, rw[4][:], rw[5][:], rw[6][:], rw[7][:], (rw[4], rw[5], rw[6], rw[7], rw[8]), rw[8][:], 1024)
    P.dve(lambda e: e.tensor_tensor(rw[6][:], rw[6][:], rw[3][:], ALU.mult), r=[rw[6], rw[3]], w=[rw[6]])
    P.dve(lambda e: e.tensor_scalar(rw[6][:], rw[6][:], -1.0, None, ALU.add), r=[rw[6]], w=[rw[6]])
    P.dve(lambda e: e.tensor_tensor(rw[5][:], rw[5][:], rw[3][:], ALU.mult), r=[rw[5], rw[3]], w=[rw[5]])
    P.dve(lambda e: e.tensor_tensor(rw[9][:], rw[0][:], rw[0][:], ALU.mult), r=[rw[0]], w=[rw[9]])
    P.dve(lambda e: e.tensor_tensor(rw[7][:], rw[1][:], rw[1][:], ALU.mult), r=[rw[1]], w=[rw[7]])
    P.dve(lambda e: e.tensor_tensor(rw[9][:], rw[9][:], rw[7][:], ALU.add), r=[rw[9], rw[7]], w=[rw[9]])
    P.dve(lambda e: e.reciprocal(rw[9][:], rw[9][:]), r=[rw[9]], w=[rw[9]])
    P.dve(lambda e: e.tensor_tensor(rw[7][:], rw[6][:], rw[0][:], ALU.mult), r=[rw[6], rw[0]], w=[rw[7]])
    P.dve(lambda e: e.tensor_tensor(rw[8][:], rw[5][:], rw[1][:], ALU.mult), r=[rw[5], rw[1]], w=[rw[8]])
    P.dve(lambda e: e.tensor_tensor(rw[7][:], rw[7][:], rw[8][:], ALU.add), r=[rw[7], rw[8]], w=[rw[7]])
    P.dve(lambda e: e.tensor_tensor(rw[7][:], rw[7][:], rw[9][:], ALU.mult), r=[rw[7], rw[9]], w=[rw[7]])
    P.dve(lambda e: e.tensor_tensor(rw[8][:], rw[5][:], rw[0][:], ALU.mult), r=[rw[5], rw[0]], w=[rw[8]])
    P.dve(lambda e: e.tensor_tensor(rw[4][:], rw[6][:], rw[1][:], ALU.mult), r=[rw[6], rw[1]], w=[rw[4]])
    P.dve(lambda e: e.tensor_tensor(rw[8][:], rw[8][:], rw[4][:], ALU.subtract), r=[rw[8], rw[4]], w=[rw[8]])
    P.dve(lambda e: e.tensor_tensor(rw[8][:], rw[8][:], rw[9][:], ALU.mult), r=[rw[8], rw[9]], w=[rw[8]])
    P.dma(rw[0][:], W["wb_re"], w=[rw[0]])
    P.dma(rw[1][:], W["wb_im"], w=[rw[1]])
    P.dve(lambda e: e.tensor_tensor(rw[2][:], rw[7][:], rw[0][:], ALU.mult), r=[rw[7], rw[0]], w=[rw[2]])
    P.dve(lambda e: e.tensor_tensor(rw[3][:], rw[8][:], rw[1][:], ALU.mult), r=[rw[8], rw[1]], w=[rw[3]])
    P.dve(lambda e: e.tensor_tensor(Bb_re[:], rw[2][:], rw[3][:], ALU.subtract), r=[rw[2], rw[3]], w=[Bb_re])
    P.dve(lambda e: e.tensor_tensor(rw[2][:], rw[7][:], rw[1][:], ALU.mult), r=[rw[7], rw[1]], w=[rw[2]])
    P.dve(lambda e: e.tensor_tensor(rw[3][:], rw[8][:], rw[0][:], ALU.mult), r=[rw[8], rw[0]], w=[rw[3]])
    P.dve(lambda e: e.tensor_tensor(Bb_im[:], rw[2][:], rw[3][:], ALU.add), r=[rw[2], rw[3]], w=[Bb_im])
    P.dma(rw[4][:], W["wc_re"], w=[rw[4]])
    P.dma(rw[5][:], W["wc_im"], w=[rw[5]])
    P.act(lambda e: e.copy(Wc_re[:], rw[4][:]), r=[rw[4]], w=[Wc_re])
    P.act(lambda e: e.mul(Wc_im[:], rw[5][:], -1.0), r=[rw[5]], w=[Wc_im])
    P.dma(wglu[:], W["wglu"].rearrange("(k p) n -> p k n", p=128), w=[wglu], eng="pool")
    P.dma(cols[:, 0:2], W["dcol"], w=[(cols, 0)])
    P.dma(cols[:, 2:4], W["ncol"], w=[(cols, 1)])
    P.barrier()
    P.release(mscr)
    yield
    if concurrent:
        bx, by = ps[banks[0]], ps[banks[1]]
    u_bf = P.sb("u_bf", [128, 2, Q], BF16)
    uv = proj_d[0:256, :].rearrange("(k p) t -> p k t", p=128)
    ini = P.sb("ini", [128, 8, 2], F32)
    P.dve(lambda e: e.memset(ini[:], 0.0), w=[ini])
    ini2 = P.sb("ini2", [128, 8, 4], F32)
    t = [P.sb(f"s5t{i}", [128, Q], F32) for i in range(8)]
    Wr = [P.sb(f"Wr{i}", [128, Q], F32) for i in range(2)]
    Wi = [P.sb(f"Wi{i}", [128, Q], F32) for i in range(2)]
    S_re = P.sb("S_re", [128, 8, Q], F32)
    S_im = P.sb("S_im", [128, 8, Q], F32)
    u32 = P.sb("u32", [128, 2, Q], F32)
    pt = [P.sb(f"s5p{i}", [128, Q], F32) for i in range(4)]
    gl = P.sb("s5gl", [128, 2, Q], BF16)
    y2 = P.sb("s5y2", [128, 2, Q], F32)
    sq = P.sb("s5sq", [128, 2, Q], F32)
    ob = P.sb("s5ob", [128, 2, Q], BF16)
    for c in range(NCH):
        c0 = c * Q
        ukeys = [u_bf]
        P.dma(u32[:], uv[:, :, c0:c0 + Q], w=[u32])
        P.act(lambda e: e.copy(u_bf[:], u32[:]), r=[u32], w=[u_bf])
        for j in range(8):
            b = j % 2
            ut = j // 4
            pa, pb_ = (bx, by) if concurrent else (ps[0 + b], ps[2 + b])
            P.pe(lambda e, j=j, pa=pa, ut=ut, c0=c0: e.matmul(pa[:, 0:Q], Bb_re[:, j * 128:(j + 1) * 128], u_bf[:, ut, :], start=True, stop=True), r=[Bb_re] + ukeys, w=[pa])
            P.pe(lambda e, j=j, pb_=pb_, ut=ut, c0=c0: e.matmul(pb_[:, 0:Q], Bb_im[:, j * 128:(j + 1) * 128], u_bf[:, ut, :], start=True, stop=True), r=[Bb_im] + ukeys, w=[pb_])
            cs, sn = cosT.t[:, j, 0:Q], sinT.t[:, j, 0:Q]
            T0, T1, T2, T3 = t[4 * b:4 * b + 4]
            P.dve(lambda e, T0=T0, pa=pa, cs=cs: e.tensor_tensor(T0[:], pa[:, 0:Q], cs, ALU.mult), r=[pa, (cosT, j)], w=[T0])
            P.dve(lambda e, T1=T1, pb_=pb_, sn=sn: e.tensor_tensor(T1[:], pb_[:, 0:Q], sn, ALU.mult), r=[pb_, (sinT, j)], w=[T1])
            P.dve(lambda e, T2=T2, pb_=pb_, cs=cs: e.tensor_tensor(T2[:], pb_[:, 0:Q], cs, ALU.mult), r=[pb_, (cosT, j)], w=[T2])
            P.dve(lambda e, T3=T3, pa=pa, sn=sn: e.tensor_tensor(T3[:], pa[:, 0:Q], sn, ALU.mult), r=[pa, (sinT, j)], w=[T3])
            P.pool(lambda e, T0=T0, T1=T1: e.tensor_tensor(T0[:], T0[:], T1[:], ALU.add), r=[T0, T1], w=[T0])
            P.pool(lambda e, T2=T2, T3=T3: e.tensor_tensor(T2[:], T2[:], T3[:], ALU.subtract), r=[T2, T3], w=[T2])
            rmag = prm.t[:, 4, j:j + 1].broadcast_to([128, Q])
            P.dve(lambda e, b=b, T0=T0, rmag=rmag, j=j: e.tensor_tensor_scan(Wr[b][:], rmag, T0[:], ini.t[:, j, 0:1], ALU.mult, ALU.add), r=[T0, pk(4), ini], w=[Wr[b]])
            P.dve(lambda e, b=b, T2=T2, rmag=rmag, j=j: e.tensor_tensor_scan(Wi[b][:], rmag, T2[:], ini.t[:, j, 1:2], ALU.mult, ALU.add), r=[T2, pk(4), ini], w=[Wi[b]])
            P.pool(lambda e, T0=T0, b=b, cs=cs: e.tensor_tensor(T0[:], Wr[b][:], cs, ALU.mult), r=[Wr[b], (cosT, j)], w=[T0])
            P.pool(lambda e, T1=T1, b=b, sn=sn: e.tensor_tensor(T1[:], Wi[b][:], sn, ALU.mult), r=[Wi[b], (sinT, j)], w=[T1])
            P.pool(lambda e, T0=T0, T1=T1, j=j: e.tensor_tensor(S_re.t[:, j, :], T0[:], T1[:], ALU.subtract), r=[T0, T1], w=[(S_re, j)])
            P.pool(lambda e, T2=T2, b=b, sn=sn: e.tensor_tensor(T2[:], Wr[b][:], sn, ALU.mult), r=[Wr[b], (sinT, j)], w=[T2])
            P.pool(lambda e, T3=T3, b=b, cs=cs: e.tensor_tensor(T3[:], Wi[b][:], cs, ALU.mult), r=[Wi[b], (cosT, j)], w=[T3])
            P.pool(lambda e, T2=T2, T3=T3, j=j: e.tensor_tensor(S_im.t[:, j, :], T2[:], T3[:], ALU.add), r=[T2, T3], w=[(S_im, j)])
            cq, sq_ = cosT.t[:, j, Q:Q + 1], sinT.t[:, j, Q:Q + 1]
            P.dve(lambda e, b=b, j=j, cq=cq: e.tensor_tensor(ini2.t[:, j, 0:1], Wr[b][:, Q - 1:Q], cq, ALU.mult), r=[Wr[b], (cosT, j)], w=[ini2])
            P.dve(lambda e, b=b, j=j, sq_=sq_: e.tensor_tensor(ini2.t[:, j, 1:2], Wi[b][:, Q - 1:Q], sq_, ALU.mult), r=[Wi[b], (sinT, j)], w=[ini2])
            P.dve(lambda e, b=b, j=j, sq_=sq_: e.tensor_tensor(ini2.t[:, j, 2:3], Wr[b][:, Q - 1:Q], sq_, ALU.mult), r=[Wr[b], (sinT, j)], w=[ini2])
            P.dve(lambda e, b=b, j=j, cq=cq: e.tensor_tensor(ini2.t[:, j, 3:4], Wi[b][:, Q - 1:Q], cq, ALU.mult), r=[Wi[b], (cosT, j)], w=[ini2])
            P.dve(lambda e, j=j: e.tensor_tensor(ini.t[:, j, 0:1], ini2.t[:, j, 0:1], ini2.t[:, j, 1:2], ALU.subtract), r=[ini2], w=[ini])
            P.dve(lambda e, j=j: e.tensor_tensor(ini.t[:, j, 1:2], ini2.t[:, j, 2:3], ini2.t[:, j, 3:4], ALU.add), r=[ini2], w=[ini])
        for ut in range(2):
            py = (bx, by)[ut] if concurrent else ps[4 + ut]
            n = 0
            for j in range(4 * ut, 4 * ut + 4):
                P.pe(lambda e, j=j, py=py, n=n: e.matmul(py[:, 0:Q], Wc_re[:, j * 128:(j + 1) * 128], S_re[:, j, :], start=(n == 0), stop=False), r=[Wc_re, (S_re, j)], w=[py])
                n += 1
                P.pe(lambda e, j=j, py=py, n=n: e.matmul(py[:, 0:Q], Wc_im[:, j * 128:(j + 1) * 128], S_im[:, j, :], start=False, stop=(n == 7)), r=[Wc_im, (S_im, j)], w=[py])
                n += 1
            Y, A1, A2, A3 = pt
            P.dve(lambda e, ut=ut, py=py: e.scalar_tensor_tensor(Y[:], u32[:, ut, :], cols[:, ut:ut + 1], py[:, 0:Q], ALU.mult, ALU.add), r=[u32, (cols, 0), py], w=[Y])
            P.pool(lambda e: e.tensor_tensor(A1[:], Y[:], Y[:], ALU.mult), r=[Y], w=[A1])
            P.pool(lambda e: e.tensor_scalar(A1[:], A1[:], 0.044715, 1.0, ALU.mult, ALU.add), r=[A1], w=[A1])
            P.pool(lambda e: e.tensor_tensor(A1[:], A1[:], Y[:], ALU.mult), r=[A1, Y], w=[A1])
            P.act(lambda e: e.activation(A2[:], A1[:], AF.Sigmoid, scale=1.5957691216057308), r=[A1], w=[A2])
            P.dve(lambda e, ut=ut: e.tensor_tensor(y2.t[:, ut, :], Y[:], A2[:], ALU.mult), r=[Y, A2], w=[(y2, ut)])
            P.act(lambda e, ut=ut: e.copy(gl.t[:, ut, :], y2.t[:, ut, :]), r=[(y2, ut)], w=[(gl, ut)])
        for mo in range(2):
            pz = (bx, by)[mo] if concurrent else ps[6 + mo]
            for k in range(2):
                P.pe(lambda e, k=k, mo=mo, pz=pz: e.matmul(pz[:, 0:Q], wglu[:, k, mo * 128:(mo + 1) * 128], gl[:, k, :], start=(k == 0), stop=(k == 1)), r=[wglu, (gl, 0), (gl, 1)], w=[pz])
            A1 = pt[1]
            P.act(lambda e, pz=pz: e.activation(A1[:], pz[:, 0:Q], AF.Sigmoid), r=[pz], w=[A1])
            P.dve(lambda e, mo=mo: e.tensor_tensor(y2.t[:, mo, :], y2.t[:, mo, :], A1[:], ALU.mult), r=[(y2, mo), A1], w=[(y2, mo)])
            P.pool(lambda e, mo=mo: e.tensor_tensor(sq.t[:, mo, :], y2.t[:, mo, :], y2.t[:, mo, :], ALU.mult), r=[(y2, mo)], w=[(sq, mo)])
        pss = bx if concurrent else ps[4]
        for k in range(2):
            P.pe(lambda e, k=k: e.matmul(pss[:, 0:Q], C.ones[:], sq[:, k, :], start=(k == 0), stop=(k == 1)), r=[C.ones, (sq, k)], w=[pss])
        R1, R2 = pt[2], pt[3]
        P.dve(lambda e: e.tensor_scalar(R1[:], pss[:, 0:Q], 1.0 / 256, EPS, ALU.mult, ALU.add), r=[pss], w=[R1])
        P.act(lambda e: e.activation(R1[:], R1[:], AF.Sqrt), r=[R1], w=[R1])
        P.dve(lambda e: e.reciprocal(R2[:], R1[:]), r=[R1], w=[R2])
        for mo in range(2):
            P.dve(lambda e, mo=mo: e.scalar_tensor_tensor(ob.t[:, mo, :], y2.t[:, mo, :], cols[:, 2 + mo:3 + mo], R2[:], ALU.mult, ALU.mult), r=[(y2, mo), (cols, 1), R2], w=[(ob, mo)])
            P.dma(mix_d[mo * 128:(mo + 1) * 128, c0:c0 + Q], ob.t[:, mo, :], r=[(ob, mo)], w=[("mix", mo, c)])
    if not concurrent:
        P.barrier()
        P.release(m0)
    yield


def rep128(v):
    v = np.asarray(v, np.float32).reshape(1, -1)
    return np.ascontiguousarray(np.broadcast_to(v, (128, v.shape[1])))


def host_layer_inputs(inp, l):
    o = {}
    f = lambda a: np.ascontiguousarray(np.asarray(a, np.float32))
    win = f(inp["w_in"][l])
    wp = np.zeros((D, NPROJ), np.float32)
    wp[:, 0:1536] = win[:, 0:1536]
    wp[:, 1536:2688] = win[:, 1542:2694]
    wp[:, 2688:3072] = win[:, 2694:3078]
    wp[:, 3072:3078] = win[:, 1536:1542]
    wp[:, 3078:3084] = win[:, 3078:3084]
    wp[:, 3084:3090] = win[:, 3084:3090]
    o["win"] = wp
    o["wout"] = f(inp["w_out"][l])
    o["nmix"] = rep128(inp["norm_mix"][l])
    o["nffn"] = rep128(inp["norm_ffn"][l])
    def slay(a):
        return np.ascontiguousarray(f(a).reshape(8, 2, 64).transpose(1, 2, 0).reshape(128, 8))
    o["a_re_s"] = slay(inp["s5_a_re"][l])
    o["a_im_s"] = slay(inp["s5_a_im"][l])
    o["ls_s"] = slay(np.broadcast_to(f(inp["s5_log_step"][l])[:, None], (16, 64)))
    o["a_re_r"] = rep128(f(inp["s5_a_re"][l]).reshape(-1))
    o["a_im_r"] = rep128(f(inp["s5_a_im"][l]).reshape(-1))
    o["ls_r"] = rep128(np.broadcast_to(f(inp["s5_log_step"][l])[:, None], (16, 64)).reshape(-1))
    for nm, src in (("wb_re", inp["s5_b_re"][l]), ("wb_im", inp["s5_b_im"][l])):
        B = f(src)
        wb = np.zeros((128, 8, 128), np.float32)
        for g in range(16):
            j, two = divmod(g, 2)
            r0 = (g % 8) * 16
            wb[r0:r0 + 16, j, two * 64:(two + 1) * 64] = B[g].T
        o[nm] = wb.reshape(128, 1024)
    for nm, src in (("wc_re", inp["s5_c_re"][l]), ("wc_im", inp["s5_c_im"][l])):
        Cm = f(src)
        wc = np.zeros((128, 8, 128), np.float32)
        for g in range(16):
            j, two = divmod(g, 2)
            c0 = (g % 8) * 16
            wc[two * 64:(two + 1) * 64, j, c0:c0 + 16] = Cm[g].T
        o[nm] = wc.reshape(128, 1024)
    o["dcol"] = np.ascontiguousarray(f(inp["s5_d"][l]).reshape(2, 128).T)
    o["ncol"] = np.ascontiguousarray(f(inp["s5_norm"][l]).reshape(2, 128).T)
    o["wglu"] = f(inp["s5_w_glu"][l])
    o["ssd_cw"] = np.ascontiguousarray(f(inp["ssd_conv_w"][l]).reshape(4, 7, 128).transpose(2, 1, 0))
    o["ssd_cb"] = np.ascontiguousarray(f(inp["ssd_conv_b"][l]).reshape(7, 128).T)
    o["ssd_dtb"] = rep128(inp["ssd_dt_bias"][l])
    o["ssd_alog"] = rep128(inp["ssd_a_log"][l])
    o["ssd_drep"] = rep128(np.repeat(f(inp["ssd_d"][l]), 64))
    o["ssd_nw"] = rep128(inp["ssd_norm"][l])
    o["gdn_cw"] = np.ascontiguousarray(f(inp["gdn_conv_w"][l]).reshape(4, 9, 128).transpose(2, 1, 0))
    o["gdn_alog"] = rep128(inp["gdn_a_log"][l])
    o["gdn_dtb"] = rep128(inp["gdn_dt_bias"][l])
    o["gdn_nw"] = rep128(np.tile(f(inp["gdn_norm"][l]), 6))
    return o


F32R = mybir.dt.float32r
USE_F32R = False


def mm(P, out, lhsT, rhs, r, w, start=True, stop=True, exact=False):
    if USE_F32R and not exact and lhsT.dtype == F32 and rhs.dtype == F32:
        lhsT = lhsT.bitcast(F32R)
        rhs = rhs.bitcast(F32R)
    P.pe(lambda e: e.matmul(out, lhsT, rhs, start=start, stop=stop), r=r, w=w)


def tr(P, C, out, in_, n, r, w):
    P.pe(lambda e: e.transpose(out, in_, C.ID[0:n, 0:n]), r=list(r) + [C.ID], w=w)


def tt(P, eng, out, a, b, op, r, w):
    P.op(eng, lambda e: e.tensor_tensor(out, a, b, op), r, w)


def ts(P, eng, out, a, s1, s2, op0, op1, r, w):
    if op1 is None:
        P.op(eng, lambda e: e.tensor_scalar(out, a, s1, None, op0), r, w)
    else:
        P.op(eng, lambda e: e.tensor_scalar(out, a, s1, s2, op0, op1), r, w)


def stt(P, out, a, s, b, op0, op1, r, w):
    P.dve(lambda e: e.scalar_tensor_tensor(out, a, s, b, op0, op1), r, w)


def actf(P, out, in_, func, r, w, **kw):
    P.act(lambda e: e.activation(out, in_, func, **kw), r, w)


def conv_silu(P, C, SEQ, proj_d, row0, ntile, cw, cb, dst, xin, acc):
    for i in range(ntile):
        P.dve(lambda e: e.memset(xin[:, 0:3], 0.0), w=[xin])
        P.dma(xin[:, 3:3 + SEQ], proj_d[row0 + i * 128:row0 + (i + 1) * 128, :], w=[xin])
        ts(P, "dve", acc[:], xin[:, 0:SEQ], cw[:, i, 0:1], None, ALU.mult, None, [xin, cw], [acc])
        for k in range(1, 4):
            stt(P, acc[:], xin[:, k:k + SEQ], cw[:, i, k:k + 1], acc[:], ALU.mult, ALU.add, [xin, cw, acc], [acc])
        if cb is not None:
            actf(P, dst[i][:], acc[:], AF.Silu, [acc, cb], [dst[i]], bias=cb[:, i:i + 1])
        else:
            actf(P, dst[i][:], acc[:], AF.Silu, [acc], [dst[i]])


def conv_blk(P, SEQ, proj_d, row0, ntile, cw, cb, dst, xin, acc, t0):
    for i in range(ntile):
        xi = xin[i % 2]
        if t0 == 0:
            P.dve(lambda e, xi=xi: e.memset(xi[:, 0:3], 0.0), w=[xi])
            P.dma(xi[:, 3:131], proj_d[row0 + i * 128:row0 + (i + 1) * 128, 0:128], w=[xi])
        else:
            P.dma(xi[:, 0:131], proj_d[row0 + i * 128:row0 + (i + 1) * 128, t0 - 3:t0 + 128], w=[xi])
        ts(P, "dve", acc[:], xi[:, 0:128], cw[:, i, 0:1], None, ALU.mult, None, [xi, cw], [acc])
        for k in range(1, 4):
            stt(P, acc[:], xi[:, k:k + 128], cw[:, i, k:k + 1], acc[:], ALU.mult, ALU.add, [xi, cw, acc], [acc])
        if cb is not None:
            actf(P, dst[i][:], acc[:], AF.Silu, [acc, cb], [dst[i]], bias=cb[:, i:i + 1])
        else:
            actf(P, dst[i][:], acc[:], AF.Silu, [acc], [dst[i]])


def make_masks(P, C):
    def tri(name, cmp_base, cm, step):
        t = P.sb(name, [128, 128], F32)
        P.pool(lambda e: e.memset(t[:], 1.0), w=[t])
        P.pool(lambda e: e.affine_select(t[:], t[:], [[step, 128]], ALU.is_ge, 0.0, base=cmp_base, channel_multiplier=cm), r=[t], w=[t])
        return t
    C.U = tri("U", 0, -1, 1)
    C.Ls = tri("Ls", -1, 1, -1)
    C.L = tri("L", 0, 1, -1)
    C.BD = P.sb("BD", [128, 128], F32)
    P.pool(lambda e: e.memset(C.BD[:], 0.0), w=[C.BD])
    P.pool(lambda e: e.memset(C.BD[0:64, 0:64], 1.0), r=[C.BD], w=[C.BD])
    P.pool(lambda e: e.memset(C.BD[64:128, 64:128], 1.0), r=[C.BD], w=[C.BD])
    C.SEL0 = P.sb("SEL0", [128, 128], F32)
    C.SEL1 = P.sb("SEL1", [128, 128], F32)
    P.pool(lambda e: e.memset(C.SEL0[:], 0.0), w=[C.SEL0])
    P.pool(lambda e: e.memset(C.SEL0[0:64, :], 1.0), r=[C.SEL0], w=[C.SEL0])
    P.pool(lambda e: e.memset(C.SEL1[:], 0.0), w=[C.SEL1])
    P.pool(lambda e: e.memset(C.SEL1[64:128, :], 1.0), r=[C.SEL1], w=[C.SEL1])
    def neg(name, m01, extra=None):
        t = P.sb(name, [128, 128], F32)
        if extra is not None:
            tt(P, "pool", t[:], m01[:], extra[:], ALU.mult, [m01, extra], [t])
            ts(P, "pool", t[:], t[:], 1e30, -1e30, ALU.mult, ALU.add, [t], [t])
        else:
            ts(P, "pool", t[:], m01[:], 1e30, -1e30, ALU.mult, ALU.add, [m01], [t])
        return t
    C.negU = neg("negU", C.U)
    C.U2 = P.sb("U2", [128, 128], F32)
    tt(P, "pool", C.U2[:], C.U[:], C.BD[:], ALU.mult, [C.U, C.BD], [C.U2])
    C.negU2 = neg("negU2", C.U2)
    C.negLs2 = neg("negLs2", C.Ls, C.BD)


def mixer_ssd(P, C, SEQ, proj_d, mix_d, W, concurrent=False):
    m0 = P.mark()
    ps = C.ps
    NB = SEQ // 128
    cw = P.sb("ssd_cw", [128, 7, 4], F32)
    cb = P.sb("ssd_cb", [128, 7], F32)
    P.dma(cw[:], W["ssd_cw"], w=[cw])
    P.dma(cb[:], W["ssd_cb"], w=[cb])
    bc = P.sb("ssd_bc", [128, 4, 384], F32)
    P.dma(bc.t[:, 0, 0:6], W["ssd_dtb"], w=[(bc, 0)])
    P.dma(bc.t[:, 1, 0:6], W["ssd_alog"], w=[(bc, 1)])
    P.dma(bc.t[:, 2, :], W["ssd_drep"], w=[(bc, 2)])
    P.dma(bc.t[:, 3, :], W["ssd_nw"], w=[(bc, 3)])
    actf(P, bc.t[:, 1, 0:6], bc.t[:, 1, 0:6], AF.Exp, [(bc, 1)], [(bc, 1)])
    ts(P, "dve", bc.t[:, 1, 0:6], bc.t[:, 1, 0:6], -1.0, None, ALU.mult, None, [(bc, 1)], [(bc, 1)])
    xin = [P.sb(f"cv_in{i}", [128, 131], F32) for i in range(2)]
    acc = P.sb("cv_acc", [128, 128], F32)
    F = [P.sb(f"ssdF{i}", [128, 128], F32) for i in range(7)]
    zT = [P.sb(f"ssdz{i}", [128, 128], F32) for i in range(3)]
    sm = P.sb("ssd_sm", [6, 128], F32)
    S = P.sb("ssdS", [128, 6, 64], F32)
    P.dve(lambda e: e.memset(S[:], 0.0), w=[S])
    v = P.sb("ssdv", [128, 64], F32)
    E = [P.sb(f"ssdE{i}", [128, 128], F32) for i in range(2)]
    Mt = [P.sb(f"ssdM{i}", [128, 128], F32) for i in range(2)]
    Xtm = P.sb("ssdXtm", [128, 384], F32)
    xdt = P.sb("ssdxdt", [128, 384], F32)
    xdd = P.sb("ssdxdd", [128, 384], F32)
    Btm = P.sb("ssdBtm", [128, 256], F32)
    yd = P.sb("ssdyd", [128, 384], F32)
    y = P.sb("ssdy", [128, 384], F32)
    zs = P.sb("ssdzs", [128, 384], F32)
    junk = P.sb("ssdjunk", [128, 192], F32)
    ssq = P.sb("ssdss", [128, 8], F32)
    ob = P.sb("ssdob", [128, 3, 128], BF16)
    if concurrent:
        b0, b1, b2, b3, b6, b7, o7 = ps[3], ps[4], ps[4], ps[4], ps[5], ps[3], 64
        bR = lambda b: (ps[4], 256 + b * 128)
    else:
        b0, b1, b2, b3, b6, b7, o7 = ps[0], ps[1], ps[2], ps[3], ps[6], ps[7], 0
        bR = lambda b: (ps[4 + b], 0)
    for blk in range(NB):
        t0 = blk * 128
        cs = slice(t0, t0 + 128)
        conv_blk(P, SEQ, proj_d, 640, 7, cw, cb, F, xin, acc, t0)
        for i in range(3):
            P.dma(zT[i][:], proj_d[256 + i * 128:256 + (i + 1) * 128, cs], w=[zT[i]])
        P.dma(sm[:], proj_d[3072:3078, cs], w=[sm])
        tr(P, C, b0[:, 0:6], sm[0:6, :], 6, [sm], [b0])
        tt(P, "dve", v[:, 0:6], b0[:, 0:6], bc.t[:, 0, 0:6], ALU.add, [b0, (bc, 0)], [v])
        actf(P, v[:, 0:6], v[:, 0:6], AF.Exp, [v], [v])
        actf(P, v[:, 0:6], v[:, 0:6], AF.Ln, [v], [v], bias=C.ones[:, 0:1])
        tt(P, "dve", v[:, 6:12], v[:, 0:6], bc.t[:, 1, 0:6], ALU.mult, [v, (bc, 1)], [v])
        mm(P, b0[:, 8:14], C.U[:], v[:, 6:12], [C.U, v], [b0])
        mm(P, b0[:, 16:22], C.ones[:], v[:, 6:12], [C.ones, v], [b0])
        P.dve(lambda e: e.tensor_copy(v[:, 12:18], b0[:, 8:14]), r=[b0], w=[v])
        ts(P, "dve", v[:, 18:24], v[:, 12:18], -1.0, None, ALU.mult, None, [v], [v])
        P.dve(lambda e: e.tensor_copy(v[:, 24:30], b0[:, 16:22]), r=[b0], w=[v])
        tt(P, "dve", v[:, 30:36], v[:, 24:30], v[:, 12:18], ALU.subtract, [v], [v])
        actf(P, v[:, 30:36], v[:, 30:36], AF.Exp, [v], [v])
        tt(P, "dve", v[:, 30:36], v[:, 30:36], v[:, 0:6], ALU.mult, [v], [v])
        actf(P, v[:, 36:42], v[:, 12:18], AF.Exp, [v], [v])
        actf(P, v[:, 42:48], v[:, 24:30], AF.Exp, [v], [v])
        for i in range(3):
            tr(P, C, b1[:, i * 128:(i + 1) * 128], F[i][:], 128, [F[i]], [b1])
        P.act(lambda e: e.copy(Xtm[:], b1[:, 0:384]), r=[b1], w=[Xtm])
        for h in range(6):
            hs = slice(h * 64, (h + 1) * 64)
            actf(P, xdt[:, hs], Xtm[:, hs], AF.Copy, [Xtm, v], [(xdt, h)], scale=v[:, h:h + 1])
            actf(P, xdd[:, hs], Xtm[:, hs], AF.Copy, [Xtm, v], [(xdd, h)], scale=v[:, 30 + h:31 + h])
        for g in range(2):
            tr(P, C, b2[:, g * 128:(g + 1) * 128], F[3 + g][:], 128, [F[3 + g]], [b2])
        P.dve(lambda e: e.tensor_copy(Btm[:], b2[:, 0:256]), r=[b2], w=[Btm])
        for g in range(2):
            mm(P, b3[:, g * 128:(g + 1) * 128], F[3 + g][:], F[5 + g][:], [F[3 + g], F[5 + g]], [b3])
        for h in range(6):
            g = h // 3
            b = h % 2
            pr_, ro = bR(b)
            mm(P, pr_[:, ro:ro + 128], v[:, 6 + h:7 + h].broadcast_to([128, 128]), C.U[:], [v, C.U], [pr_])
            stt(P, E[b][:], pr_[:, ro:ro + 128], v[:, 18 + h:19 + h], C.negU[:], ALU.add, ALU.add, [pr_, v, C.negU], [E[b]])
            actf(P, E[b][:], E[b][:], AF.Exp, [E[b]], [E[b]])
            tt(P, "dve", Mt[b][:], b3[:, g * 128:(g + 1) * 128], E[b][:], ALU.mult, [b3, E[b]], [Mt[b]])
            hs = slice(h * 64, (h + 1) * 64)
            mm(P, b6[:, hs], Mt[b][:], xdt[:, hs], [Mt[b], (xdt, h)], [b6])
            mm(P, b7[:, o7 + h * 64:o7 + (h + 1) * 64], F[5 + g][:], S[:, h, :], [F[5 + g], (S, h)], [b7])
        P.act(lambda e: e.copy(yd[:], b6[:, 0:384]), r=[b6], w=[yd])
        for h in range(6):
            hs = slice(h * 64, (h + 1) * 64)
            stt(P, y[:, hs], b7[:, o7 + h * 64:o7 + (h + 1) * 64], v[:, 36 + h:37 + h], yd[:, hs], ALU.mult, ALU.add, [b7, v, yd], [(y, h)])
        for g in range(2):
            mm(P, b2[:, 256 + 0:256 + 192] if False else b0[:, 64 + g * 192:64 + (g + 1) * 192], Btm[:, g * 128:(g + 1) * 128], xdd[:, g * 192:(g + 1) * 192],
               [Btm] + [(xdd, h) for h in range(3 * g, 3 * g + 3)], [b0])
        for h in range(6):
            stt(P, S[:, h, :], S[:, h, :], v[:, 42 + h:43 + h], b0[:, 64 + h * 64:64 + (h + 1) * 64], ALU.mult, ALU.add, [(S, h), v, b0], [(S, h)])
        yk = [(y, h) for h in range(6)]
        tt(P, "pool", xdt[:], Xtm[:], bc.t[:, 2, :], ALU.mult, [Xtm, (bc, 2)] + [(xdt, h) for h in range(6)], [(xdt, h) for h in range(6)])
        tt(P, "pool", y[:], y[:], xdt[:], ALU.add, yk + [(xdt, h) for h in range(6)], yk)
        for i in range(3):
            tr(P, C, b1[:, i * 128:(i + 1) * 128], zT[i][:], 128, [zT[i]], [b1])
        actf(P, zs[:], b1[:, 0:384], AF.Silu, [b1], [zs])
        tt(P, "dve", y[:], y[:], zs[:], ALU.mult, yk + [zs], yk)
        for g in range(2):
            gs = slice(g * 192, (g + 1) * 192)
            actf(P, junk[:], y[:, gs], AF.Square, yk, [junk, (ssq, g)], accum_out=ssq[:, g:g + 1])
            ts(P, "dve", ssq[:, 2 + g:3 + g], ssq[:, g:g + 1], 1.0 / 192, EPS, ALU.mult, ALU.add, [(ssq, g)], [(ssq, g)])
            actf(P, ssq[:, 4 + g:5 + g], ssq[:, 2 + g:3 + g], AF.Sqrt, [(ssq, g)], [(ssq, g)])
            P.dve(lambda e, g=g: e.reciprocal(ssq[:, 6 + g:7 + g], ssq[:, 4 + g:5 + g]), r=[(ssq, g)], w=[(ssq, g)])
            stt(P, y[:, gs], y[:, gs], ssq[:, 6 + g:7 + g], bc.t[:, 3, gs], ALU.mult, ALU.mult, yk + [(ssq, g), (bc, 3)], yk)
        for i in range(3):
            tr(P, C, b2[:, i * 128:(i + 1) * 128], y[:, i * 128:(i + 1) * 128], 128, yk, [b2])
        P.act(lambda e: e.copy(ob[:], b2.t[:, 0:384].rearrange("p (k t) -> p k t", k=3)), r=[b2], w=[ob])
        P.dma(mix_d[256:640, cs].rearrange("(k p) t -> p k t", p=128), ob[:], r=[ob], w=[("mixssd", blk)])
    if not concurrent:
        P.barrier()
        P.release(m0)


def mixer_gdn(P, C, SEQ, proj_d, mix_d, W, concurrent=False):
    m0 = P.mark()
    ps = C.ps
    NBK = 3 if concurrent else 8
    if concurrent:
        ps = [ps[0], ps[1], ps[2], ps[1]]
    NB = SEQ // 128
    cw = P.sb("gdn_cw", [128, 9, 4], F32)
    P.dma(cw[:], W["gdn_cw"], w=[cw])
    bc = P.sb("gdn_bc", [128, 3, 384], F32)
    P.dma(bc.t[:, 0, 0:6], W["gdn_alog"], w=[(bc, 0)])
    P.dma(bc.t[:, 1, 0:6], W["gdn_dtb"], w=[(bc, 1)])
    P.dma(bc.t[:, 2, :], W["gdn_nw"], w=[(bc, 2)])
    actf(P, bc.t[:, 0, 0:6], bc.t[:, 0, 0:6], AF.Exp, [(bc, 0)], [(bc, 0)])
    ts(P, "dve", bc.t[:, 0, 0:6], bc.t[:, 0, 0:6], -1.0, None, ALU.mult, None, [(bc, 0)], [(bc, 0)])
    xin = [P.sb(f"gcv_in{i}", [128, 131], F32) for i in range(2)]
    acc = P.sb("gcv_acc", [128, 128], F32)
    F = [P.sb(f"gdnF{i}", [128, 128], F32) for i in range(9)]
    zT = [P.sb(f"gdnz{i}", [128, 128], F32) for i in range(3)]
    sm = P.sb("gdn_sm", [12, 128], F32)
    S = [P.sb(f"gdnS{h}", [64, 64], F32) for h in range(6)]
    for h in range(6):
        P.dve(lambda e, h=h: e.memset(S[h][:], 0.0), w=[S[h]])
    v = P.sb("gdnv", [128, 96], F32)
    big = {n: P.sb("gdn_" + n, [128, 384], F32) for n in ("q", "k", "v", "sq", "qn", "kn", "qd", "kbg", "kend", "vb", "o", "zs")}
    v3 = lambda t: t.t[:, :].rearrange("p (h d) -> p h d", h=6)
    hb = lambda c0: v[:, c0:c0 + 6].unsqueeze(2).broadcast_to([128, 6, 64])
    knT = [P.sb(f"knT{h}", [64, 128], F32) for h in range(6)]
    qnT = [P.sb(f"qnT{h}", [64, 128], F32) for h in range(6)]
    qdT = [P.sb(f"qdT{h}", [64, 128], F32) for h in range(6)]
    E = [P.sb(f"gE{h}", [128, 128], F32) for h in range(6)]
    DL = [P.sb(f"gDL{h}", [128, 128], F32) for h in range(6)]
    qkT = [P.sb(f"gqkT{h}", [128, 128], F32) for h in range(6)]
    Pk = [[P.sb(f"gP{h}_{i}", [128, 128], F32) for i in range(2)] for h in range(6)]
    PkT = [[P.sb(f"gPT{h}_{i}", [128, 128], F32) for i in range(2)] for h in range(6)]
    YT = [P.sb(f"gYT{h}", [128, 128], F32) for h in range(6)]
    WT = [P.sb(f"gWT{h}", [64, 128], F32) for h in range(6)]
    Us = [P.sb(f"gU{h}", [128, 64], F32) for h in range(6)]
    vn = [P.sb(f"gvn{h}", [128, 64], F32) for h in range(6)]
    oq = [P.sb(f"goq{h}", [128, 64], F32) for h in range(6)]
    ob = P.sb("gob", [128, 3, 128], BF16)
    allv = [v]
    for blk in range(NB):
        t0 = blk * 128
        cs = slice(t0, t0 + 128)
        conv_blk(P, SEQ, proj_d, 1536, 9, cw, None, F, xin, acc, t0)
        for i in range(3):
            P.dma(zT[i][:], proj_d[2688 + i * 128:2688 + (i + 1) * 128, cs], w=[zT[i]])
        P.dma(sm[:], proj_d[3078:3090, cs], w=[sm])
        tr(P, C, ps[0][:, 0:12], sm[0:12, :], 12, [sm], [ps[0]])
        actf(P, v[:, 0:6], ps[0][:, 0:6], AF.Sigmoid, [ps[0]], [v])
        tt(P, "dve", v[:, 6:12], ps[0][:, 6:12], bc.t[:, 1, 0:6], ALU.add, [ps[0], (bc, 1)], [v])
        actf(P, v[:, 6:12], v[:, 6:12], AF.Exp, [v], [v])
        actf(P, v[:, 6:12], v[:, 6:12], AF.Ln, [v], [v], bias=C.ones[:, 0:1])
        tt(P, "dve", v[:, 6:12], v[:, 6:12], bc.t[:, 0, 0:6], ALU.mult, [v, (bc, 0)], [v])
        mm(P, ps[0][:, 16:22], C.U2[:], v[:, 6:12], [C.U2, v], [ps[0]])
        mm(P, ps[0][:, 24:30], C.BD[:], v[:, 6:12], [C.BD, v], [ps[0]])
        mm(P, ps[0][:, 32:38], C.SEL0[:], v[:, 6:12], [C.SEL0, v], [ps[0]])
        mm(P, ps[0][:, 40:46], C.SEL1[:], v[:, 6:12], [C.SEL1, v], [ps[0]])
        P.dve(lambda e: e.tensor_copy(v[:, 12:18], ps[0][:, 16:22]), r=[ps[0]], w=[v])
        ts(P, "dve", v[:, 18:24], v[:, 12:18], -1.0, None, ALU.mult, None, [v], [v])
        tt(P, "dve", v[:, 30:36], ps[0][:, 24:30], v[:, 12:18], ALU.subtract, [ps[0], v], [v])
        actf(P, v[:, 30:36], v[:, 30:36], AF.Exp, [v], [v])
        actf(P, v[:, 36:42], v[:, 12:18], AF.Exp, [v], [v])
        actf(P, v[:, 42:48], ps[0][:, 32:38], AF.Exp, [ps[0]], [v])
        actf(P, v[:, 48:54], ps[0][:, 40:46], AF.Exp, [ps[0]], [v])
        ts(P, "dve", v[:, 78:84], v[:, 0:6], -1.0, None, ALU.mult, None, [v], [v])
        for n_, base, bank in (("q", 0, 1), ("k", 3, 2), ("v", 6, 3)):
            for i in range(3):
                tr(P, C, ps[bank][:, i * 128:(i + 1) * 128], F[base + i][:], 128, [F[base + i]], [ps[bank]])
            P.act(lambda e, n_=n_, bank=bank: e.copy(big[n_][:], ps[bank][:, 0:384]), r=[ps[bank]], w=[big[n_]])
        for n_, c_ss, c_r, sc in (("q", 54, 60, 0.125), ("k", 66, 72, 1.0)):
            tt(P, "pool", big["sq"][:], big[n_][:], big[n_][:], ALU.mult, [big[n_]], [big["sq"]])
            P.dve(lambda e, c_ss=c_ss: e.tensor_reduce(v[:, c_ss:c_ss + 6], v3(big["sq"]), AX.X, ALU.add), r=[big["sq"]], w=[v])
            ts(P, "dve", v[:, c_ss:c_ss + 6], v[:, c_ss:c_ss + 6], EPS, None, ALU.add, None, [v], [v])
            actf(P, v[:, c_ss:c_ss + 6], v[:, c_ss:c_ss + 6], AF.Sqrt, [v], [v])
            P.dve(lambda e, c_ss=c_ss, c_r=c_r: e.reciprocal(v[:, c_r:c_r + 6], v[:, c_ss:c_ss + 6]), r=[v], w=[v])
            if sc != 1.0:
                ts(P, "dve", v[:, c_r:c_r + 6], v[:, c_r:c_r + 6], sc, None, ALU.mult, None, [v], [v])
        tt(P, "dve", v3(big["qn"]), v3(big["q"]), hb(60), ALU.mult, [big["q"], v], [big["qn"]])
        tt(P, "dve", v3(big["kn"]), v3(big["k"]), hb(72), ALU.mult, [big["k"], v], [big["kn"]])
        tt(P, "pool", v3(big["qd"]), v3(big["qn"]), hb(36), ALU.mult, [big["qn"], v], [big["qd"]])
        tt(P, "pool", v3(big["kend"]), v3(big["kn"]), hb(30), ALU.mult, [big["kn"], v], [big["kend"]])
        tt(P, "dve", v3(big["kbg"]), v3(big["kn"]), hb(0), ALU.mult, [big["kn"], v], [big["kbg"]])
        tt(P, "dve", v3(big["kbg"]), v3(big["kbg"]), hb(36), ALU.mult, [big["kbg"], v], [big["kbg"]])
        tt(P, "pool", v3(big["vb"]), v3(big["v"]), hb(0), ALU.mult, [big["v"], v], [big["vb"]])
        for h in range(6):
            hs = slice(h * 64, (h + 1) * 64)
            for src, dstl, col in ((big["kn"], knT, 0), (big["qn"], qnT, 128), (big["qd"], qdT, 256)):
                tr(P, C, ps[1][0:64, col:col + 128], src[:, hs], 128, [src], [ps[1]])
            P.act(lambda e, h=h: e.copy(knT[h][:], ps[1][0:64, 0:128]), r=[ps[1]], w=[knT[h]])
            P.dve(lambda e, h=h: e.tensor_copy(qnT[h][:], ps[1][0:64, 128:256]), r=[ps[1]], w=[qnT[h]])
            P.act(lambda e, h=h: e.copy(qdT[h][:], ps[1][0:64, 256:384]), r=[ps[1]], w=[qdT[h]])
        H6 = range(6)
        hsl = [slice(h * 64, (h + 1) * 64) for h in H6]
        bk = lambda stage, h: ps[(2 * stage + (h % 2)) % NBK]
        for h in H6:
            pb = bk(1, h)
            mm(P, pb[:, 0:128], v[:, 6 + h:7 + h].broadcast_to([128, 128]), C.U2[:], [v, C.U2], [pb])
            stt(P, E[h][:], pb[:, 0:128], v[:, 18 + h:19 + h], C.negU2[:], ALU.add, ALU.add, [pb, v, C.negU2], [E[h]])
            actf(P, E[h][:], E[h][:], AF.Exp, [E[h]], [E[h]])
            stt(P, DL[h][:], pb[:, 0:128], -1.0, C.negLs2[:], ALU.mult, ALU.add, [pb, C.negLs2], [DL[h]])
            actf(P, DL[h][:], DL[h][:], AF.Exp, [DL[h], v], [DL[h]], bias=v[:, 12 + h:13 + h])
        for h in H6:
            pb = bk(2, h)
            mm(P, pb[:, 0:128], knT[h][:], qnT[h][:], [knT[h], qnT[h]], [pb])
            mm(P, pb[:, 128:256], knT[h][:], knT[h][:], [knT[h]], [pb])
            tt(P, "dve", qkT[h][:], pb[:, 0:128], E[h][:], ALU.mult, [pb, E[h]], [qkT[h]])
            stt(P, Pk[h][0][:], pb[:, 128:256], v[:, 78 + h:79 + h], DL[h][:], ALU.mult, ALU.mult, [pb, v, DL[h]], [Pk[h][0]])
        for h in H6:
            pb = bk(3, h)
            tr(P, C, pb[:, 0:128], Pk[h][0][:], 128, [Pk[h][0]], [pb])
            P.act(lambda e, h=h, pb=pb: e.copy(PkT[h][0][:], pb[:, 0:128]), r=[pb], w=[PkT[h][0]])
            tt(P, "dve", YT[h][:], pb[:, 0:128], C.ID[:], ALU.add, [pb, C.ID], [YT[h]])
        cur = 0
        for lvl in range(1, 6):
            nxt = 1 - cur
            for h in H6:
                pb = bk(4 + 2 * lvl, h)
                mm(P, pb[:, 0:128], PkT[h][cur][:], Pk[h][cur][:], [PkT[h][cur], Pk[h][cur]], [pb])
                if lvl < 5:
                    mm(P, pb[:, 128:256], Pk[h][cur][:], PkT[h][cur][:], [PkT[h][cur], Pk[h][cur]], [pb])
                P.act(lambda e, h=h, pb=pb, nxt=nxt: e.copy(Pk[h][nxt][:], pb[:, 0:128]), r=[pb], w=[Pk[h][nxt]])
                if lvl < 5:
                    P.dve(lambda e, h=h, pb=pb, nxt=nxt: e.tensor_copy(PkT[h][nxt][:], pb[:, 128:256]), r=[pb], w=[PkT[h][nxt]])
            for h in H6:
                pb = bk(5 + 2 * lvl, h)
                mm(P, pb[:, 0:128], Pk[h][nxt][:], YT[h][:], [Pk[h][nxt], YT[h]], [pb])
                tt(P, "dve", YT[h][:], YT[h][:], pb[:, 0:128], ALU.add, [YT[h], pb], [YT[h]])
            cur = nxt
        for h in H6:
            pb = bk(0, h)
            mm(P, pb[:, 0:64], YT[h][:], big["vb"][:, hsl[h]], [YT[h], big["vb"]], [pb])
            mm(P, pb[0:64, 128:256], big["kbg"][:, hsl[h]], YT[h][:], [YT[h], big["kbg"]], [pb])
            P.act(lambda e, h=h, pb=pb: e.copy(Us[h][:], pb[:, 0:64]), r=[pb], w=[Us[h]])
            P.dve(lambda e, h=h, pb=pb: e.tensor_copy(WT[h][:], pb[0:64, 128:256]), r=[pb], w=[WT[h]])
        for half in range(2):
            r0 = 64 * half
            rs = slice(r0, r0 + 64)
            for h in H6:
                pb = bk(1, h)
                mm(P, pb[:, 0:64], WT[h][:], S[h][:], [WT[h], S[h]], [pb])
                mm(P, pb[:, 64:128], qdT[h][:], S[h][:], [qdT[h], S[h]], [pb])
                tt(P, "dve", vn[h][rs, :], Us[h][rs, :], pb[rs, 0:64], ALU.subtract, [Us[h], pb], [(vn[h], half)])
                P.act(lambda e, h=h, pb=pb, rs=rs: e.copy(oq[h][rs, :], pb[rs, 64:128]), r=[pb], w=[(oq[h], half)])
            for h in H6:
                pb = bk(2, h)
                mm(P, pb[0:64, 0:64], big["kend"][rs, hsl[h]], vn[h][rs, :], [big["kend"], (vn[h], half)], [pb])
                stt(P, S[h][:], S[h][:], v[0:64, 42 + 6 * half + h:43 + 6 * half + h], pb[0:64, 0:64], ALU.mult, ALU.add, [S[h], v, pb], [S[h]])
        for h in H6:
            pb = bk(3, h)
            mm(P, pb[:, 0:64], qkT[h][:], vn[h][:], [qkT[h], (vn[h], 0), (vn[h], 1)], [pb])
            tt(P, "dve", big["o"][:, hsl[h]], oq[h][:], pb[:, 0:64], ALU.add, [(oq[h], 0), (oq[h], 1), pb], [(big["o"], h, 0), (big["o"], h, 1)])
        ok = [(big["o"], h, k) for h in range(6) for k in range(2)]
        tt(P, "pool", big["sq"][:], big["o"][:], big["o"][:], ALU.mult, ok, [big["sq"]])
        P.dve(lambda e: e.tensor_reduce(v[:, 84:90], v3(big["sq"]), AX.X, ALU.add), r=[big["sq"]], w=[v])
        ts(P, "dve", v[:, 84:90], v[:, 84:90], 1.0 / 64, EPS, ALU.mult, ALU.add, [v], [v])
        actf(P, v[:, 84:90], v[:, 84:90], AF.Sqrt, [v], [v])
        P.dve(lambda e: e.reciprocal(v[:, 90:96], v[:, 84:90]), r=[v], w=[v])
        tt(P, "dve", v3(big["qn"]), v3(big["o"]), hb(90), ALU.mult, ok + [v, big["qn"]], [big["qn"]])
        tt(P, "pool", big["qn"][:], big["qn"][:], bc.t[:, 2, :], ALU.mult, [big["qn"], (bc, 2)], [big["qn"]])
        for i in range(3):
            tr(P, C, ps[1][:, i * 128:(i + 1) * 128], zT[i][:], 128, [zT[i]], [ps[1]])
        actf(P, big["zs"][:], ps[1][:, 0:384], AF.Silu, [ps[1]], [big["zs"]])
        tt(P, "dve", big["qn"][:], big["qn"][:], big["zs"][:], ALU.mult, [big["qn"], big["zs"]], [big["qn"]])
        for i in range(3):
            tr(P, C, ps[2][:, i * 128:(i + 1) * 128], big["qn"][:, i * 128:(i + 1) * 128], 128, [big["qn"]], [ps[2]])
        P.act(lambda e: e.copy(ob[:], ps[2].t[:, 0:384].rearrange("p (k t) -> p k t", k=3)), r=[ps[2]], w=[ob])
        P.dma(mix_d[640:1024, cs].rearrange("(k p) t -> p k t", p=128), ob[:], r=[ob], w=[("mixgdn", blk)])
    if not concurrent:
        P.barrier()
        P.release(m0)


SEQ_FULL = 4096
N_CORES = 8
_LKEYS = None


def build_program(SEQ, layer_shapes):
    nc = bass.Bass("TRN2", target_bir_lowering=False)
    x_d = nc.dram_tensor("x", [SEQ, D], F32, kind="ExternalInput").ap()
    Ws = []
    for l in range(2):
        Ws.append({k: nc.dram_tensor(f"L{l}_{k}", list(shp), F32, kind="ExternalInput").ap() for k, shp in layer_shapes.items()})
    ffg = nc.dram_tensor("ff_g", [1, D, DFF], F32, kind="ExternalInput").ap()
    ffu = nc.dram_tensor("ff_u", [1, D, DFF], F32, kind="ExternalInput").ap()
    ffd = nc.dram_tensor("ff_d", [1, DFF, D], F32, kind="ExternalInput").ap()
    mog = nc.dram_tensor("moe_g", [NE, D, DFF], F32, kind="ExternalInput").ap()
    mou = nc.dram_tensor("moe_u", [NE, D, DFF], F32, kind="ExternalInput").ap()
    mod = nc.dram_tensor("moe_d", [NE, DFF, D], F32, kind="ExternalInput").ap()
    mor = nc.dram_tensor("moe_r", [D, NE], F32, kind="ExternalInput").ap()
    nfin = nc.dram_tensor("nfin", [128, D], F32, kind="ExternalInput").ap()
    out_d = nc.dram_tensor("out", [SEQ, D], F32, kind="ExternalOutput").ap()
    proj_d = nc.dram_tensor("proj_s", [NPROJ, SEQ], F32).ap()
    mix_d = nc.dram_tensor("mix_s", [D, SEQ], BF16).ap()
    xres = nc.dram_tensor("xres_s", [SEQ, D], F32).ap()
    P = Prog(nc)
    C = Ctx()
    setup_common(P, C)
    make_masks(P, C)
    P.barrier()
    for l in range(2):
        W = Ws[l]
        src = x_d if l == 0 else xres
        phase_inproj(P, C, SEQ, src, W["nmix"], W["win"], proj_d)
        mc = P.mark()
        g5 = mixer_s5(P, C, SEQ, proj_d, mix_d, W, concurrent=True, banks=(6, 7))
        next(g5)
        P.stream_begin()
        next(g5)
        st_s5 = P.stream_end()
        P.stream_begin()
        mixer_ssd(P, C, SEQ, proj_d, mix_d, W, concurrent=True)
        st_ssd = P.stream_end()
        P.stream_begin()
        mixer_gdn(P, C, SEQ, proj_d, mix_d, W, concurrent=True)
        st_gdn = P.stream_end()
        P.merge([st_s5, st_ssd, st_gdn])
        P.barrier()
        P.release(mc)
        phase_outproj(P, C, SEQ, src, xres, mix_d, W["wout"])
        if l == 0:
            phase_ffn(P, C, SEQ, xres, xres, W["nffn"], ffg, ffu, ffd, 1)
        else:
            phase_ffn(P, C, SEQ, xres, out_d, W["nffn"], mog, mou, mod, NE, wr_d=mor, nfin_d=nfin)
    finals = [o for e in ENGS for o in P.ops[e] if o.is_dma]
    P.emit(finals[-64:])
    return nc, P


def kernel(**inp):
    inp = {k: np.asarray(v) for k, v in inp.items()}
    x = np.ascontiguousarray(inp["x"], dtype=np.float32)
    B, SEQ, _ = x.shape
    layers = [host_layer_inputs(inp, l) for l in range(2)]
    shapes = {k: v.shape for k, v in layers[0].items()}
    nc, P = build_program(SEQ, shapes)
    f = lambda a: np.ascontiguousarray(np.asarray(a, np.float32))
    common = {}
    for l in range(2):
        for k, v in layers[l].items():
            common[f"L{l}_{k}"] = np.ascontiguousarray(v, dtype=np.float32)
    common["ff_g"] = f(inp["ff_w_gate"])
    common["ff_u"] = f(inp["ff_w_up"])
    common["ff_d"] = f(inp["ff_w_down"])
    common["moe_g"] = f(inp["moe_w_gate"][0])
    common["moe_u"] = f(inp["moe_w_up"][0])
    common["moe_d"] = f(inp["moe_w_down"][0])
    common["moe_r"] = f(inp["moe_router"][0])
    common["nfin"] = rep128(inp["norm_final"])
    in_maps = []
    for c in range(B):
        m = dict(common)
        m["x"] = np.ascontiguousarray(x[c])
        in_maps.append(m)
    res = run_bass_kernel_spmd(nc, in_maps, core_ids=list(range(B)))
    return np.stack([np.asarray(r["out"], dtype=np.float32) for r in res.results], axis=0)
```

```python
import contextlib
import numpy as np
import concourse.bass as bass
import concourse.mybir as mybir
from concourse.bass_utils import run_bass_kernel_spmd

F32 = mybir.dt.float32
BF16 = mybir.dt.bfloat16
I32 = mybir.dt.int32
ALU = mybir.AluOpType
AF = mybir.ActivationFunctionType
AX = mybir.AxisListType

ENGS = ("pe", "act", "dve", "pool", "sp")
NDMASEM = 8


class Op:
    __slots__ = ("eng", "fn", "deps", "sig", "is_dma", "sem", "semval", "barriered")

    def __init__(self, eng, fn, is_dma):
        self.eng = eng
        self.fn = fn
        self.deps = []
        self.sig = None
        self.is_dma = is_dma
        self.sem = None
        self.semval = None
        self.barriered = False


class Tile:
    _n = 0

    def __init__(self, t, name, psum=False):
        self.t = t
        self.name = name
        self.psum = psum
        Tile._n += 1
        self.id = Tile._n

    def __getitem__(self, k):
        return self.t[k]

    def __hash__(self):
        return self.id

    def __eq__(self, o):
        return self is o


class Prog:
    def __init__(self, nc, same_engine_sync=True):
        self.nc = nc
        self.ops = {e: [] for e in ENGS}
        self.lastw = {}
        self.readers = {}
        self.same_engine_sync = same_engine_sync
        self.ndma = {e: 0 for e in ENGS}
        self.dma_last = {}
        self.sb_off = 16640
        self.sb_max = 0
        self.nalloc = 0
        self.cur_stream = None

    def sb(self, name, shape, dtype, align=64):
        nb = int(np.prod(shape[1:])) * mybir.dt.size(dtype)
        off = (self.sb_off + align - 1) // align * align
        self.nalloc += 1
        t = self.nc.alloc_sbuf_tensor_at(f"{name}_{self.nalloc}", list(shape), dtype, offset=off)
        self.sb_off = off + nb
        self.sb_max = max(self.sb_max, self.sb_off)
        assert self.sb_off <= 229376, f"SBUF overflow {self.sb_off} at {name}"
        return Tile(t, name)

    def mark(self):
        return self.sb_off

    def release(self, m):
        self.sb_off = m

    def op(self, eng, fn, r=(), w=(), is_dma=False):
        o = Op(eng, fn, is_dma)
        if any(isinstance(k, Tile) and k.psum for k in r):
            w = list(w) + [k for k in r if isinstance(k, Tile) and k.psum]
            r = [k for k in r if not (isinstance(k, Tile) and k.psum)]
        deps = {}
        for k in r:
            lw = self.lastw.get(k)
            if lw is not None:
                deps[id(lw)] = lw
        for k in w:
            lw = self.lastw.get(k)
            if lw is not None:
                deps[id(lw)] = lw
            for rd in self.readers.get(k, ()):
                deps[id(rd)] = rd
        if is_dma:
            self.ndma[eng] += 1
        for d in deps.values():
            if d is o:
                continue
            if (not d.is_dma) and d.eng == eng:
                if eng == "pe" or not self.same_engine_sync:
                    continue
            o.deps.append(d)
        for k in r:
            lst = self.readers.setdefault(k, [])
            if not is_dma:
                lst[:] = [x for x in lst if x.is_dma or x.eng != eng]
            lst.append(o)
        for k in w:
            self.lastw[k] = o
            self.readers[k] = []
        if self.cur_stream is not None:
            self.cur_stream.append(o)
        else:
            self.ops[eng].append(o)
        return o

    def stream_begin(self):
        assert self.cur_stream is None
        self.cur_stream = []

    def stream_end(self):
        st, self.cur_stream = self.cur_stream, None
        return st

    def merge(self, streams):
        pos = [0] * len(streams)
        tot = [max(1, len(st)) for st in streams]
        while True:
            best = None
            for i, st in enumerate(streams):
                if pos[i] < len(st):
                    f = pos[i] / tot[i]
                    if best is None or f < best[0]:
                        best = (f, i)
            if best is None:
                break
            i = best[1]
            o = streams[i][pos[i]]
            pos[i] += 1
            self.ops[o.eng].append(o)

    def pe(self, fn, r=(), w=()):
        return self.op("pe", fn, r, w)

    def act(self, fn, r=(), w=()):
        return self.op("act", fn, r, w)

    def dve(self, fn, r=(), w=()):
        return self.op("dve", fn, r, w)

    def pool(self, fn, r=(), w=()):
        return self.op("pool", fn, r, w)

    def dma(self, out, in_, r=(), w=(), eng="sp", **kw):
        return self.op(eng, lambda e: e.dma_start(out, in_, **kw), r, w, is_dma=True)

    def barrier(self):
        tails = []
        for e in ENGS:
            if e == "sp":
                continue
            for o in reversed(self.ops[e]):
                if o.fn is not None and not o.is_dma:
                    tails.append(o)
                    break
        dmas = [o for e in ENGS for o in self.ops[e] if o.is_dma and not o.barriered]
        for o in dmas:
            o.barriered = True
        for e in ENGS:
            b = Op(e, None, False)
            b.deps = [t for t in tails if t.eng != e] + list(dmas)
            self.ops[e].append(b)
        self.lastw.clear()
        self.readers.clear()

    def emit(self, final_waits=()):
        nc = self.nc
        fin = Op("sp", None, False)
        fin.deps = list(final_waits)
        self.ops["sp"].append(fin)
        for e in ENGS:
            n = 0
            last = {}
            for o in self.ops[e]:
                if o.is_dma:
                    slot = n % NDMASEM
                    n += 1
                    prev = last.get(slot)
                    if prev is not None and all(d is not prev for d in o.deps):
                        o.deps.append(prev)
                    last[slot] = o
                    o.sem = (e, slot)
        for e in ENGS:
            for o in self.ops[e]:
                for d in o.deps:
                    d.sig = True
        esem = {}
        dsem = {}
        with contextlib.ExitStack() as st:
            for e in ENGS:
                esem[e] = st.enter_context(nc.semaphore(f"s_{e}"))
            for e in ENGS:
                if self.ndma[e]:
                    for s in range(min(NDMASEM, self.ndma[e])):
                        dsem[(e, s)] = st.enter_context(nc.semaphore(f"d_{e}{s}"))
            dcount = {}
            for e in ENGS:
                c = 0
                for o in self.ops[e]:
                    if o.is_dma:
                        n = dcount.get(o.sem, 0) + 16
                        dcount[o.sem] = n
                        o.semval = n
                        o.sig = True
                    elif o.sig:
                        c += 1
                        o.semval = c
                        o.sem = e
                assert c < 60000, (e, c)
            maxd = max(dcount.values()) if dcount else 0
            assert maxd < 60000, maxd
            self.stats = {e: len(self.ops[e]) for e in ENGS}
            block = st.enter_context(nc.Block())
            engmap = {"pe": block.tensor, "act": block.scalar, "dve": block.vector,
                      "pool": block.gpsimd, "sp": block.sync}
            nw = [0]
            for e in ENGS:
                ops = self.ops[e]
                if not ops:
                    continue

                def body(eng, ops=ops, e=e):
                    seen = {}
                    for o in ops:
                        need = {}
                        for d in o.deps:
                            if d.semval is None:
                                continue
                            if seen.get(d.sem, 0) >= d.semval:
                                continue
                            if need.get(d.sem, 0) < d.semval:
                                need[d.sem] = d.semval
                        for s, v in need.items():
                            sh = esem[s] if isinstance(s, str) else dsem[s]
                            eng.wait_ge(sh, v)
                            seen[s] = v
                            nw[0] += 1
                        if o.fn is None:
                            continue
                        ins = o.fn(eng)
                        if o.is_dma:
                            ins.then_inc(dsem[o.sem], 16)
                        elif o.sig:
                            ins.then_inc(esem[e], 1)

                engmap[e](body)
            self.stats["waits"] = nw[0]
        return nc


D = 1024
DFF = 3584
NE = 8
EPS = 1e-6
NPROJ = 3200
KT = 8


class Ctx:
    pass


def setup_common(P, C):
    nc = P.nc
    C.ps = [Tile(nc.alloc_psum_tensor(f"psb{i}", [128, 512], F32), f"ps{i}", psum=True) for i in range(8)]
    C.ID = P.sb("ID", [128, 128], F32)
    C.IDb = P.sb("IDb", [128, 128], BF16)
    P.pool(lambda e: e.memset(C.ID[:], 1.0), w=[C.ID])
    P.pool(lambda e: e.affine_select(C.ID[:], C.ID[:], [[1, 128]], ALU.is_equal, 0.0, base=0,
                                     channel_multiplier=-1), r=[C.ID], w=[C.ID])
    P.dve(lambda e: e.tensor_copy(C.IDb[:], C.ID[:]), r=[C.ID], w=[C.IDb])
    C.ones = P.sb("ones", [128, 128], F32)
    P.pool(lambda e: e.memset(C.ones[:], 1.0), w=[C.ones])


def norm_tile(P, C, xt, nwbc, xn, ss, junk):
    P.act(lambda e: e.activation(junk[:], xt[:], AF.Square, accum_out=ss[:, 0:1]), r=[xt], w=[junk, ss])
    P.dve(lambda e: e.tensor_scalar(ss[:, 1:2], ss[:, 0:1], 1.0 / D, EPS, ALU.mult, ALU.add), r=[ss], w=[ss])
    P.act(lambda e: e.activation(ss[:, 2:3], ss[:, 1:2], AF.Sqrt), r=[ss], w=[ss])
    P.dve(lambda e: e.reciprocal(ss[:, 3:4], ss[:, 2:3]), r=[ss], w=[ss])
    P.dve(lambda e: e.scalar_tensor_tensor(xn[:], xt[:], ss[:, 3:4], nwbc[:], ALU.mult, ALU.mult),
          r=[xt, ss, nwbc], w=[xn])


def transpose_to_T(P, C, xn, banks, hnT, col0, hn32=None):
    for half in range(2):
        bk = banks[half]
        for j in range(4):
            k = half * 4 + j
            P.pe(lambda e, bk=bk, j=j, k=k: e.transpose(bk[:, j * 128:(j + 1) * 128], xn[:, k * 128:(k + 1) * 128], C.ID[:]),
                 r=[xn, C.ID], w=[bk])
        src = bk.t[:, :].rearrange("p (k t) -> p k t", k=4)
        dst = hnT.t[:, half * 4:half * 4 + 4, col0:col0 + 128]
        if half == 0:
            P.act(lambda e, dst=dst, src=src: e.copy(dst, src), r=[bk], w=[(hnT, col0, 0)])
        else:
            P.dve(lambda e, dst=dst, src=src: e.tensor_copy(dst, src), r=[bk], w=[(hnT, col0, 1)])
        if hn32 is not None:
            d32 = hn32.t[:, half * 4:half * 4 + 4, :]
            if half == 0:
                P.dve(lambda e, d32=d32, src=src: e.tensor_copy(d32, src), r=[bk], w=[(hn32, half)])
            else:
                P.act(lambda e, d32=d32, src=src: e.copy(d32, src), r=[bk], w=[(hn32, half)])


def phase_ffn(P, C, SEQ, xsrc, xdst, nw_d, wg_d, wu_d, wd_d, n_exp, wr_d=None, nfin_d=None, T=1024):
    m0 = P.mark()
    T = min(T, SEQ)
    NTB = T // 512
    moe = wr_d is not None
    FG = 512
    NFG = DFF // FG
    ps = C.ps
    nwbc = P.sb("nwbc", [128, D], F32)
    P.dma(nwbc[:], nw_d, w=[nwbc])
    if nfin_d is not None:
        nfbc = P.sb("nfbc", [128, D], F32)
        P.dma(nfbc[:], nfin_d, w=[nfbc])
    hnT = P.sb("hnT", [128, KT, T], BF16)
    acc = P.sb("acc", [128, KT, T], F32)
    hT = [P.sb(f"hT{i}", [128, 4, T], BF16) for i in range(2)]
    wg = [P.sb(f"wg{i}", [128, KT, FG], BF16) for i in range(2)]
    wu = [P.sb(f"wu{i}", [128, KT, FG], BF16) for i in range(2)]
    wd = [P.sb(f"wd{i}", [128, 4, D], BF16) for i in range(2)]
    sg = [P.sb(f"sg{i}", [128, 512], BF16) for i in range(2)]
    xt = [P.sb(f"xt{i}", [128, D], F32) for i in range(2)]
    xn = [P.sb(f"xn{i}", [128, D], F32) for i in range(2)]
    junk = P.sb("junk", [128, D], F32)
    ssb = [P.sb(f"ss{i}", [128, 8], F32) for i in range(2)]
    if moe:
        wr = P.sb("wr", [128, KT, NE], F32)
        P.dma(wr[:], wr_d.rearrange("(k p) e -> p k e", p=128), w=[wr])
        hn32 = [P.sb(f"hn32{i}", [128, KT, 128], F32) for i in range(2)]
        cbc = P.sb("cbc", [128, NE, T], BF16)
        rt = [P.sb(f"rt{i}", [128, 64], F32) for i in range(2)]
    nsb = SEQ // T
    wcount = 0
    for sbi in range(nsb):
        t0 = sbi * T
        for tt in range(T // 128):
            b = tt % 2
            P.dma(xt[b][:], xsrc[t0 + tt * 128:t0 + (tt + 1) * 128, :], w=[xt[b]])
            norm_tile(P, C, xt[b], nwbc, xn[b], ssb[b], junk)
            banks = (ps[4 + 2 * b], ps[5 + 2 * b])
            transpose_to_T(P, C, xn[b], banks, hnT, tt * 128, hn32[b] if moe else None)
            if moe:
                lg = ps[0] if b == 0 else ps[1]
                R = rt[b]
                for k in range(KT):
                    P.pe(lambda e, k=k, lg=lg, b=b: e.matmul(lg[:, 0:NE], hn32[b][:, k, :], wr[:, k, :], start=(k == 0), stop=(k == KT - 1)),
                         r=[(hn32[b], k // 4), wr], w=[lg])
                P.dve(lambda e, R=R, lg=lg: e.tensor_copy(R[:, 0:8], lg[:, 0:8]), r=[lg], w=[R])
                P.dve(lambda e, R=R: e.tensor_reduce(R[:, 8:9], R[:, 0:8], AX.X, ALU.max), r=[R], w=[R])
                P.dve(lambda e, R=R: e.tensor_scalar(R[:, 9:17], R[:, 0:8], R[:, 8:9], None, ALU.is_equal), r=[R], w=[R])
                P.dve(lambda e, R=R: e.scalar_tensor_tensor(R[:, 17:25], R[:, 9:17], -1e30, R[:, 0:8], ALU.mult, ALU.add), r=[R], w=[R])
                P.dve(lambda e, R=R: e.tensor_reduce(R[:, 25:26], R[:, 17:25], AX.X, ALU.max), r=[R], w=[R])
                P.dve(lambda e, R=R: e.tensor_scalar(R[:, 26:34], R[:, 17:25], R[:, 25:26], None, ALU.is_equal), r=[R], w=[R])
                P.dve(lambda e, R=R: e.tensor_tensor(R[:, 34:35], R[:, 25:26], R[:, 8:9], ALU.subtract), r=[R], w=[R])
                P.act(lambda e, R=R: e.activation(R[:, 35:36], R[:, 34:35], AF.Exp), r=[R], w=[R])
                P.dve(lambda e, R=R: e.tensor_scalar(R[:, 36:37], R[:, 35:36], 1.0, None, ALU.add), r=[R], w=[R])
                P.dve(lambda e, R=R: e.reciprocal(R[:, 37:38], R[:, 36:37]), r=[R], w=[R])
                P.dve(lambda e, R=R: e.tensor_tensor(R[:, 38:39], R[:, 35:36], R[:, 37:38], ALU.mult), r=[R], w=[R])
                P.dve(lambda e, R=R: e.tensor_scalar(R[:, 40:48], R[:, 9:17], R[:, 37:38], None, ALU.mult), r=[R], w=[R])
                P.dve(lambda e, R=R: e.scalar_tensor_tensor(R[:, 40:48], R[:, 26:34], R[:, 38:39], R[:, 40:48], ALU.mult, ALU.add), r=[R], w=[R])
                bb = ps[2] if b == 0 else ps[3]
                for half in range(2):
                    for j in range(4):
                        ex = half * 4 + j
                        P.pe(lambda e, ex=ex, j=j, R=R, bb=bb: e.matmul(bb[:, j * 128:(j + 1) * 128], R[:, 40 + ex:41 + ex].broadcast_to([128, 128]), C.ID[:], start=True, stop=True),
                             r=[R, C.ID], w=[bb])
                    src = bb.t[:, :].rearrange("p (k t) -> p k t", k=4)
                    dst = cbc.t[:, half * 4:half * 4 + 4, tt * 128:(tt + 1) * 128]
                    P.act(lambda e, dst=dst, src=src: e.copy(dst, src), r=[bb], w=[(cbc, tt)])
        nfg_total = n_exp * NFG

        def load_w(g, wb):
            ex, fg = divmod(g, NFG)
            f0 = fg * FG
            P.dma(wg[wb][:], wg_d[ex, :, f0:f0 + FG].rearrange("(k p) f -> p k f", p=128), w=[wg[wb]], eng="pool")
            P.dma(wu[wb][:], wu_d[ex, :, f0:f0 + FG].rearrange("(k p) f -> p k f", p=128), w=[wu[wb]], eng="pool")
            P.dma(wd[wb][:], wd_d[ex, f0:f0 + FG, :].rearrange("(c p) d -> p c d", p=128), w=[wd[wb]], eng="pool")
        for ex in range(n_exp):
            for fg in range(NFG):
                gi = ex * NFG + fg
                wb = wcount % 2
                wcount += 1
                f0 = fg * FG
                if gi == 0:
                    load_w(0, wb)
                if gi + 1 < nfg_total:
                    load_w(gi + 1, 1 - wb)
                hb = hT[wb]
                for fc in range(4):
                    for tb in range(NTB):
                        pg = ps[0 + (fc * NTB + tb) % 2]
                        pu = ps[2 + (fc * NTB + tb) % 2]
                        s = sg[(fc * NTB + tb) % 2]
                        rd = [(hnT, c * 128, h2) for c in range(tb * 4, tb * 4 + 4) for h2 in range(2)]
                        for k in range(KT):
                            P.pe(lambda e, k=k, pg=pg, wb=wb, fc=fc, tb=tb: e.matmul(pg[:], wg[wb][:, k, fc * 128:(fc + 1) * 128], hnT[:, k, tb * 512:(tb + 1) * 512], start=(k == 0), stop=(k == KT - 1)),
                                 r=[wg[wb]] + rd, w=[pg])
                        for k in range(KT):
                            P.pe(lambda e, k=k, pu=pu, wb=wb, fc=fc, tb=tb: e.matmul(pu[:], wu[wb][:, k, fc * 128:(fc + 1) * 128], hnT[:, k, tb * 512:(tb + 1) * 512], start=(k == 0), stop=(k == KT - 1)),
                                 r=[wu[wb]] + rd, w=[pu])
                        P.act(lambda e, s=s, pg=pg: e.activation(s[:], pg[:], AF.Silu), r=[pg], w=[s])
                        hdst = hb.t[:, fc, tb * 512:(tb + 1) * 512]
                        P.dve(lambda e, hdst=hdst, s=s, pu=pu: e.tensor_tensor(hdst, s[:], pu[:], ALU.mult), r=[s, pu], w=[(hb, fc, tb)])
                        if moe:
                            csrc = cbc.t[:, ex, tb * 512:(tb + 1) * 512]
                            P.dve(lambda e, hdst=hdst, csrc=csrc: e.tensor_tensor(hdst, hdst, csrc, ALU.mult),
                                   r=[(hb, fc, tb)] + [(cbc, c) for c in range(tb * 4, tb * 4 + 4)], w=[(hb, fc, tb)])
                for dc in range(KT):
                    for tb in range(NTB):
                        pd = ps[4 + (dc * NTB + tb) % 4]
                        for fc in range(4):
                            P.pe(lambda e, fc=fc, pd=pd, wb=wb, dc=dc, tb=tb, hb=hb: e.matmul(pd[:], wd[wb][:, fc, dc * 128:(dc + 1) * 128], hb[:, fc, tb * 512:(tb + 1) * 512], start=(fc == 0), stop=(fc == 3)),
                                 r=[wd[wb], (hb, fc, tb)], w=[pd])
                        adst = acc.t[:, dc, tb * 512:(tb + 1) * 512]
                        if gi == 0:
                            P.act(lambda e, adst=adst, pd=pd: e.copy(adst, pd[:]), r=[pd], w=[(acc, dc, tb)])
                        else:
                            P.dve(lambda e, adst=adst, pd=pd: e.tensor_tensor(adst, adst, pd[:], ALU.add), r=[pd, (acc, dc, tb)], w=[(acc, dc, tb)])
        for tt in range(T // 128):
            b = tt % 2
            tb = tt // 4
            P.dma(xt[b][:], xsrc[t0 + tt * 128:t0 + (tt + 1) * 128, :], w=[xt[b]])
            banks = (ps[0 + 2 * b], ps[1 + 2 * b])
            for half in range(2):
                bk = banks[half]
                for j in range(4):
                    k = half * 4 + j
                    P.pe(lambda e, bk=bk, j=j, k=k, tt=tt: e.transpose(bk[:, j * 128:(j + 1) * 128], acc[:, k, tt * 128:(tt + 1) * 128], C.ID[:]),
                         r=[(acc, k, tb), C.ID], w=[bk])
                P.dve(lambda e, bk=bk, half=half, b=b: e.tensor_tensor(xn[b][:, half * 512:(half + 1) * 512], xt[b][:, half * 512:(half + 1) * 512], bk[:], ALU.add),
                      r=[bk, xt[b]], w=[xn[b]] if half == 0 else [xn[b]])
            if nfin_d is not None:
                norm_tile(P, C, xn[b], nfbc, xt[b], ssb[b], junk)
                P.dma(xdst[t0 + tt * 128:t0 + (tt + 1) * 128, :], xt[b][:], r=[xt[b]], w=[("xdst", id(xdst), t0 + tt * 128)])
            else:
                P.dma(xdst[t0 + tt * 128:t0 + (tt + 1) * 128, :], xn[b][:], r=[xn[b]], w=[("xdst", id(xdst), t0 + tt * 128)])
    P.barrier()
    P.release(m0)


def phase_inproj(P, C, SEQ, xsrc, nw_d, win_d, proj_d):
    m0 = P.mark()
    ps = C.ps
    nwbc = P.sb("nwbc", [128, D], F32)
    P.dma(nwbc[:], nw_d, w=[nwbc])
    W = P.sb("Win", [128, KT, NPROJ], BF16)
    wv = win_d.rearrange("(k p) n -> p k n", p=128)
    for k in range(KT):
        for h in range(2):
            P.dma(W.t[:, k, h * 1600:(h + 1) * 1600], wv[:, k, h * 1600:(h + 1) * 1600], w=[(W, k, h)], eng="pool")
    wkeys = [(W, k, h) for k in range(KT) for h in range(2)]
    hnT = P.sb("hnT", [128, KT, SEQ], BF16)
    xt = [P.sb(f"xt{i}", [128, D], F32) for i in range(2)]
    xn = [P.sb(f"xn{i}", [128, D], F32) for i in range(2)]
    junk = P.sb("junk", [128, D], F32)
    ssb = [P.sb(f"ss{i}", [128, 8], F32) for i in range(2)]
    stg = [P.sb(f"stg{i}", [128, 512], F32) for i in range(4)]
    for tt in range(SEQ // 128):
        b = tt % 2
        P.dma(xt[b][:], xsrc[tt * 128:(tt + 1) * 128, :], w=[xt[b]])
        norm_tile(P, C, xt[b], nwbc, xn[b], ssb[b], junk)
        transpose_to_T(P, C, xn[b], (ps[4 + 2 * b], ps[5 + 2 * b]), hnT, tt * 128)
    n = 0
    for tb in range(SEQ // 512):
        rd = [(hnT, c * 128, h2) for c in range(tb * 4, tb * 4 + 4) for h2 in range(2)]
        for mc in range(NPROJ // 128):
            pb = ps[n % 4]
            s = stg[n % 4]
            for k in range(KT):
                P.pe(lambda e, k=k, pb=pb, mc=mc, tb=tb: e.matmul(pb[:], W[:, k, mc * 128:(mc + 1) * 128], hnT[:, k, tb * 512:(tb + 1) * 512], start=(k == 0), stop=(k == KT - 1)),
                     r=wkeys + rd, w=[pb])
            if n % 2 == 0:
                P.act(lambda e, s=s, pb=pb: e.copy(s[:], pb[:]), r=[pb], w=[s])
            else:
                P.dve(lambda e, s=s, pb=pb: e.tensor_copy(s[:], pb[:]), r=[pb], w=[s])
            P.dma(proj_d[mc * 128:(mc + 1) * 128, tb * 512:(tb + 1) * 512], s[:], r=[s], w=[("proj", mc, tb)])
            n += 1
    P.barrier()
    P.release(m0)


def phase_outproj(P, C, SEQ, xsrc, xdst, mix_d, wout_d):
    m0 = P.mark()
    ps = C.ps
    Wo = P.sb("Wo", [128, KT, D], BF16)
    P.dma(Wo[:], wout_d.rearrange("(k p) n -> p k n", p=128), w=[Wo], eng="pool")
    mT = P.sb("mT", [128, KT, SEQ], BF16)
    mv = mix_d.rearrange("(k p) t -> p k t", p=128)
    for k in range(KT):
        P.dma(mT.t[:, k, :], mv[:, k, :], w=[(mT, k)])
    mk = [(mT, k) for k in range(KT)]
    xt = [P.sb(f"xt{i}", [128, D], F32) for i in range(2)]
    xn = [P.sb(f"xn{i}", [128, D], F32) for i in range(2)]
    for tt in range(SEQ // 128):
        b = tt % 2
        P.dma(xt[b][:], xsrc[tt * 128:(tt + 1) * 128, :], w=[xt[b]])
        for half in range(2):
            pb = ps[(tt * 2 + half) % 4]
            for k in range(KT):
                P.pe(lambda e, k=k, pb=pb, half=half, tt=tt: e.matmul(pb[:], mT[:, k, tt * 128:(tt + 1) * 128], Wo[:, k, half * 512:(half + 1) * 512], start=(k == 0), stop=(k == KT - 1)),
                     r=mk + [Wo], w=[pb])
            P.dve(lambda e, pb=pb, half=half, b=b: e.tensor_tensor(xn[b][:, half * 512:(half + 1) * 512], xt[b][:, half * 512:(half + 1) * 512], pb[:], ALU.add),
                  r=[pb, xt[b]], w=[xn[b]])
        P.dma(xdst[tt * 128:(tt + 1) * 128, :], xn[b][:], r=[xn[b]], w=[("xo", tt)])
    P.barrier()
    P.release(m0)


TWO_PI = 6.283185307179586
CW1 = 6.28125
CW2 = TWO_PI - CW1
RMAGIC = 12582912.0
PI_SAFE = 3.1415925


def sincos(P, x, sin_o, cos_o, kt, ks, tmp, N):
    xk, sk, ck, kk, tk = ks
    for phase, o, ok in ((0.0, sin_o, sk), (0.25, cos_o, ck)):
        P.dve(lambda e, phase=phase: e.tensor_scalar(kt, x, 1.0 / TWO_PI, phase, ALU.mult, ALU.add), r=[xk], w=[kk])
        P.dve(lambda e: e.tensor_scalar(kt, kt, RMAGIC, RMAGIC, ALU.add, ALU.subtract), r=[kk], w=[kk])
        P.dve(lambda e: e.scalar_tensor_tensor(tmp, kt, -CW1, x, ALU.mult, ALU.add), r=[kk, xk], w=[tk])
        P.dve(lambda e: e.scalar_tensor_tensor(tmp, kt, -CW2, tmp, ALU.mult, ALU.add), r=[kk, tk], w=[tk])
        if phase:
            P.dve(lambda e: e.tensor_scalar(tmp, tmp, 0.25 * TWO_PI, PI_SAFE, ALU.add, ALU.min), r=[tk], w=[tk])
        else:
            P.dve(lambda e: e.tensor_scalar(tmp, tmp, PI_SAFE, None, ALU.min), r=[tk], w=[tk])
        P.dve(lambda e: e.tensor_scalar(tmp, tmp, -PI_SAFE, None, ALU.max), r=[tk], w=[tk])
        P.act(lambda e, o=o: e.activation(o, tmp, AF.Sin), r=[tk], w=[ok])


def mixer_s5(P, C, SEQ, proj_d, mix_d, W, concurrent=False, banks=None):
    m0 = P.mark()
    ps = C.ps
    Q = 256 if concurrent else 512
    NCH = SEQ // Q
    prm = P.sb("s5prm", [128, 12, 8], F32)
    for i, nm in enumerate(("a_re_s", "a_im_s", "ls_s")):
        P.dma(prm.t[:, i, :], W[nm], w=[(prm, i)])
    pk = lambda i: (prm, i)
    P.act(lambda e: e.activation(prm.t[:, 3, :], prm.t[:, 2, :], AF.Exp), r=[pk(2)], w=[pk(3)])
    P.dve(lambda e: e.tensor_tensor(prm.t[:, 4, :], prm.t[:, 0, :], prm.t[:, 3, :], ALU.mult), r=[pk(0), pk(3)], w=[pk(4)])
    P.act(lambda e: e.activation(prm.t[:, 4, :], prm.t[:, 4, :], AF.Exp), r=[pk(4)], w=[pk(4)])
    P.dve(lambda e: e.tensor_tensor(prm.t[:, 5, :], prm.t[:, 1, :], prm.t[:, 3, :], ALU.mult), r=[pk(1), pk(3)], w=[pk(5)])
    QT = Q + 1
    cosT = P.sb("cosT", [128, 8, QT], F32)
    sinT = P.sb("sinT", [128, 8, QT], F32)
    Bb_re = P.sb("Bb_re", [128, 1024], BF16)
    Bb_im = P.sb("Bb_im", [128, 1024], BF16)
    Wc_re = P.sb("Wc_re", [128, 1024], F32)
    Wc_im = P.sb("Wc_im", [128, 1024], F32)
    wglu = P.sb("wglu", [128, 2, 256], BF16)
    cols = P.sb("s5cols", [128, 4], F32)
    mscr = P.mark()
    io = P.sb("iota", [128, QT], F32)
    P.pool(lambda e: e.iota(io[:], [[1, QT]], base=0, channel_multiplier=0, allow_small_or_imprecise_dtypes=True), w=[io])
    xa = P.sb("xa", [128, QT], F32)
    kt_ = P.sb("kts", [128, QT], F32)
    tp_ = P.sb("tps", [128, QT], F32)
    for j in range(8):
        P.dve(lambda e, j=j: e.tensor_scalar(xa[:], io[:], prm.t[:, 5, j:j + 1], None, ALU.mult), r=[io, pk(5)], w=[xa])
        sincos(P, xa[:], sinT.t[:, j, :], cosT.t[:, j, :], kt_[:], (xa, (sinT, j), (cosT, j), kt_, tp_), tp_[:], QT)
    m1 = P.mark()
    rw = [P.sb(f"s5rw{i}", [128, 1024], F32) for i in range(10)]
    P.dma(rw[0][:], W["a_re_r"], w=[rw[0]])
    P.dma(rw[1][:], W["a_im_r"], w=[rw[1]])
    P.dma(rw[2][:], W["ls_r"], w=[rw[2]])
    P.act(lambda e: e.activation(rw[2][:], rw[2][:], AF.Exp), r=[rw[2]], w=[rw[2]])
    P.dve(lambda e: e.tensor_tensor(rw[3][:], rw[0][:], rw[2][:], ALU.mult), r=[rw[0], rw[2]], w=[rw[3]])
    P.act(lambda e: e.activation(rw[3][:], rw[3][:], AF.Exp), r=[rw[3]], w=[rw[3]])
    P.dve(lambda e: e.tensor_tensor(rw[4][:], rw[1][:], rw[2][:], ALU.mult), r=[rw[1], rw[2]], w=[rw[4]])
    sincos(P, rw[4][:], rw[5][:], rw[6][:], rw[7][:], (rw[4], rw[5], rw[6], rw[7], rw[8]), rw[8][:], 1024)
    P.dve(lambda e: e.tensor_tensor(rw[6][:], rw[6][:], rw[3][:], ALU.mult), r=[rw[6], rw[3]], w=[rw[6]])
    P.dve(lambda e: e.tensor_scalar(rw[6][:], rw[6][:], -1.0, None, ALU.add), r=[rw[6]], w=[rw[6]])
    P.dve(lambda e: e.tensor_tensor(rw[5][:], rw[5][:], rw[3][:], ALU.mult), r=[rw[5], rw[3]], w=[rw[5]])
    P.dve(lambda e: e.tensor_tensor(rw[9][:], rw[0][:], rw[0][:], ALU.mult), r=[rw[0]], w=[rw[9]])
    P.dve(lambda e: e.tensor_tensor(rw[7][:], rw[1][:], rw[1][:], ALU.mult), r=[rw[1]], w=[rw[7]])
    P.dve(lambda e: e.tensor_tensor(rw[9][:], rw[9][:], rw[7][:], ALU.add), r=[rw[9], rw[7]], w=[rw[9]])
    P.dve(lambda e: e.reciprocal(rw[9][:], rw[9][:]), r=[rw[9]], w=[rw[9]])
    P.dve(lambda e: e.tensor_tensor(rw[7][:], rw[6][:], rw[0][:], ALU.mult), r=[rw[6], rw[0]], w=[rw[7]])
    P.dve(lambda e: e.tensor_tensor(rw[8][:], rw[5][:], rw[1][:], ALU.mult), r=[rw[5], rw[1]], w=[rw[8]])
    P.dve(lambda e: e.tensor_tensor(rw[7][:], rw[7][:], rw[8][:], ALU.add), r=[rw[7], rw[8]], w=[rw[7]])
    P.dve(lambda e: e.tensor_tensor(rw[7][:], rw[7][:], rw[9][:], ALU.mult), r=[rw[7], rw[9]], w=[rw[7]])
    P.dve(lambda e: e.tensor_tensor(rw[8][:], rw[5][:], rw[0][:], ALU.mult), r=[rw[5], rw[0]], w=[rw[8]])
    P.dve(lambda e: e.tensor_tensor(rw[4][:], rw[6][:], rw[1][:], ALU.mult), r=[rw[6], rw[1]], w=[rw[4]])
    P.dve(lambda e: e.tensor_tensor(rw[8][:], rw[8][:], rw[4][:], ALU.subtract), r=[rw[8], rw[4]], w=[rw[8]])
    P.dve(lambda e: e.tensor_tensor(rw[8][:], rw[8][:], rw[9][:], ALU.mult), r=[rw[8], rw[9]], w=[rw[8]])
    P.dma(rw[0][:], W["wb_re"], w=[rw[0]])
    P.dma(rw[1][:], W["wb_im"], w=[rw[1]])
    P.dve(lambda e: e.tensor_tensor(rw[2][:], rw[7][:], rw[0][:], ALU.mult), r=[rw[7], rw[0]], w=[rw[2]])
    P.dve(lambda e: e.tensor_tensor(rw[3][:], rw[8][:], rw[1][:], ALU.mult), r=[rw[8], rw[1]], w=[rw[3]])
    P.dve(lambda e: e.tensor_tensor(Bb_re[:], rw[2][:], rw[3][:], ALU.subtract), r=[rw[2], rw[3]], w=[Bb_re])
    P.dve(lambda e: e.tensor_tensor(rw[2][:], rw[7][:], rw[1][:], ALU.mult), r=[rw[7], rw[1]], w=[rw[2]])
    P.dve(lambda e: e.tensor_tensor(rw[3][:], rw[8][:], rw[0][:], ALU.mult), r=[rw[8], rw[0]], w=[rw[3]])
    P.dve(lambda e: e.tensor_tensor(Bb_im[:], rw[2][:], rw[3][:], ALU.add), r=[rw[2], rw[3]], w=[Bb_im])
    P.dma(rw[4][:], W["wc_re"], w=[rw[4]])
    P.dma(rw[5][:], W["wc_im"], w=[rw[5]])
    P.act(lambda e: e.copy(Wc_re[:], rw[4][:]), r=[rw[4]], w=[Wc_re])
    P.act(lambda e: e.mul(Wc_im[:], rw[5][:], -1.0), r=[rw[5]], w=[Wc_im])
    P.dma(wglu[:], W["wglu"].rearrange("(k p) n -> p k n", p=128), w=[wglu], eng="pool")
    P.dma(cols[:, 0:2], W["dcol"], w=[(cols, 0)])
    P.dma(cols[:, 2:4], W["ncol"], w=[(cols, 1)])
    P.barrier()
    P.release(mscr)
    yield
    if concurrent:
        bx, by = ps[banks[0]], ps[banks[1]]
    u_bf = P.sb("u_bf", [128, 2, Q], BF16)
    uv = proj_d[0:256, :].rearrange("(k p) t -> p k t", p=128)
    ini = P.sb("ini", [128, 8, 2], F32)
    P.dve(lambda e: e.memset(ini[:], 0.0), w=[ini])
    ini2 = P.sb("ini2", [128, 8, 4], F32)
    t = [P.sb(f"s5t{i}", [128, Q], F32) for i in range(8)]
    Wr = [P.sb(f"Wr{i}", [128, Q], F32) for i in range(2)]
    Wi = [P.sb(f"Wi{i}", [128, Q], F32) for i in range(2)]
    S_re = P.sb("S_re", [128, 8, Q], F32)
    S_im = P.sb("S_im", [128, 8, Q], F32)
    u32 = P.sb("u32", [128, 2, Q], F32)
    pt = [P.sb(f"s5p{i}", [128, Q], F32) for i in range(4)]
    gl = P.sb("s5gl", [128, 2, Q], BF16)
    y2 = P.sb("s5y2", [128, 2, Q], F32)
    sq = P.sb("s5sq", [128, 2, Q], F32)
    ob = P.sb("s5ob", [128, 2, Q], BF16)
    for c in range(NCH):
        c0 = c * Q
        ukeys = [u_bf]
        P.dma(u32[:], uv[:, :, c0:c0 + Q], w=[u32])
        P.act(lambda e: e.copy(u_bf[:], u32[:]), r=[u32], w=[u_bf])
        for j in range(8):
            b = j % 2
            ut = j // 4
            pa, pb_ = (bx, by) if concurrent else (ps[0 + b], ps[2 + b])
            P.pe(lambda e, j=j, pa=pa, ut=ut, c0=c0: e.matmul(pa[:, 0:Q], Bb_re[:, j * 128:(j + 1) * 128], u_bf[:, ut, :], start=True, stop=True), r=[Bb_re] + ukeys, w=[pa])
            P.pe(lambda e, j=j, pb_=pb_, ut=ut, c0=c0: e.matmul(pb_[:, 0:Q], Bb_im[:, j * 128:(j + 1) * 128], u_bf[:, ut, :], start=True, stop=True), r=[Bb_im] + ukeys, w=[pb_])
            cs, sn = cosT.t[:, j, 0:Q], sinT.t[:, j, 0:Q]
            T0, T1, T2, T3 = t[4 * b:4 * b + 4]
            P.dve(lambda e, T0=T0, pa=pa, cs=cs: e.tensor_tensor(T0[:], pa[:, 0:Q], cs, ALU.mult), r=[pa, (cosT, j)], w=[T0])
            P.dve(lambda e, T1=T1, pb_=pb_, sn=sn: e.tensor_tensor(T1[:], pb_[:, 0:Q], sn, ALU.mult), r=[pb_, (sinT, j)], w=[T1])
            P.dve(lambda e, T2=T2, pb_=pb_, cs=cs: e.tensor_tensor(T2[:], pb_[:, 0:Q], cs, ALU.mult), r=[pb_, (cosT, j)], w=[T2])
            P.dve(lambda e, T3=T3, pa=pa, sn=sn: e.tensor_tensor(T3[:], pa[:, 0:Q], sn, ALU.mult), r=[pa, (sinT, j)], w=[T3])
            P.pool(lambda e, T0=T0, T1=T1: e.tensor_tensor(T0[:], T0[:], T1[:], ALU.add), r=[T0, T1], w=[T0])
            P.pool(lambda e, T2=T2, T3=T3: e.tensor_tensor(T2[:], T2[:], T3[:], ALU.subtract), r=[T2, T3], w=[T2])
            rmag = prm.t[:, 4, j:j + 1].broadcast_to([128, Q])
            P.dve(lambda e, b=b, T0=T0, rmag=rmag, j=j: e.tensor_tensor_scan(Wr[b][:], rmag, T0[:], ini.t[:, j, 0:1], ALU.mult, ALU.add), r=[T0, pk(4), ini], w=[Wr[b]])
            P.dve(lambda e, b=b, T2=T2, rmag=rmag, j=j: e.tensor_tensor_scan(Wi[b][:], rmag, T2[:], ini.t[:, j, 1:2], ALU.mult, ALU.add), r=[T2, pk(4), ini], w=[Wi[b]])
            P.pool(lambda e, T0=T0, b=b, cs=cs: e.tensor_tensor(T0[:], Wr[b][:], cs, ALU.mult), r=[Wr[b], (cosT, j)], w=[T0])
            P.pool(lambda e, T1=T1, b=b, sn=sn: e.tensor_tensor(T1[:], Wi[b][:], sn, ALU.mult), r=[Wi[b], (sinT, j)], w=[T1])
            P.pool(lambda e, T0=T0, T1=T1, j=j: e.tensor_tensor(S_re.t[:, j, :], T0[:], T1[:], ALU.subtract), r=[T0, T1], w=[(S_re, j)])
            P.pool(lambda e, T2=T2, b=b, sn=sn: e.tensor_tensor(T2[:], Wr[b][:], sn, ALU.mult), r=[Wr[b], (sinT, j)], w=[T2])
            P.pool(lambda e, T3=T3, b=b, cs=cs: e.tensor_tensor(T3[:], Wi[b][:], cs, ALU.mult), r=[Wi[b], (cosT, j)], w=[T3])
            P.pool(lambda e, T2=T2, T3=T3, j=j: e.tensor_tensor(S_im.t[:, j, :], T2[:], T3[:], ALU.add), r=[T2, T3], w=[(S_im, j)])
            cq, sq_ = cosT.t[:, j, Q:Q + 1], sinT.t[:, j, Q:Q + 1]
            P.dve(lambda e, b=b, j=j, cq=cq: e.tensor_tensor(ini2.t[:, j, 0:1], Wr[b][:, Q - 1:Q], cq, ALU.mult), r=[Wr[b], (cosT, j)], w=[ini2])
            P.dve(lambda e, b=b, j=j, sq_=sq_: e.tensor_tensor(ini2.t[:, j, 1:2], Wi[b][:, Q - 1:Q], sq_, ALU.mult), r=[Wi[b], (sinT, j)], w=[ini2])
            P.dve(lambda e, b=b, j=j, sq_=sq_: e.tensor_tensor(ini2.t[:, j, 2:3], Wr[b][:, Q - 1:Q], sq_, ALU.mult), r=[Wr[b], (sinT, j)], w=[ini2])
            P.dve(lambda e, b=b, j=j, cq=cq: e.tensor_tensor(ini2.t[:, j, 3:4], Wi[b][:, Q - 1:Q], cq, ALU.mult), r=[Wi[b], (cosT, j)], w=[ini2])
            P.dve(lambda e, j=j: e.tensor_tensor(ini.t[:, j, 0:1], ini2.t[:, j, 0:1], ini2.t[:, j, 1:2], ALU.subtract), r=[ini2], w=[ini])
            P.dve(lambda e, j=j: e.tensor_tensor(ini.t[:, j, 1:2], ini2.t[:, j, 2:3], ini2.t[:, j, 3:4], ALU.add), r=[ini2], w=[ini])
        for ut in range(2):
            py = (bx, by)[ut] if concurrent else ps[4 + ut]
            n = 0
            for j in range(4 * ut, 4 * ut + 4):
                P.pe(lambda e, j=j, py=py, n=n: e.matmul(py[:, 0:Q], Wc_re[:, j * 128:(j + 1) * 128], S_re[:, j, :], start=(n == 0), stop=False), r=[Wc_re, (S_re, j)], w=[py])
                n += 1
                P.pe(lambda e, j=j, py=py, n=n: e.matmul(py[:, 0:Q], Wc_im[:, j * 128:(j + 1) * 128], S_im[:, j, :], start=False, stop=(n == 7)), r=[Wc_im, (S_im, j)], w=[py])
                n += 1
            Y, A1, A2, A3 = pt
            P.dve(lambda e, ut=ut, py=py: e.scalar_tensor_tensor(Y[:], u32[:, ut, :], cols[:, ut:ut + 1], py[:, 0:Q], ALU.mult, ALU.add), r=[u32, (cols, 0), py], w=[Y])
            P.pool(lambda e: e.tensor_tensor(A1[:], Y[:], Y[:], ALU.mult), r=[Y], w=[A1])
            P.pool(lambda e: e.tensor_scalar(A1[:], A1[:], 0.044715, 1.0, ALU.mult, ALU.add), r=[A1], w=[A1])
            P.pool(lambda e: e.tensor_tensor(A1[:], A1[:], Y[:], ALU.mult), r=[A1, Y], w=[A1])
            P.act(lambda e: e.activation(A2[:], A1[:], AF.Sigmoid, scale=1.5957691216057308), r=[A1], w=[A2])
            P.dve(lambda e, ut=ut: e.tensor_tensor(y2.t[:, ut, :], Y[:], A2[:], ALU.mult), r=[Y, A2], w=[(y2, ut)])
            P.act(lambda e, ut=ut: e.copy(gl.t[:, ut, :], y2.t[:, ut, :]), r=[(y2, ut)], w=[(gl, ut)])
        for mo in range(2):
            pz = (bx, by)[mo] if concurrent else ps[6 + mo]
            for k in range(2):
                P.pe(lambda e, k=k, mo=mo, pz=pz: e.matmul(pz[:, 0:Q], wglu[:, k, mo * 128:(mo + 1) * 128], gl[:, k, :], start=(k == 0), stop=(k == 1)), r=[wglu, (gl, 0), (gl, 1)], w=[pz])
            A1 = pt[1]
            P.act(lambda e, pz=pz: e.activation(A1[:], pz[:, 0:Q], AF.Sigmoid), r=[pz], w=[A1])
            P.dve(lambda e, mo=mo: e.tensor_tensor(y2.t[:, mo, :], y2.t[:, mo, :], A1[:], ALU.mult), r=[(y2, mo), A1], w=[(y2, mo)])
            P.pool(lambda e, mo=mo: e.tensor_tensor(sq.t[:, mo, :], y2.t[:, mo, :], y2.t[:, mo, :], ALU.mult), r=[(y2, mo)], w=[(sq, mo)])
        pss = bx if concurrent else ps[4]
        for k in range(2):
            P.pe(lambda e, k=k: e.matmul(pss[:, 0:Q], C.ones[:], sq[:, k, :], start=(k == 0), stop=(k == 1)), r=[C.ones, (sq, k)], w=[pss])
        R1, R2 = pt[2], pt[3]
        P.dve(lambda e: e.tensor_scalar(R1[:], pss[:, 0:Q], 1.0 / 256, EPS, ALU.mult, ALU.add), r=[pss], w=[R1])
        P.act(lambda e: e.activation(R1[:], R1[:], AF.Sqrt), r=[R1], w=[R1])
        P.dve(lambda e: e.reciprocal(R2[:], R1[:]), r=[R1], w=[R2])
        for mo in range(2):
            P.dve(lambda e, mo=mo: e.scalar_tensor_tensor(ob.t[:, mo, :], y2.t[:, mo, :], cols[:, 2 + mo:3 + mo], R2[:], ALU.mult, ALU.mult), r=[(y2, mo), (cols, 1), R2], w=[(ob, mo)])
            P.dma(mix_d[mo * 128:(mo + 1) * 128, c0:c0 + Q], ob.t[:, mo, :], r=[(ob, mo)], w=[("mix", mo, c)])
    if not concurrent:
        P.barrier()
        P.release(m0)
    yield


def rep128(v):
    v = np.asarray(v, np.float32).reshape(1, -1)
    return np.ascontiguousarray(np.broadcast_to(v, (128, v.shape[1])))


def host_layer_inputs(inp, l):
    o = {}
    f = lambda a: np.ascontiguousarray(np.asarray(a, np.float32))
    win = f(inp["w_in"][l])
    wp = np.zeros((D, NPROJ), np.float32)
    wp[:, 0:1536] = win[:, 0:1536]
    wp[:, 1536:2688] = win[:, 1542:2694]
    wp[:, 2688:3072] = win[:, 2694:3078]
    wp[:, 3072:3078] = win[:, 1536:1542]
    wp[:, 3078:3084] = win[:, 3078:3084]
    wp[:, 3084:3090] = win[:, 3084:3090]
    o["win"] = wp
    o["wout"] = f(inp["w_out"][l])
    o["nmix"] = rep128(inp["norm_mix"][l])
    o["nffn"] = rep128(inp["norm_ffn"][l])
    def slay(a):
        return np.ascontiguousarray(f(a).reshape(8, 2, 64).transpose(1, 2, 0).reshape(128, 8))
    o["a_re_s"] = slay(inp["s5_a_re"][l])
    o["a_im_s"] = slay(inp["s5_a_im"][l])
    o["ls_s"] = slay(np.broadcast_to(f(inp["s5_log_step"][l])[:, None], (16, 64)))
    o["a_re_r"] = rep128(f(inp["s5_a_re"][l]).reshape(-1))
    o["a_im_r"] = rep128(f(inp["s5_a_im"][l]).reshape(-1))
    o["ls_r"] = rep128(np.broadcast_to(f(inp["s5_log_step"][l])[:, None], (16, 64)).reshape(-1))
    for nm, src in (("wb_re", inp["s5_b_re"][l]), ("wb_im", inp["s5_b_im"][l])):
        B = f(src)
        wb = np.zeros((128, 8, 128), np.float32)
        for g in range(16):
            j, two = divmod(g, 2)
            r0 = (g % 8) * 16
            wb[r0:r0 + 16, j, two * 64:(two + 1) * 64] = B[g].T
        o[nm] = wb.reshape(128, 1024)
    for nm, src in (("wc_re", inp["s5_c_re"][l]), ("wc_im", inp["s5_c_im"][l])):
        Cm = f(src)
        wc = np.zeros((128, 8, 128), np.float32)
        for g in range(16):
            j, two = divmod(g, 2)
            c0 = (g % 8) * 16
            wc[two * 64:(two + 1) * 64, j, c0:c0 + 16] = Cm[g].T
        o[nm] = wc.reshape(128, 1024)
    o["dcol"] = np.ascontiguousarray(f(inp["s5_d"][l]).reshape(2, 128).T)
    o["ncol"] = np.ascontiguousarray(f(inp["s5_norm"][l]).reshape(2, 128).T)
    o["wglu"] = f(inp["s5_w_glu"][l])
    o["ssd_cw"] = np.ascontiguousarray(f(inp["ssd_conv_w"][l]).reshape(4, 7, 128).transpose(2, 1, 0))
    o["ssd_cb"] = np.ascontiguousarray(f(inp["ssd_conv_b"][l]).reshape(7, 128).T)
    o["ssd_dtb"] = rep128(inp["ssd_dt_bias"][l])
    o["ssd_alog"] = rep128(inp["ssd_a_log"][l])
    o["ssd_drep"] = rep128(np.repeat(f(inp["ssd_d"][l]), 64))
    o["ssd_nw"] = rep128(inp["ssd_norm"][l])
    o["gdn_cw"] = np.ascontiguousarray(f(inp["gdn_conv_w"][l]).reshape(4, 9, 128).transpose(2, 1, 0))
    o["gdn_alog"] = rep128(inp["gdn_a_log"][l])
    o["gdn_dtb"] = rep128(inp["gdn_dt_bias"][l])
    o["gdn_nw"] = rep128(np.tile(f(inp["gdn_norm"][l]), 6))
    return o


F32R = mybir.dt.float32r
USE_F32R = False


def mm(P, out, lhsT, rhs, r, w, start=True, stop=True, exact=False):
    if USE_F32R and not exact and lhsT.dtype == F32 and rhs.dtype == F32:
        lhsT = lhsT.bitcast(F32R)
        rhs = rhs.bitcast(F32R)
    P.pe(lambda e: e.matmul(out, lhsT, rhs, start=start, stop=stop), r=r, w=w)


def tr(P, C, out, in_, n, r, w):
    P.pe(lambda e: e.transpose(out, in_, C.ID[0:n, 0:n]), r=list(r) + [C.ID], w=w)


def tt(P, eng, out, a, b, op, r, w):
    P.op(eng, lambda e: e.tensor_tensor(out, a, b, op), r, w)


def ts(P, eng, out, a, s1, s2, op0, op1, r, w):
    if op1 is None:
        P.op(eng, lambda e: e.tensor_scalar(out, a, s1, None, op0), r, w)
    else:
        P.op(eng, lambda e: e.tensor_scalar(out, a, s1, s2, op0, op1), r, w)


def stt(P, out, a, s, b, op0, op1, r, w):
    P.dve(lambda e: e.scalar_tensor_tensor(out, a, s, b, op0, op1), r, w)


def actf(P, out, in_, func, r, w, **kw):
    P.act(lambda e: e.activation(out, in_, func, **kw), r, w)


def conv_silu(P, C, SEQ, proj_d, row0, ntile, cw, cb, dst, xin, acc):
    for i in range(ntile):
        P.dve(lambda e: e.memset(xin[:, 0:3], 0.0), w=[xin])
        P.dma(xin[:, 3:3 + SEQ], proj_d[row0 + i * 128:row0 + (i + 1) * 128, :], w=[xin])
        ts(P, "dve", acc[:], xin[:, 0:SEQ], cw[:, i, 0:1], None, ALU.mult, None, [xin, cw], [acc])
        for k in range(1, 4):
            stt(P, acc[:], xin[:, k:k + SEQ], cw[:, i, k:k + 1], acc[:], ALU.mult, ALU.add, [xin, cw, acc], [acc])
        if cb is not None:
            actf(P, dst[i][:], acc[:], AF.Silu, [acc, cb], [dst[i]], bias=cb[:, i:i + 1])
        else:
            actf(P, dst[i][:], acc[:], AF.Silu, [acc], [dst[i]])


def conv_blk(P, SEQ, proj_d, row0, ntile, cw, cb, dst, xin, acc, t0):
    for i in range(ntile):
        xi = xin[i % 2]
        if t0 == 0:
            P.dve(lambda e, xi=xi: e.memset(xi[:, 0:3], 0.0), w=[xi])
            P.dma(xi[:, 3:131], proj_d[row0 + i * 128:row0 + (i + 1) * 128, 0:128], w=[xi])
        else:
            P.dma(xi[:, 0:131], proj_d[row0 + i * 128:row0 + (i + 1) * 128, t0 - 3:t0 + 128], w=[xi])
        ts(P, "dve", acc[:], xi[:, 0:128], cw[:, i, 0:1], None, ALU.mult, None, [xi, cw], [acc])
        for k in range(1, 4):
            stt(P, acc[:], xi[:, k:k + 128], cw[:, i, k:k + 1], acc[:], ALU.mult, ALU.add, [xi, cw, acc], [acc])
        if cb is not None:
            actf(P, dst[i][:], acc[:], AF.Silu, [acc, cb], [dst[i]], bias=cb[:, i:i + 1])
        else:
            actf(P, dst[i][:], acc[:], AF.Silu, [acc], [dst[i]])


def conv_load(P, proj_d, row0, ntile, xall, t0):
    src = proj_d[row0:row0 + ntile * 128, :].rearrange("(i p) t -> p i t", p=128)
    if t0 == 0:
        P.dve(lambda e: e.memset(xall.t[:, :, 0:3], 0.0), w=[xall])
        P.dma(xall.t[:, :, 3:131], src[:, :, 0:128], w=[xall])
    else:
        P.dma(xall.t[:, :, 0:131], src[:, :, t0 - 3:t0 + 128], w=[xall])


def conv_blk2(P, ntile, cw, cb, dst, xall, acc):
    for i in range(ntile):
        ts(P, "dve", acc[:], xall.t[:, i, 0:128], cw[:, i, 0:1], None, ALU.mult, None, [xall, cw], [acc])
        for k in range(1, 4):
            stt(P, acc[:], xall.t[:, i, k:k + 128], cw[:, i, k:k + 1], acc[:], ALU.mult, ALU.add, [xall, cw, acc], [acc])
        if cb is not None:
            actf(P, dst[i][:], acc[:], AF.Silu, [acc, cb], [dst[i]], bias=cb[:, i:i + 1])
        else:
            actf(P, dst[i][:], acc[:], AF.Silu, [acc], [dst[i]])


def make_masks(P, C):
    def tri(name, cmp_base, cm, step):
        t = P.sb(name, [128, 128], F32)
        P.pool(lambda e: e.memset(t[:], 1.0), w=[t])
        P.pool(lambda e: e.affine_select(t[:], t[:], [[step, 128]], ALU.is_ge, 0.0, base=cmp_base, channel_multiplier=cm), r=[t], w=[t])
        return t
    C.U = tri("U", 0, -1, 1)
    C.Ls = tri("Ls", -1, 1, -1)
    C.L = tri("L", 0, 1, -1)
    C.BD = P.sb("BD", [128, 128], F32)
    P.pool(lambda e: e.memset(C.BD[:], 0.0), w=[C.BD])
    P.pool(lambda e: e.memset(C.BD[0:64, 0:64], 1.0), r=[C.BD], w=[C.BD])
    P.pool(lambda e: e.memset(C.BD[64:128, 64:128], 1.0), r=[C.BD], w=[C.BD])
    C.SEL0 = P.sb("SEL0", [128, 128], F32)
    C.SEL1 = P.sb("SEL1", [128, 128], F32)
    P.pool(lambda e: e.memset(C.SEL0[:], 0.0), w=[C.SEL0])
    P.pool(lambda e: e.memset(C.SEL0[0:64, :], 1.0), r=[C.SEL0], w=[C.SEL0])
    P.pool(lambda e: e.memset(C.SEL1[:], 0.0), w=[C.SEL1])
    P.pool(lambda e: e.memset(C.SEL1[64:128, :], 1.0), r=[C.SEL1], w=[C.SEL1])
    def neg(name, m01, extra=None):
        t = P.sb(name, [128, 128], F32)
        if extra is not None:
            tt(P, "pool", t[:], m01[:], extra[:], ALU.mult, [m01, extra], [t])
            ts(P, "pool", t[:], t[:], 1e30, -1e30, ALU.mult, ALU.add, [t], [t])
        else:
            ts(P, "pool", t[:], m01[:], 1e30, -1e30, ALU.mult, ALU.add, [m01], [t])
        return t
    C.negU = neg("negU", C.U)
    C.U2 = P.sb("U2", [128, 128], F32)
    tt(P, "pool", C.U2[:], C.U[:], C.BD[:], ALU.mult, [C.U, C.BD], [C.U2])
    C.negU2 = neg("negU2", C.U2)
    C.negLs2 = neg("negLs2", C.Ls, C.BD)


def mixer_ssd(P, C, SEQ, proj_d, mix_d, W, concurrent=False):
    m0 = P.mark()
    ps = C.ps
    NB = SEQ // 128
    cw = P.sb("ssd_cw", [128, 7, 4], F32)
    cb = P.sb("ssd_cb", [128, 7], F32)
    P.dma(cw[:], W["ssd_cw"], w=[cw])
    P.dma(cb[:], W["ssd_cb"], w=[cb])
    bc = P.sb("ssd_bc", [128, 4, 384], F32)
    P.dma(bc.t[:, 0, 0:6], W["ssd_dtb"], w=[(bc, 0)])
    P.dma(bc.t[:, 1, 0:6], W["ssd_alog"], w=[(bc, 1)])
    P.dma(bc.t[:, 2, :], W["ssd_drep"], w=[(bc, 2)])
    P.dma(bc.t[:, 3, :], W["ssd_nw"], w=[(bc, 3)])
    actf(P, bc.t[:, 1, 0:6], bc.t[:, 1, 0:6], AF.Exp, [(bc, 1)], [(bc, 1)])
    ts(P, "dve", bc.t[:, 1, 0:6], bc.t[:, 1, 0:6], -1.0, None, ALU.mult, None, [(bc, 1)], [(bc, 1)])
    xall = [P.sb(f"cv_all{i}", [128, 7, 131], F32) for i in range(2)]
    conv_load(P, proj_d, 640, 7, xall[0], 0)
    zall = P.sb("ssd_zall", [128, 3, 128], F32)
    acc = P.sb("cv_acc", [128, 128], F32)
    F = [P.sb(f"ssdF{i}", [128, 128], F32) for i in range(7)]
    zT = [P.sb(f"ssdz{i}", [128, 128], F32) for i in range(3)]
    sm = P.sb("ssd_sm", [6, 128], F32)
    S = P.sb("ssdS", [128, 6, 64], F32)
    P.dve(lambda e: e.memset(S[:], 0.0), w=[S])
    v = P.sb("ssdv", [128, 64], F32)
    E = [P.sb(f"ssdE{i}", [128, 128], F32) for i in range(2)]
    Mt = [P.sb(f"ssdM{i}", [128, 128], F32) for i in range(2)]
    Xtm = P.sb("ssdXtm", [128, 384], F32)
    xdt = P.sb("ssdxdt", [128, 384], F32)
    xdd = P.sb("ssdxdd", [128, 384], F32)
    Btm = P.sb("ssdBtm", [128, 256], F32)
    yd = P.sb("ssdyd", [128, 384], F32)
    y = P.sb("ssdy", [128, 384], F32)
    zs = P.sb("ssdzs", [128, 384], F32)
    junk = P.sb("ssdjunk", [128, 192], F32)
    ssq = P.sb("ssdss", [128, 8], F32)
    ob = P.sb("ssdob", [128, 3, 128], BF16)
    if concurrent:
        b0, b1, b2, b3, b6, b7, o7 = ps[3], ps[4], ps[4], ps[4], ps[5], ps[3], 64
        bR = lambda b: (ps[4], 256 + b * 128)
    else:
        b0, b1, b2, b3, b6, b7, o7 = ps[0], ps[1], ps[2], ps[3], ps[6], ps[7], 0
        bR = lambda b: (ps[4 + b], 0)
    for blk in range(NB):
        t0 = blk * 128
        cs = slice(t0, t0 + 128)
        if blk + 1 < NB:
            conv_load(P, proj_d, 640, 7, xall[(blk + 1) % 2], t0 + 128)
        conv_blk2(P, 7, cw, cb, F, xall[blk % 2], acc)
        P.dma(zall[:], proj_d[256:640, cs].rearrange("(i p) t -> p i t", p=128), w=[zall])
        P.dma(sm[:], proj_d[3072:3078, cs], w=[sm])
        tr(P, C, b0[:, 0:6], sm[0:6, :], 6, [sm], [b0])
        tt(P, "dve", v[:, 0:6], b0[:, 0:6], bc.t[:, 0, 0:6], ALU.add, [b0, (bc, 0)], [v])
        actf(P, v[:, 0:6], v[:, 0:6], AF.Exp, [v], [v])
        actf(P, v[:, 0:6], v[:, 0:6], AF.Ln, [v], [v], bias=C.ones[:, 0:1])
        tt(P, "dve", v[:, 6:12], v[:, 0:6], bc.t[:, 1, 0:6], ALU.mult, [v, (bc, 1)], [v])
        mm(P, b0[:, 8:14], C.U[:], v[:, 6:12], [C.U, v], [b0])
        mm(P, b0[:, 16:22], C.ones[:], v[:, 6:12], [C.ones, v], [b0])
        P.dve(lambda e: e.tensor_copy(v[:, 12:18], b0[:, 8:14]), r=[b0], w=[v])
        ts(P, "dve", v[:, 18:24], v[:, 12:18], -1.0, None, ALU.mult, None, [v], [v])
        P.dve(lambda e: e.tensor_copy(v[:, 24:30], b0[:, 16:22]), r=[b0], w=[v])
        tt(P, "dve", v[:, 30:36], v[:, 24:30], v[:, 12:18], ALU.subtract, [v], [v])
        actf(P, v[:, 30:36], v[:, 30:36], AF.Exp, [v], [v])
        tt(P, "dve", v[:, 30:36], v[:, 30:36], v[:, 0:6], ALU.mult, [v], [v])
        actf(P, v[:, 36:42], v[:, 12:18], AF.Exp, [v], [v])
        actf(P, v[:, 42:48], v[:, 24:30], AF.Exp, [v], [v])
        for i in range(3):
            tr(P, C, b1[:, i * 128:(i + 1) * 128], F[i][:], 128, [F[i]], [b1])
        P.act(lambda e: e.copy(Xtm[:], b1[:, 0:384]), r=[b1], w=[Xtm])
        for h in range(6):
            hs = slice(h * 64, (h + 1) * 64)
            actf(P, xdt[:, hs], Xtm[:, hs], AF.Copy, [Xtm, v], [(xdt, h)], scale=v[:, h:h + 1])
            actf(P, xdd[:, hs], Xtm[:, hs], AF.Copy, [Xtm, v], [(xdd, h)], scale=v[:, 30 + h:31 + h])
        for g in range(2):
            tr(P, C, b2[:, g * 128:(g + 1) * 128], F[3 + g][:], 128, [F[3 + g]], [b2])
        P.dve(lambda e: e.tensor_copy(Btm[:], b2[:, 0:256]), r=[b2], w=[Btm])
        for g in range(2):
            mm(P, b3[:, g * 128:(g + 1) * 128], F[3 + g][:], F[5 + g][:], [F[3 + g], F[5 + g]], [b3])
        for h in range(6):
            g = h // 3
            b = h % 2
            pr_, ro = bR(b)
            mm(P, pr_[:, ro:ro + 128], v[:, 6 + h:7 + h].broadcast_to([128, 128]), C.U[:], [v, C.U], [pr_])
            stt(P, E[b][:], pr_[:, ro:ro + 128], v[:, 18 + h:19 + h], C.negU[:], ALU.add, ALU.add, [pr_, v, C.negU], [E[b]])
            actf(P, E[b][:], E[b][:], AF.Exp, [E[b]], [E[b]])
            tt(P, "dve", Mt[b][:], b3[:, g * 128:(g + 1) * 128], E[b][:], ALU.mult, [b3, E[b]], [Mt[b]])
            hs = slice(h * 64, (h + 1) * 64)
            mm(P, b6[:, hs], Mt[b][:], xdt[:, hs], [Mt[b], (xdt, h)], [b6])
            mm(P, b7[:, o7 + h * 64:o7 + (h + 1) * 64], F[5 + g][:], S[:, h, :], [F[5 + g], (S, h)], [b7])
        P.act(lambda e: e.copy(yd[:], b6[:, 0:384]), r=[b6], w=[yd])
        for h in range(6):
            hs = slice(h * 64, (h + 1) * 64)
            stt(P, y[:, hs], b7[:, o7 + h * 64:o7 + (h + 1) * 64], v[:, 36 + h:37 + h], yd[:, hs], ALU.mult, ALU.add, [b7, v, yd], [(y, h)])
        for g in range(2):
            mm(P, b2[:, 256 + 0:256 + 192] if False else b0[:, 64 + g * 192:64 + (g + 1) * 192], Btm[:, g * 128:(g + 1) * 128], xdd[:, g * 192:(g + 1) * 192],
               [Btm] + [(xdd, h) for h in range(3 * g, 3 * g + 3)], [b0])
        for h in range(6):
            stt(P, S[:, h, :], S[:, h, :], v[:, 42 + h:43 + h], b0[:, 64 + h * 64:64 + (h + 1) * 64], ALU.mult, ALU.add, [(S, h), v, b0], [(S, h)])
        yk = [(y, h) for h in range(6)]
        tt(P, "pool", xdt[:], Xtm[:], bc.t[:, 2, :], ALU.mult, [Xtm, (bc, 2)] + [(xdt, h) for h in range(6)], [(xdt, h) for h in range(6)])
        tt(P, "pool", y[:], y[:], xdt[:], ALU.add, yk + [(xdt, h) for h in range(6)], yk)
        for i in range(3):
            tr(P, C, b1[:, i * 128:(i + 1) * 128], zall.t[:, i, :], 128, [zall], [b1])
        actf(P, zs[:], b1[:, 0:384], AF.Silu, [b1], [zs])
        tt(P, "dve", y[:], y[:], zs[:], ALU.mult, yk + [zs], yk)
        for g in range(2):
            gs = slice(g * 192, (g + 1) * 192)
            actf(P, junk[:], y[:, gs], AF.Square, yk, [junk, (ssq, g)], accum_out=ssq[:, g:g + 1])
            ts(P, "dve", ssq[:, 2 + g:3 + g], ssq[:, g:g + 1], 1.0 / 192, EPS, ALU.mult, ALU.add, [(ssq, g)], [(ssq, g)])
            actf(P, ssq[:, 4 + g:5 + g], ssq[:, 2 + g:3 + g], AF.Sqrt, [(ssq, g)], [(ssq, g)])
            P.dve(lambda e, g=g: e.reciprocal(ssq[:, 6 + g:7 + g], ssq[:, 4 + g:5 + g]), r=[(ssq, g)], w=[(ssq, g)])
            stt(P, y[:, gs], y[:, gs], ssq[:, 6 + g:7 + g], bc.t[:, 3, gs], ALU.mult, ALU.mult, yk + [(ssq, g), (bc, 3)], yk)
        for i in range(3):
            tr(P, C, b2[:, i * 128:(i + 1) * 128], y[:, i * 128:(i + 1) * 128], 128, yk, [b2])
        P.act(lambda e: e.copy(ob[:], b2.t[:, 0:384].rearrange("p (k t) -> p k t", k=3)), r=[b2], w=[ob])
        P.dma(mix_d[256:640, cs].rearrange("(k p) t -> p k t", p=128), ob[:], r=[ob], w=[("mixssd", blk)])
    if not concurrent:
        P.barrier()
        P.release(m0)


def mixer_gdn(P, C, SEQ, proj_d, mix_d, W, concurrent=False):
    m0 = P.mark()
    ps = C.ps
    NBK = 3 if concurrent else 8
    if concurrent:
        ps = [ps[0], ps[1], ps[2], ps[1]]
    NB = SEQ // 128
    cw = P.sb("gdn_cw", [128, 9, 4], F32)
    P.dma(cw[:], W["gdn_cw"], w=[cw])
    bc = P.sb("gdn_bc", [128, 3, 384], F32)
    P.dma(bc.t[:, 0, 0:6], W["gdn_alog"], w=[(bc, 0)])
    P.dma(bc.t[:, 1, 0:6], W["gdn_dtb"], w=[(bc, 1)])
    P.dma(bc.t[:, 2, :], W["gdn_nw"], w=[(bc, 2)])
    actf(P, bc.t[:, 0, 0:6], bc.t[:, 0, 0:6], AF.Exp, [(bc, 0)], [(bc, 0)])
    ts(P, "dve", bc.t[:, 0, 0:6], bc.t[:, 0, 0:6], -1.0, None, ALU.mult, None, [(bc, 0)], [(bc, 0)])
    xall = [P.sb(f"gcv_all{i}", [128, 9, 131], F32) for i in range(2)]
    conv_load(P, proj_d, 1536, 9, xall[0], 0)
    zall = P.sb("gdn_zall", [128, 3, 128], F32)
    acc = P.sb("gcv_acc", [128, 128], F32)
    F = [P.sb(f"gdnF{i}", [128, 128], F32) for i in range(9)]
    zT = [P.sb(f"gdnz{i}", [128, 128], F32) for i in range(3)]
    sm = P.sb("gdn_sm", [12, 128], F32)
    S = [P.sb(f"gdnS{h}", [64, 64], F32) for h in range(6)]
    for h in range(6):
        P.dve(lambda e, h=h: e.memset(S[h][:], 0.0), w=[S[h]])
    v = P.sb("gdnv", [128, 96], F32)
    big = {n: P.sb("gdn_" + n, [128, 384], F32) for n in ("q", "k", "v", "sq", "qn", "kn", "qd", "kbg", "kend", "vb", "o", "zs")}
    v3 = lambda t: t.t[:, :].rearrange("p (h d) -> p h d", h=6)
    hb = lambda c0: v[:, c0:c0 + 6].unsqueeze(2).broadcast_to([128, 6, 64])
    knT = [P.sb(f"knT{h}", [64, 128], F32) for h in range(6)]
    qnT = [P.sb(f"qnT{h}", [64, 128], F32) for h in range(6)]
    qdT = [P.sb(f"qdT{h}", [64, 128], F32) for h in range(6)]
    E = [P.sb(f"gE{h}", [128, 128], F32) for h in range(6)]
    DL = [P.sb(f"gDL{h}", [128, 128], F32) for h in range(6)]
    qkT = [P.sb(f"gqkT{h}", [128, 128], F32) for h in range(6)]
    Pk = [[P.sb(f"gP{h}_{i}", [128, 128], F32) for i in range(2)] for h in range(6)]
    PkT = [[P.sb(f"gPT{h}_{i}", [128, 128], F32) for i in range(2)] for h in range(6)]
    YT = [P.sb(f"gYT{h}", [128, 128], F32) for h in range(6)]
    WT = [P.sb(f"gWT{h}", [64, 128], F32) for h in range(6)]
    Us = [P.sb(f"gU{h}", [128, 64], F32) for h in range(6)]
    vn = [P.sb(f"gvn{h}", [128, 64], F32) for h in range(6)]
    oq = [P.sb(f"goq{h}", [128, 64], F32) for h in range(6)]
    ob = P.sb("gob", [128, 3, 128], BF16)
    allv = [v]
    for blk in range(NB):
        t0 = blk * 128
        cs = slice(t0, t0 + 128)
        if blk + 1 < NB:
            conv_load(P, proj_d, 1536, 9, xall[(blk + 1) % 2], t0 + 128)
        conv_blk2(P, 9, cw, None, F, xall[blk % 2], acc)
        P.dma(zall[:], proj_d[2688:3072, cs].rearrange("(i p) t -> p i t", p=128), w=[zall])
        P.dma(sm[:], proj_d[3078:3090, cs], w=[sm])
        tr(P, C, ps[0][:, 0:12], sm[0:12, :], 12, [sm], [ps[0]])
        actf(P, v[:, 0:6], ps[0][:, 0:6], AF.Sigmoid, [ps[0]], [v])
        tt(P, "dve", v[:, 6:12], ps[0][:, 6:12], bc.t[:, 1, 0:6], ALU.add, [ps[0], (bc, 1)], [v])
        actf(P, v[:, 6:12], v[:, 6:12], AF.Exp, [v], [v])
        actf(P, v[:, 6:12], v[:, 6:12], AF.Ln, [v], [v], bias=C.ones[:, 0:1])
        tt(P, "dve", v[:, 6:12], v[:, 6:12], bc.t[:, 0, 0:6], ALU.mult, [v, (bc, 0)], [v])
        mm(P, ps[0][:, 16:22], C.U2[:], v[:, 6:12], [C.U2, v], [ps[0]])
        mm(P, ps[0][:, 24:30], C.BD[:], v[:, 6:12], [C.BD, v], [ps[0]])
        mm(P, ps[0][:, 32:38], C.SEL0[:], v[:, 6:12], [C.SEL0, v], [ps[0]])
        mm(P, ps[0][:, 40:46], C.SEL1[:], v[:, 6:12], [C.SEL1, v], [ps[0]])
        P.dve(lambda e: e.tensor_copy(v[:, 12:18], ps[0][:, 16:22]), r=[ps[0]], w=[v])
        ts(P, "dve", v[:, 18:24], v[:, 12:18], -1.0, None, ALU.mult, None, [v], [v])
        tt(P, "dve", v[:, 30:36], ps[0][:, 24:30], v[:, 12:18], ALU.subtract, [ps[0], v], [v])
        actf(P, v[:, 30:36], v[:, 30:36], AF.Exp, [v], [v])
        actf(P, v[:, 36:42], v[:, 12:18], AF.Exp, [v], [v])
        actf(P, v[:, 42:48], ps[0][:, 32:38], AF.Exp, [ps[0]], [v])
        actf(P, v[:, 48:54], ps[0][:, 40:46], AF.Exp, [ps[0]], [v])
        ts(P, "dve", v[:, 78:84], v[:, 0:6], -1.0, None, ALU.mult, None, [v], [v])
        for n_, base, bank in (("q", 0, 1), ("k", 3, 2), ("v", 6, 3)):
            for i in range(3):
                tr(P, C, ps[bank][:, i * 128:(i + 1) * 128], F[base + i][:], 128, [F[base + i]], [ps[bank]])
            P.act(lambda e, n_=n_, bank=bank: e.copy(big[n_][:], ps[bank][:, 0:384]), r=[ps[bank]], w=[big[n_]])
        for n_, c_ss, c_r, sc in (("q", 54, 60, 0.125), ("k", 66, 72, 1.0)):
            tt(P, "pool", big["sq"][:], big[n_][:], big[n_][:], ALU.mult, [big[n_]], [big["sq"]])
            P.dve(lambda e, c_ss=c_ss: e.tensor_reduce(v[:, c_ss:c_ss + 6], v3(big["sq"]), AX.X, ALU.add), r=[big["sq"]], w=[v])
            ts(P, "dve", v[:, c_ss:c_ss + 6], v[:, c_ss:c_ss + 6], EPS, None, ALU.add, None, [v], [v])
            actf(P, v[:, c_ss:c_ss + 6], v[:, c_ss:c_ss + 6], AF.Sqrt, [v], [v])
            P.dve(lambda e, c_ss=c_ss, c_r=c_r: e.reciprocal(v[:, c_r:c_r + 6], v[:, c_ss:c_ss + 6]), r=[v], w=[v])
            if sc != 1.0:
                ts(P, "dve", v[:, c_r:c_r + 6], v[:, c_r:c_r + 6], sc, None, ALU.mult, None, [v], [v])
        tt(P, "dve", v3(big["qn"]), v3(big["q"]), hb(60), ALU.mult, [big["q"], v], [big["qn"]])
        tt(P, "dve", v3(big["kn"]), v3(big["k"]), hb(72), ALU.mult, [big["k"], v], [big["kn"]])
        tt(P, "pool", v3(big["qd"]), v3(big["qn"]), hb(36), ALU.mult, [big["qn"], v], [big["qd"]])
        tt(P, "pool", v3(big["kend"]), v3(big["kn"]), hb(30), ALU.mult, [big["kn"], v], [big["kend"]])
        tt(P, "dve", v3(big["kbg"]), v3(big["kn"]), hb(0), ALU.mult, [big["kn"], v], [big["kbg"]])
        tt(P, "dve", v3(big["kbg"]), v3(big["kbg"]), hb(36), ALU.mult, [big["kbg"], v], [big["kbg"]])
        tt(P, "pool", v3(big["vb"]), v3(big["v"]), hb(0), ALU.mult, [big["v"], v], [big["vb"]])
        for h in range(6):
            hs = slice(h * 64, (h + 1) * 64)
            for src, dstl, col in ((big["kn"], knT, 0), (big["qn"], qnT, 128), (big["qd"], qdT, 256)):
                tr(P, C, ps[1][0:64, col:col + 128], src[:, hs], 128, [src], [ps[1]])
            P.act(lambda e, h=h: e.copy(knT[h][:], ps[1][0:64, 0:128]), r=[ps[1]], w=[knT[h]])
            P.dve(lambda e, h=h: e.tensor_copy(qnT[h][:], ps[1][0:64, 128:256]), r=[ps[1]], w=[qnT[h]])
            P.act(lambda e, h=h: e.copy(qdT[h][:], ps[1][0:64, 256:384]), r=[ps[1]], w=[qdT[h]])
        H6 = range(6)
        hsl = [slice(h * 64, (h + 1) * 64) for h in H6]
        bk = lambda stage, h: ps[(2 * stage + (h % 2)) % NBK]
        for h in H6:
            pb = bk(1, h)
            mm(P, pb[:, 0:128], v[:, 6 + h:7 + h].broadcast_to([128, 128]), C.U2[:], [v, C.U2], [pb])
            stt(P, E[h][:], pb[:, 0:128], v[:, 18 + h:19 + h], C.negU2[:], ALU.add, ALU.add, [pb, v, C.negU2], [E[h]])
            actf(P, E[h][:], E[h][:], AF.Exp, [E[h]], [E[h]])
            stt(P, DL[h][:], pb[:, 0:128], -1.0, C.negLs2[:], ALU.mult, ALU.add, [pb, C.negLs2], [DL[h]])
            actf(P, DL[h][:], DL[h][:], AF.Exp, [DL[h], v], [DL[h]], bias=v[:, 12 + h:13 + h])
        for h in H6:
            pb = bk(2, h)
            mm(P, pb[:, 0:128], knT[h][:], qnT[h][:], [knT[h], qnT[h]], [pb])
            mm(P, pb[:, 128:256], knT[h][:], knT[h][:], [knT[h]], [pb])
            tt(P, "dve", qkT[h][:], pb[:, 0:128], E[h][:], ALU.mult, [pb, E[h]], [qkT[h]])
            stt(P, Pk[h][0][:], pb[:, 128:256], v[:, 78 + h:79 + h], DL[h][:], ALU.mult, ALU.mult, [pb, v, DL[h]], [Pk[h][0]])
        for h in H6:
            pb = bk(3, h)
            tr(P, C, pb[:, 0:128], Pk[h][0][:], 128, [Pk[h][0]], [pb])
            P.act(lambda e, h=h, pb=pb: e.copy(PkT[h][0][:], pb[:, 0:128]), r=[pb], w=[PkT[h][0]])
            tt(P, "dve", YT[h][:], pb[:, 0:128], C.ID[:], ALU.add, [pb, C.ID], [YT[h]])
        cur = 0
        for lvl in range(1, 6):
            nxt = 1 - cur
            for h in H6:
                pb = bk(4 + 2 * lvl, h)
                mm(P, pb[:, 0:128], PkT[h][cur][:], Pk[h][cur][:], [PkT[h][cur], Pk[h][cur]], [pb])
                if lvl < 5:
                    mm(P, pb[:, 128:256], Pk[h][cur][:], PkT[h][cur][:], [PkT[h][cur], Pk[h][cur]], [pb])
                P.act(lambda e, h=h, pb=pb, nxt=nxt: e.copy(Pk[h][nxt][:], pb[:, 0:128]), r=[pb], w=[Pk[h][nxt]])
                if lvl < 5:
                    P.dve(lambda e, h=h, pb=pb, nxt=nxt: e.tensor_copy(PkT[h][nxt][:], pb[:, 128:256]), r=[pb], w=[PkT[h][nxt]])
            for h in H6:
                pb = bk(5 + 2 * lvl, h)
                mm(P, pb[:, 0:128], Pk[h][nxt][:], YT[h][:], [Pk[h][nxt], YT[h]], [pb])
                tt(P, "dve", YT[h][:], YT[h][:], pb[:, 0:128], ALU.add, [YT[h], pb], [YT[h]])
            cur = nxt
        for h in H6:
            pb = bk(0, h)
            mm(P, pb[:, 0:64], YT[h][:], big["vb"][:, hsl[h]], [YT[h], big["vb"]], [pb])
            mm(P, pb[0:64, 128:256], big["kbg"][:, hsl[h]], YT[h][:], [YT[h], big["kbg"]], [pb])
            P.act(lambda e, h=h, pb=pb: e.copy(Us[h][:], pb[:, 0:64]), r=[pb], w=[Us[h]])
            P.dve(lambda e, h=h, pb=pb: e.tensor_copy(WT[h][:], pb[0:64, 128:256]), r=[pb], w=[WT[h]])
        for half in range(2):
            r0 = 64 * half
            rs = slice(r0, r0 + 64)
            for h in H6:
                pb = bk(1, h)
                mm(P, pb[:, 0:64], WT[h][:], S[h][:], [WT[h], S[h]], [pb])
                mm(P, pb[:, 64:128], qdT[h][:], S[h][:], [qdT[h], S[h]], [pb])
                tt(P, "dve", vn[h][rs, :], Us[h][rs, :], pb[rs, 0:64], ALU.subtract, [Us[h], pb], [(vn[h], half)])
                P.act(lambda e, h=h, pb=pb, rs=rs: e.copy(oq[h][rs, :], pb[rs, 64:128]), r=[pb], w=[(oq[h], half)])
            for h in H6:
                pb = bk(2, h)
                mm(P, pb[0:64, 0:64], big["kend"][rs, hsl[h]], vn[h][rs, :], [big["kend"], (vn[h], half)], [pb])
                stt(P, S[h][:], S[h][:], v[0:64, 42 + 6 * half + h:43 + 6 * half + h], pb[0:64, 0:64], ALU.mult, ALU.add, [S[h], v, pb], [S[h]])
        for h in H6:
            pb = bk(3, h)
            mm(P, pb[:, 0:64], qkT[h][:], vn[h][:], [qkT[h], (vn[h], 0), (vn[h], 1)], [pb])
            tt(P, "dve", big["o"][:, hsl[h]], oq[h][:], pb[:, 0:64], ALU.add, [(oq[h], 0), (oq[h], 1), pb], [(big["o"], h, 0), (big["o"], h, 1)])
        ok = [(big["o"], h, k) for h in range(6) for k in range(2)]
        tt(P, "pool", big["sq"][:], big["o"][:], big["o"][:], ALU.mult, ok, [big["sq"]])
        P.dve(lambda e: e.tensor_reduce(v[:, 84:90], v3(big["sq"]), AX.X, ALU.add), r=[big["sq"]], w=[v])
        ts(P, "dve", v[:, 84:90], v[:, 84:90], 1.0 / 64, EPS, ALU.mult, ALU.add, [v], [v])
        actf(P, v[:, 84:90], v[:, 84:90], AF.Sqrt, [v], [v])
        P.dve(lambda e: e.reciprocal(v[:, 90:96], v[:, 84:90]), r=[v], w=[v])
        tt(P, "dve", v3(big["qn"]), v3(big["o"]), hb(90), ALU.mult, ok + [v, big["qn"]], [big["qn"]])
        tt(P, "pool", big["qn"][:], big["qn"][:], bc.t[:, 2, :], ALU.mult, [big["qn"], (bc, 2)], [big["qn"]])
        for i in range(3):
            tr(P, C, ps[1][:, i * 128:(i + 1) * 128], zall.t[:, i, :], 128, [zall], [ps[1]])
        actf(P, big["zs"][:], ps[1][:, 0:384], AF.Silu, [ps[1]], [big["zs"]])
        tt(P, "dve", big["qn"][:], big["qn"][:], big["zs"][:], ALU.mult, [big["qn"], big["zs"]], [big["qn"]])
        for i in range(3):
            tr(P, C, ps[2][:, i * 128:(i + 1) * 128], big["qn"][:, i * 128:(i + 1) * 128], 128, [big["qn"]], [ps[2]])
        P.act(lambda e: e.copy(ob[:], ps[2].t[:, 0:384].rearrange("p (k t) -> p k t", k=3)), r=[ps[2]], w=[ob])
        P.dma(mix_d[640:1024, cs].rearrange("(k p) t -> p k t", p=128), ob[:], r=[ob], w=[("mixgdn", blk)])
    if not concurrent:
        P.barrier()
        P.release(m0)


SEQ_FULL = 4096
N_CORES = 8
_LKEYS = None


def build_program(SEQ, layer_shapes):
    nc = bass.Bass("TRN2", target_bir_lowering=False)
    x_d = nc.dram_tensor("x", [SEQ, D], F32, kind="ExternalInput").ap()
    Ws = []
    for l in range(2):
        Ws.append({k: nc.dram_tensor(f"L{l}_{k}", list(shp), F32, kind="ExternalInput").ap() for k, shp in layer_shapes.items()})
    ffg = nc.dram_tensor("ff_g", [1, D, DFF], F32, kind="ExternalInput").ap()
    ffu = nc.dram_tensor("ff_u", [1, D, DFF], F32, kind="ExternalInput").ap()
    ffd = nc.dram_tensor("ff_d", [1, DFF, D], F32, kind="ExternalInput").ap()
    mog = nc.dram_tensor("moe_g", [NE, D, DFF], F32, kind="ExternalInput").ap()
    mou = nc.dram_tensor("moe_u", [NE, D, DFF], F32, kind="ExternalInput").ap()
    mod = nc.dram_tensor("moe_d", [NE, DFF, D], F32, kind="ExternalInput").ap()
    mor = nc.dram_tensor("moe_r", [D, NE], F32, kind="ExternalInput").ap()
    nfin = nc.dram_tensor("nfin", [128, D], F32, kind="ExternalInput").ap()
    out_d = nc.dram_tensor("out", [SEQ, D], F32, kind="ExternalOutput").ap()
    proj_d = nc.dram_tensor("proj_s", [NPROJ, SEQ], F32).ap()
    mix_d = nc.dram_tensor("mix_s", [D, SEQ], BF16).ap()
    xres = nc.dram_tensor("xres_s", [SEQ, D], F32).ap()
    P = Prog(nc)
    C = Ctx()
    setup_common(P, C)
    make_masks(P, C)
    P.barrier()
    for l in range(2):
        W = Ws[l]
        src = x_d if l == 0 else xres
        phase_inproj(P, C, SEQ, src, W["nmix"], W["win"], proj_d)
        mc = P.mark()
        g5 = mixer_s5(P, C, SEQ, proj_d, mix_d, W, concurrent=True, banks=(6, 7))
        next(g5)
        P.stream_begin()
        next(g5)
        st_s5 = P.stream_end()
        P.stream_begin()
        mixer_ssd(P, C, SEQ, proj_d, mix_d, W, concurrent=True)
        st_ssd = P.stream_end()
        P.stream_begin()
        mixer_gdn(P, C, SEQ, proj_d, mix_d, W, concurrent=True)
        st_gdn = P.stream_end()
        P.merge([st_s5, st_ssd, st_gdn])
        P.barrier()
        P.release(mc)
        phase_outproj(P, C, SEQ, src, xres, mix_d, W["wout"])
        if l == 0:
            phase_ffn(P, C, SEQ, xres, xres, W["nffn"], ffg, ffu, ffd, 1)
        else:
            phase_ffn(P, C, SEQ, xres, out_d, W["nffn"], mog, mou, mod, NE, wr_d=mor, nfin_d=nfin)
    finals = [o for e in ENGS for o in P.ops[e] if o.is_dma]
    P.emit(finals[-64:])
    return nc, P


def kernel(**inp):
    inp = {k: np.asarray(v) for k, v in inp.items()}
    x = np.ascontiguousarray(inp["x"], dtype=np.float32)
    B, SEQ, _ = x.shape
    layers = [host_layer_inputs(inp, l) for l in range(2)]
    shapes = {k: v.shape for k, v in layers[0].items()}
    nc, P = build_program(SEQ, shapes)
    f = lambda a: np.ascontiguousarray(np.asarray(a, np.float32))
    common = {}
    for l in range(2):
        for k, v in layers[l].items():
            common[f"L{l}_{k}"] = np.ascontiguousarray(v, dtype=np.float32)
    common["ff_g"] = f(inp["ff_w_gate"])
    common["ff_u"] = f(inp["ff_w_up"])
    common["ff_d"] = f(inp["ff_w_down"])
    common["moe_g"] = f(inp["moe_w_gate"][0])
    common["moe_u"] = f(inp["moe_w_up"][0])
    common["moe_d"] = f(inp["moe_w_down"][0])
    common["moe_r"] = f(inp["moe_router"][0])
    common["nfin"] = rep128(inp["norm_final"])
    in_maps = []
    for c in range(B):
        m = dict(common)
        m["x"] = np.ascontiguousarray(x[c])
        in_maps.append(m)
    res = run_bass_kernel_spmd(nc, in_maps, core_ids=list(range(B)))
    return np.stack([np.asarray(r["out"], dtype=np.float32) for r in res.results], axis=0)
```
